# Optimizing a Trainium2 kernel written in Bass

```python
import math
import jax, jax.numpy as jnp
from jax import lax
import numpy as np

D_MODEL = 1024
BATCH = 8
SEQ = 8192
DEPTH = 1

N_META = 16
BLOCK = 128
META_PAD = (-N_META) % BLOCK
MLA_HEADS = 4
QK_NOPE = 128
QK_ROPE = 64
V_DIM = 128
Q_LORA = 256
KV_LORA = 256
ROPE_THETA = 10000.0
SB_HEADS = 8
SB_DIM = 64
MLA_W = MLA_HEADS * V_DIM
SB_W = SB_HEADS * SB_DIM
MIX_W = MLA_W + SB_W
IN_SPLITS = (Q_LORA, KV_LORA, QK_ROPE, SB_W, SB_W, SB_W)
IN_W = Q_LORA + KV_LORA + QK_ROPE + 3 * SB_W
N_EXPERTS = 32
TOP_K = 4
D_FF = 1024
SWIGLU_LIMIT = 7.0
SWIGLU_ALPHA = 1.702
EXPERT_BLOCK = 256
DN_ALPHA = (2 * DEPTH) ** 0.25
DN_BETA = (8 * DEPTH) ** -0.25
LN_EPS = 1e-5
RMS_EPS = 1e-6
NEG_INF = -1e30

kernel_name = 'hybrid_mla_stickbreak_moe_deepnorm'


def rms_norm(x, g):
    xf = x.astype(jnp.float32)
    y = xf * lax.rsqrt(jnp.mean(xf * xf, axis=-1, keepdims=True) + RMS_EPS) * g.astype(jnp.float32)
    return y.astype(x.dtype)


def layer_norm(x, g, b):
    xf = x.astype(jnp.float32)
    mu = jnp.mean(xf, axis=-1, keepdims=True)
    xc = xf - mu
    var = jnp.mean(xc * xc, axis=-1, keepdims=True)
    y = xc * lax.rsqrt(var + LN_EPS) * g.astype(jnp.float32) + b.astype(jnp.float32)
    return y.astype(x.dtype)


def rope_tables(length):
    half = QK_ROPE // 2
    freqs = ROPE_THETA ** (-jnp.arange(half, dtype=jnp.float32) * 2.0 / QK_ROPE)
    ang = jnp.arange(length, dtype=jnp.float32)[:, None] * freqs[None, :]
    return jnp.cos(ang)[:, None, :], jnp.sin(ang)[:, None, :]


def rope(x, cos, sin):
    half = x.shape[-1] // 2
    x1 = x[..., :half].astype(jnp.float32)
    x2 = x[..., half:].astype(jnp.float32)
    return jnp.concatenate([x1 * cos - x2 * sin, x2 * cos + x1 * sin], axis=-1).astype(x.dtype)


def to_blocks(t):
    b, lp, h, d = t.shape
    return t.reshape(b, lp // BLOCK, BLOCK, h, d).transpose(1, 0, 2, 3, 4)


def from_blocks(t):
    nb, b, q, h, d = t.shape
    return t.transpose(1, 0, 2, 3, 4).reshape(b, nb * q, h, d)


def mla_attention(q, k, v):
    lp = q.shape[1]
    k_pos = jnp.arange(lp)
    scale = (QK_NOPE + QK_ROPE) ** -0.5

    def one_block(args):
        qb, bi = args
        q_pos = bi * BLOCK + jnp.arange(BLOCK)
        valid = (k_pos[None, :] <= q_pos[:, None]) & (k_pos[None, :] >= META_PAD)
        s = jnp.einsum('bqhd,bkhd->bhqk', qb, k).astype(jnp.float32) * scale
        p = jax.nn.softmax(jnp.where(valid, s, NEG_INF), axis=-1)
        return jnp.einsum('bhqk,bkhd->bqhd', p.astype(v.dtype), v)

    out = lax.map(one_block, (to_blocks(q), jnp.arange(lp // BLOCK)))
    return from_blocks(out)


def stick_breaking_attention(q, k, v):
    lp = q.shape[1]
    k_pos = jnp.arange(lp)
    scale = SB_DIM ** -0.5

    def one_block(args):
        qb, bi = args
        q_pos = bi * BLOCK + jnp.arange(BLOCK)
        valid = (k_pos[None, :] < q_pos[:, None]) & (k_pos[None, :] >= META_PAD)
        z = jnp.einsum('bqhd,bkhd->bhqk', qb, k).astype(jnp.float32) * scale
        log_keep = jnp.where(valid, jax.nn.log_sigmoid(-z), 0.0)
        later = lax.cumsum(log_keep, axis=3, reverse=True) - log_keep
        w = jnp.where(valid, jnp.exp(jax.nn.log_sigmoid(z) + later), 0.0)
        return jnp.einsum('bhqk,bkhd->bqhd', w.astype(v.dtype), v)

    out = lax.map(one_block, (to_blocks(q), jnp.arange(lp // BLOCK)))
    return from_blocks(out)


def token_mixing(h, cos, sin, w_in, q_norm_g, w_uq, kv_norm_g, w_ukv, mla_out_g, sb_out_g, w_o):
    b, l, _ = h.shape
    split_at = np.cumsum(IN_SPLITS)[:-1].tolist()
    c_q, c_kv, k_r, sb_q, sb_k, sb_v = jnp.split(h @ w_in, split_at, axis=-1)
    q = (rms_norm(c_q, q_norm_g) @ w_uq).reshape(b, l, MLA_HEADS, QK_NOPE + QK_ROPE)
    q = jnp.concatenate([q[..., :QK_NOPE], rope(q[..., QK_NOPE:], cos, sin)], axis=-1)
    kv = (rms_norm(c_kv, kv_norm_g) @ w_ukv).reshape(b, l, MLA_HEADS, QK_NOPE + V_DIM)
    k_rope = rope(k_r.reshape(b, l, 1, QK_ROPE), cos, sin)
    k = jnp.concatenate([kv[..., :QK_NOPE], jnp.broadcast_to(k_rope, (b, l, MLA_HEADS, QK_ROPE))], axis=-1)
    v = kv[..., QK_NOPE:]

    def pad(t):
        return jnp.pad(t, ((0, 0), (META_PAD, 0), (0, 0), (0, 0)))

    o_mla = mla_attention(pad(q), pad(k), pad(v))[:, META_PAD:].reshape(b, l, MLA_W)
    sq = sb_q.reshape(b, l, SB_HEADS, SB_DIM)
    sk = sb_k.reshape(b, l, SB_HEADS, SB_DIM)
    sv = sb_v.reshape(b, l, SB_HEADS, SB_DIM)
    o_sb = stick_breaking_attention(pad(sq), pad(sk), pad(sv))[:, META_PAD:].reshape(b, l, SB_W)
    o = jnp.concatenate([rms_norm(o_mla, mla_out_g), rms_norm(o_sb, sb_out_g)], axis=-1)
    return o @ w_o


def moe_ffn(h, w_router, b_router, w_gate, b_gate, w_up, b_up, w_down, b_down):
    t, d = h.shape
    tk = t * TOP_K
    logits = (h @ w_router + b_router).astype(jnp.float32)
    top_v, top_i = lax.top_k(logits, TOP_K)
    gates = jax.nn.softmax(top_v, axis=-1).astype(h.dtype)
    flat_e = top_i.reshape(tk)
    order = jnp.argsort(flat_e)
    s_e = flat_e[order]
    s_tok = order // TOP_K
    s_gate = gates.reshape(tk)[order]
    counts = jnp.bincount(flat_e, length=N_EXPERTS)
    padded = (counts + EXPERT_BLOCK - 1) // EXPERT_BLOCK * EXPERT_BLOCK
    start = jnp.cumsum(counts) - counts
    p_end = jnp.cumsum(padded)
    p_start = p_end - padded
    dest = p_start[s_e] + jnp.arange(tk) - start[s_e]
    n_blk = (tk + N_EXPERTS * (EXPERT_BLOCK - 1) + EXPERT_BLOCK - 1) // EXPERT_BLOCK
    buf = jnp.zeros((n_blk * EXPERT_BLOCK, d), h.dtype).at[dest].set(h[s_tok])
    blk_e = jnp.minimum(jnp.sum(jnp.arange(n_blk)[:, None] * EXPERT_BLOCK >= p_end[None, :], axis=1), N_EXPERTS - 1)

    def expert_block(args):
        xb, e = args
        g = jnp.minimum(xb @ w_gate[e] + b_gate[e], SWIGLU_LIMIT)
        u = jnp.clip(xb @ w_up[e] + b_up[e], -SWIGLU_LIMIT, SWIGLU_LIMIT)
        a = (u + 1.0) * (g * jax.nn.sigmoid(SWIGLU_ALPHA * g))
        return a @ w_down[e] + b_down[e]

    y_buf = lax.map(expert_block, (buf.reshape(n_blk, EXPERT_BLOCK, d), blk_e)).reshape(n_blk * EXPERT_BLOCK, d)
    y = y_buf[dest] * s_gate[:, None]
    return jax.ops.segment_sum(y, s_tok, num_segments=t)


def setup_inputs(seed: int = 0) -> dict:
    key = jax.random.key(seed)
    ks = jax.random.split(key, 24)
    f32 = jnp.float32

    def nrm(k, shape, scale):
        return jax.random.normal(k, shape, f32) * scale

    return {
        'x': nrm(ks[0], (BATCH, SEQ, D_MODEL), 1.0),
        'meta_tokens': nrm(ks[1], (N_META, D_MODEL), 1.0),
        'w_in': nrm(ks[2], (DEPTH, D_MODEL, IN_W), D_MODEL ** -0.5),
        'q_norm_g': 1.0 + nrm(ks[3], (DEPTH, Q_LORA), 0.02),
        'w_uq': nrm(ks[4], (DEPTH, Q_LORA, MLA_HEADS * (QK_NOPE + QK_ROPE)), Q_LORA ** -0.5),
        'kv_norm_g': 1.0 + nrm(ks[5], (DEPTH, KV_LORA), 0.02),
        'w_ukv': nrm(ks[6], (DEPTH, KV_LORA, MLA_HEADS * (QK_NOPE + V_DIM)), KV_LORA ** -0.5),
        'mla_out_g': 1.0 + nrm(ks[7], (DEPTH, MLA_W), 0.02),
        'sb_out_g': 1.0 + nrm(ks[8], (DEPTH, SB_W), 0.02),
        'w_o': nrm(ks[9], (DEPTH, MIX_W, D_MODEL), DN_BETA * MIX_W ** -0.5),
        'ln1_g': 1.0 + nrm(ks[10], (DEPTH, D_MODEL), 0.02),
        'ln1_b': nrm(ks[11], (DEPTH, D_MODEL), 0.01),
        'w_router': nrm(ks[12], (DEPTH, D_MODEL, N_EXPERTS), D_MODEL ** -0.5),
        'b_router': nrm(ks[13], (DEPTH, N_EXPERTS), 0.01),
        'w_gate': nrm(ks[14], (DEPTH, N_EXPERTS, D_MODEL, D_FF), D_MODEL ** -0.5),
        'b_gate': nrm(ks[15], (DEPTH, N_EXPERTS, D_FF), 0.01),
        'w_up': nrm(ks[16], (DEPTH, N_EXPERTS, D_MODEL, D_FF), D_MODEL ** -0.5),
        'b_up': nrm(ks[17], (DEPTH, N_EXPERTS, D_FF), 0.01),
        'w_down': nrm(ks[18], (DEPTH, N_EXPERTS, D_FF, D_MODEL), DN_BETA * D_FF ** -0.5),
        'b_down': nrm(ks[19], (DEPTH, N_EXPERTS, D_MODEL), 0.01),
        'ln2_g': 1.0 + nrm(ks[20], (DEPTH, D_MODEL), 0.02),
        'ln2_b': nrm(ks[21], (DEPTH, D_MODEL), 0.01),
    }


def reference(x, meta_tokens, w_in, q_norm_g, w_uq, kv_norm_g, w_ukv, mla_out_g, sb_out_g, w_o,
              ln1_g, ln1_b, w_router, b_router, w_gate, b_gate, w_up, b_up, w_down, b_down,
              ln2_g, ln2_b):
    b = x.shape[0]
    meta = jnp.broadcast_to(meta_tokens[None].astype(x.dtype), (b, N_META, D_MODEL))
    h = jnp.concatenate([meta, x], axis=1)
    l = h.shape[1]
    cos, sin = rope_tables(l)
    for i in range(DEPTH):
        a = token_mixing(h, cos, sin, w_in[i], q_norm_g[i], w_uq[i], kv_norm_g[i], w_ukv[i],
                         mla_out_g[i], sb_out_g[i], w_o[i])
        h = layer_norm(DN_ALPHA * h + a, ln1_g[i], ln1_b[i])
        f = moe_ffn(h.reshape(b * l, D_MODEL), w_router[i], b_router[i], w_gate[i], b_gate[i],
                    w_up[i], b_up[i], w_down[i], b_down[i]).reshape(b, l, D_MODEL)
        h = layer_norm(DN_ALPHA * h + f, ln2_g[i], ln2_b[i])
    return h[:, N_META:]
```

```python
import numpy as np
import concourse.bass as bass
import concourse.mybir as mybir
from concourse.bass_utils import run_bass_kernel_spmd

F32 = mybir.dt.float32
BF16 = mybir.dt.bfloat16
I32 = mybir.dt.int32
U32 = mybir.dt.uint32
ALU = mybir.AluOpType
AF = mybir.ActivationFunctionType

ENGS = ['pe', 'act', 'dve', 'pool', 'sp']
EPOCH = 30000
SETUP_KEYS = ('const', 'w1', 'w2')
DM = 1024
NE = 32
SCALE_MLA = float(192 ** -0.5)
ALPHA = float(2.0 ** 0.25)
LN_EPS = 1e-5
RMS_EPS = 1e-6
SB_BASE = 24576
SB_END = 229344


class Dep:
    __slots__ = ('w', 'r', 'x')

    def __init__(self, excl=False):
        self.w = None
        self.r = {}
        self.x = excl


class _Rec:
    def __getattr__(self, name):
        def f(*a, **k):
            return (name, a, k)
        return f


_REC = _Rec()


class Sched:
    def __init__(self):
        self.prog = {e: [] for e in ENGS}
        self.cnt = {e: 0 for e in ENGS}
        self.epoch = {e: 0 for e in ENGS}
        self.waited = {e: {} for e in ENGS}
        self.dma_cnt = {}
        self.semkeys = []
        for e in ENGS:
            if e != 'sp':
                self.semkeys.append((e, 0))

    def _need(self, eng, reads, writes):
        need = {}

        def add(d):
            if d is None:
                return
            k, v = d
            if need.get(k, 0) < v:
                need[k] = v
        for b in reads:
            add(b.w)
            if b.x:
                for k_, d in b.r.items():
                    if k_[0] != eng:
                        add(d)
        for b in writes:
            add(b.w)
            for d in b.r.values():
                add(d)
        wl = self.waited[eng]
        for k, v in need.items():
            if eng == 'pe' and k[0] == 'pe':
                continue
            if k[0] == 'dma' and k[1] in SETUP_KEYS:
                v = self.dma_cnt[k]
            if wl.get(k, 0) >= v:
                continue
            wl[k] = v
            self.prog[eng].append(('wait', k, v))

    def op(self, eng, fn, reads=(), writes=()):
        self._need(eng, reads, writes)
        if self.cnt[eng] >= EPOCH:
            self.epoch[eng] += 1
            self.cnt[eng] = 0
            self.semkeys.append((eng, self.epoch[eng]))
        self.cnt[eng] += 1
        key = (eng, self.epoch[eng])
        tok = (key, self.cnt[eng])
        self.prog[eng].append(('op', fn(_REC), key))
        for b in reads:
            b.r[key] = tok
        for b in writes:
            b.w = tok
            b.r = {}

    def dma(self, q, fn, key, reads=(), writes=(), n=1):
        self._need(q, reads, writes)
        k = ('dma', key, 'sw' if q == 'pool' else 'hw')
        if k not in self.dma_cnt:
            self.dma_cnt[k] = 0
            self.semkeys.append(k)
        self.dma_cnt[k] += 16 * n
        tok = (k, self.dma_cnt[k])
        self.prog[q].append(('dma', fn(_REC), k))
        for b in reads:
            b.r[k] = tok
        for b in writes:
            b.w = tok
            b.r = {}

    def wait_dma(self, eng, key, q='pool'):
        k = ('dma', key, 'sw' if q == 'pool' else 'hw')
        v = self.dma_cnt.get(k, 0)
        if v and self.waited[eng].get(k, 0) < v:
            self.waited[eng][k] = v
            self.prog[eng].append(('wait', k, v))

    def barrier(self):
        cur = {}
        for e in ENGS:
            if e != 'sp' and self.cnt[e] > 0:
                cur[(e, self.epoch[e])] = self.cnt[e]
        for k, v in self.dma_cnt.items():
            cur[k] = v
        for e in ENGS:
            wl = self.waited[e]
            for k, v in cur.items():
                if e == 'pe' and k[0] == 'pe':
                    continue
                if wl.get(k, 0) >= v:
                    continue
                wl[k] = v
                self.prog[e].append(('wait', k, v))

    def emit(self, block, sems):
        handles = {'pe': 'tensor', 'act': 'scalar', 'dve': 'vector', 'pool': 'gpsimd', 'sp': 'sync'}

        def run(eng_name):
            def body(e):
                for item in self.prog[eng_name]:
                    if item[0] == 'wait':
                        e.wait_ge(sems[item[1]], item[2])
                    elif item[0] == 'op':
                        nm, a, k = item[1]
                        getattr(e, nm)(*a, **k).then_inc(sems[item[2]], 1)
                    else:
                        nm, a, k = item[1]
                        try:
                            getattr(e, nm)(*a, **k).then_inc(sems[item[2]], 16)
                        except Exception:
                            print("DMA FAIL", eng_name, nm, item[2], k)
                            raise
            return body
        for en in ENGS:
            getattr(block, handles[en])(run(en))


class Arena:
    def __init__(self, nc, base, tag):
        self.nc = nc
        self.off = base
        self.tag = tag
        self.n = 0

    def alloc(self, shape, dt):
        sz = int(np.prod(shape[1:])) * (2 if dt == BF16 else 4)
        off = (self.off + 31) // 32 * 32
        self.n += 1
        t = self.nc.alloc_sbuf_tensor_at("%s_%d" % (self.tag, self.n), list(shape), dt, offset=off)
        self.off = off + sz
        assert self.off <= SB_END, (self.tag, self.off)
        return t


def build(NG, CAP, debug=False, phases=('mla0', 'mla1', 'sb0', 'sb1', 'merge', 'moe', 'comb')):
    T = NG * 512
    NT = NG * 4
    KT = T + 16
    NSLOT = NE * CAP
    nc = bass.Bass("TRN2", target_bir_lowering=False)
    S = Sched()

    def din(name, shape, dt=F32):
        return nc.dram_tensor(name, list(shape), dt, kind="ExternalInput")

    def dscratch(name, shape, dt=F32, dbg_out=True):
        return nc.dram_tensor(name, list(shape), dt, kind="ExternalOutput" if (debug and dbg_out) else "Internal")

    x_d = din("x", [T, DM])
    meta_d = din("meta", [16, DM])
    w_in_d = din("w_in", [DM, 2112])
    qg_d = din("qg", [128, 2])
    w_uq_d = din("w_uq", [256, 768])
    kvg_d = din("kvg", [128, 2])
    w_ukv_d = din("w_ukv", [256, 1024])
    gm_d = din("gm", [128, 4])
    gs_d = din("gs", [64, 8])
    w_o_d = din("w_o", [DM, DM])
    ln1g_d = din("ln1g", [128, DM])
    ln1b_d = din("ln1b", [128, DM])
    wr_d = din("wr", [DM, NE])
    br_d = din("br", [1, NE])
    if 'moe' in phases:
        wg_d = din("wg", [NE, DM, DM])
        wu_d = din("wu", [NE, DM, DM])
        wd_d = din("wd", [NE, DM, DM])
    bgu_d = din("bgu", [128, NE, 16])
    bd_d = din("bd", [NE, DM])
    ln2g_d = din("ln2g", [128, DM])
    ln2b_d = din("ln2b", [128, DM])
    ident_d = din("ident", [128, 128])
    mle_d = din("mle", [128, 128])
    mlt_d = din("mlt", [128, 128])
    negu_d = din("negu", [128, 128])
    lst_d = din("lst", [128, 128])
    iota_d = din("iota", [128, NE])
    cos_d = din("cos2", [128, KT])
    sin_d = din("sin2", [128, KT])
    out_d = nc.dram_tensor("out", [T, DM], F32, kind="ExternalOutput")

    omla_d = dscratch("omla", [4, 128, T])
    osb_d = dscratch("osb", [8, 64, T])
    h1_d = dscratch("h1", [T, DM])
    xbuf_d = dscratch("xbuf", [NSLOT + 128, DM], BF16, dbg_out=False)
    ybuf_d = dscratch("ybuf", [NSLOT, DM], dbg_out=False)
    dbg_d = dscratch("dbg", [T, 16]) if debug else None

    AP_ = Arena(nc, SB_BASE, "c")
    ident_b = AP_.alloc([128, 128], BF16)
    ident_f = AP_.alloc([128, 128], F32)
    mle_b = AP_.alloc([128, 128], BF16)
    mlt_b = AP_.alloc([128, 128], BF16)
    negu_b = AP_.alloc([128, 128], BF16)
    lst_b = AP_.alloc([128, 128], BF16)
    ones_b = AP_.alloc([128, 128], BF16)
    negones_b = AP_.alloc([128, 128], BF16)
    zeros_b = AP_.alloc([128, 128], BF16)
    ones_f = AP_.alloc([128, 128], F32)
    zeros512 = AP_.alloc([128, 512], BF16)
    iota_f = AP_.alloc([128, NE], F32)
    dtab = AP_.alloc([128, NT, 4], I32)
    gtab = AP_.alloc([128, NT, 4], F32)
    d_const = Dep()
    d_dtab = Dep()
    d_gtab = Dep()
    PBASE = AP_.off

    pTa = nc.alloc_psum_tensor("pTa", [128, 1024], BF16)
    pTb = nc.alloc_psum_tensor("pTb", [128, 1024], BF16)
    pF = [nc.alloc_psum_tensor("pF%d" % i, [128, 512], F32) for i in range(6)]
    d_pT = [Dep(True), Dep(True)]
    d_pF = [Dep(True) for _ in range(6)]
    pT = [pTa, pTb]

    def cdma(dst, src, cast):
        q = 'pool' if cast else 'sp'
        S.dma(q, lambda e: e.dma_start(out=dst, in_=src), 'const', writes=[d_const])

    cdma(ident_b[:], ident_d[:, :], True)
    cdma(ident_f[:], ident_d[:, :], False)
    cdma(mle_b[:], mle_d[:, :], True)
    cdma(mlt_b[:], mlt_d[:, :], True)
    cdma(negu_b[:], negu_d[:, :], True)
    cdma(lst_b[:], lst_d[:, :], True)
    cdma(iota_f[:], iota_d[:, :], False)
    S.op('pool', lambda e: e.memset(ones_b[:], 1.0), writes=[d_const])
    S.op('pool', lambda e: e.memset(negones_b[:], -1.0), writes=[d_const])
    S.op('pool', lambda e: e.memset(zeros_b[:], 0.0), writes=[d_const])
    S.op('pool', lambda e: e.memset(zeros512[:], 0.0), writes=[d_const])
    S.op('pool', lambda e: e.memset(ones_f[:], 1.0), writes=[d_const])

    d_xbufz = Dep()
    if 'merge' in phases:
        ztile = AP_.alloc([128, 4096], BF16)
        PBASE = AP_.off
        S.op('pool', lambda e: e.memset(ztile[:], 0.0), writes=[d_const])
        nz = (NSLOT + 128) // 512
        xz = xbuf_d[0:nz * 512, :].rearrange("(i p j) d -> i p (j d)", p=128, j=4)
        for i in range(nz):
            S.dma('sp', lambda e, i=i: e.dma_start(out=xz[i], in_=ztile[:]), 'xz', reads=[d_const], writes=[d_xbufz])
        rem = (NSLOT + 128) - nz * 512
        if rem:
            S.dma('sp', lambda e: e.dma_start(out=xbuf_d[nz * 512:nz * 512 + rem, :], in_=ztile[0:rem, 0:DM]), 'xz',
                  reads=[d_const], writes=[d_xbufz])

    x_t = x_d[:, :].rearrange("(g t p) d -> g p t d", p=128, t=4)
    w_in_t = w_in_d[:, :].rearrange("(kc p) c -> p kc c", p=128)

    def load_x_bf(xb, d_xb, g, key):
        if g < 0:
            S.dma('pool', lambda e: e.dma_start(out=xb[0:16, 0, :], in_=meta_d[:, :]), key, writes=[d_xb])
        else:
            S.dma('pool', lambda e: e.dma_start(out=xb[:, :, :], in_=x_t[g]), key, writes=[d_xb])

    def transpose_x(xb, d_xb, XT, d_XT, g):
        ntile = 1 if g < 0 else 4
        np_ = 16 if g < 0 else 128
        for t in range(ntile):
            pi = t % 2
            for c in range(8):
                S.op('pe', lambda e, t=t, c=c, pi=pi: e.transpose(out=pT[pi][:, c * 128:c * 128 + np_],
                                                                 in_=xb[0:np_, t, c * 128:(c + 1) * 128],
                                                                 identity=ident_b[0:np_, 0:np_]),
                     reads=[d_xb, d_const], writes=[d_pT[pi]])
            src = pT[pi][:, :].rearrange("p (c n) -> p c n", c=8)[:, :, 0:np_]
            S.op('dve', lambda e, t=t, src=src: e.tensor_copy(out=XT[:, :, t * 128:t * 128 + np_], in_=src),
                 reads=[d_pT[pi]], writes=[d_XT])

    def proj_fm(pbank, d_pb, wt, col0, XT, d_w, d_XT, N, nk=8, kp=128):
        for kc in range(nk):
            S.op('pe', lambda e, kc=kc: e.matmul(pbank[:, 0:N], lhsT=wt[0:kp, kc, col0:col0 + 128], rhs=XT[0:kp, kc, 0:N],
                                                 start=(kc == 0), stop=(kc == nk - 1)),
                 reads=[d_w, d_XT], writes=[d_pb])

    def rsqrt_mean(dst, d_dst, src_ps, d_src, n, eps, np_=128, N=512):
        S.op('act', lambda e: e.activation(out=dst[0:np_, 0:N], in_=src_ps[0:np_, 0:N], func=AF.Sqrt, bias=float(eps), scale=1.0 / n),
             reads=[d_src], writes=[d_dst])
        S.op('dve', lambda e: e.reciprocal(out=dst[0:np_, 0:N], in_=dst[0:np_, 0:N]), reads=[d_dst], writes=[d_dst])

    def mla_phase(hsel):
        A = Arena(nc, PBASE, "m%d" % hsel)
        heads = (2 * hsel, 2 * hsel + 1)
        KnT = A.alloc([128, 2, KT], BF16); d_KnT = Dep()
        krT = A.alloc([128, KT], BF16); d_krT = Dep()
        V = A.alloc([128, NT + 1, 256], BF16); d_V = Dep()
        win_b = A.alloc([128, 8, 768], BF16); d_win = Dep()
        wq_f = A.alloc([128, 2, 512], F32); d_wqf = Dep()
        wq_b = A.alloc([128, 2, 512], BF16); d_wq = Dep()
        wkv_f = A.alloc([128, 2, 512], F32); d_wkvf = Dep()
        wkv_b = A.alloc([128, 2, 512], BF16); d_wkv = Dep()
        qg = A.alloc([128, 2], F32)
        kvg = A.alloc([128, 2], F32)
        d_g = Dep()
        xb = [A.alloc([128, 4, DM], BF16) for _ in range(2)]; d_xb = [Dep(), Dep()]
        XT = A.alloc([128, 8, 512], BF16); d_XT = Dep()
        cqT = A.alloc([128, 2, 512], BF16); d_cq = Dep()
        ckvT = A.alloc([128, 2, 512], BF16); d_ckv = Dep()
        sqq = A.alloc([128, 2, 512], BF16); d_sqq = Dep()
        sqkv = A.alloc([128, 2, 512], BF16); d_sqkv = Dep()
        rq = A.alloc([128, 512], F32); d_rq = Dep()
        rkv = A.alloc([128, 512], F32); d_rkv = Dep()
        rcol = A.alloc([128, 64], F32); d_rcol = Dep()
        cs = A.alloc([128, 2, 512], F32); d_cs = Dep()
        t1 = A.alloc([128, 512], F32); d_t1 = Dep()
        t2 = A.alloc([128, 512], F32); d_t2 = Dep()
        QnT = A.alloc([128, 2, 512], BF16); d_Qn = Dep()
        QrT = A.alloc([128, 512], BF16); d_Qr = Dep()
        PT = [A.alloc([128, 512], BF16) for _ in range(4)]; d_PT = [Dep() for _ in range(4)]
        rec = A.alloc([128, 512], F32); d_rec = Dep()
        Racc = A.alloc([128, 512], F32); d_Racc = Dep()
        oT = [A.alloc([128, 512], F32) for _ in range(2)]; d_oT = [Dep(), Dep()]

        S.dma('pool', lambda e: e.dma_start(out=win_b[:, :, 0:576], in_=w_in_t[:, :, 0:576]), 'w1', writes=[d_win])
        S.dma('pool', lambda e: e.dma_start(out=win_b[:, :, 576:640], in_=w_in_t[:, :, 512:576]), 'w1', writes=[d_win])
        for dup in range(2):
            o = 640 + dup * 64
            S.dma('pool', lambda e, o=o: e.dma_start(out=win_b[:, :, o:o + 32], in_=w_in_t[:, :, 544:576]), 'w1', writes=[d_win])
            S.dma('pool', lambda e, o=o: e.dma_start(out=win_b[:, :, o + 32:o + 64], in_=w_in_t[:, :, 512:544]), 'w1', writes=[d_win])
        for dup in range(2):
            o = 640 + dup * 64
            S.op('pool', lambda e, o=o: e.tensor_scalar(out=win_b[:, :, o:o + 32], in0=win_b[:, :, o:o + 32], scalar1=-1.0, scalar2=None,
                                                        op0=ALU.mult), reads=[d_win], writes=[d_win])
        S.dma('sp', lambda e: e.dma_start(out=qg[:], in_=qg_d[:, :]), 'w2', writes=[d_g])
        S.dma('sp', lambda e: e.dma_start(out=kvg[:], in_=kvg_d[:, :]), 'w2', writes=[d_g])
        wuq_t = w_uq_d[:, :].rearrange("(kc p) c -> p kc c", p=128)
        wukv_t = w_ukv_d[:, :].rearrange("(kc p) c -> p kc c", p=128)
        hA, hB = heads
        S.dma('sp', lambda e: e.dma_start(out=wq_f[:, :, 0:128], in_=wuq_t[:, :, hA * 192:hA * 192 + 128]), 'w2', writes=[d_wqf])
        S.dma('sp', lambda e: e.dma_start(out=wq_f[:, :, 128:256], in_=wuq_t[:, :, hB * 192:hB * 192 + 128]), 'w2', writes=[d_wqf])
        for i, h in enumerate(heads):
            r0 = h * 192 + 128
            S.dma('sp', lambda e, i=i, r0=r0: e.dma_start(out=wq_f[:, :, 256 + i * 64:320 + i * 64], in_=wuq_t[:, :, r0:r0 + 64]), 'w2', writes=[d_wqf])
            S.dma('sp', lambda e, i=i, r0=r0: e.dma_start(out=wq_f[:, :, 384 + i * 64:416 + i * 64], in_=wuq_t[:, :, r0 + 32:r0 + 64]), 'w2', writes=[d_wqf])
            S.dma('sp', lambda e, i=i, r0=r0: e.dma_start(out=wq_f[:, :, 416 + i * 64:448 + i * 64], in_=wuq_t[:, :, r0:r0 + 32]), 'w2', writes=[d_wqf])
        for i, h in enumerate(heads):
            S.dma('sp', lambda e, i=i, h=h: e.dma_start(out=wkv_f[:, :, i * 128:(i + 1) * 128], in_=wukv_t[:, :, h * 256:h * 256 + 128]), 'w2', writes=[d_wkvf])
            S.dma('sp', lambda e, i=i, h=h: e.dma_start(out=wkv_f[:, :, 256 + i * 128:256 + (i + 1) * 128], in_=wukv_t[:, :, h * 256 + 128:h * 256 + 256]), 'w2', writes=[d_wkvf])
        for kc in range(2):
            S.op('dve', lambda e, kc=kc: e.tensor_scalar(out=wq_b[:, kc, :], in0=wq_f[:, kc, :], scalar1=qg[:, kc:kc + 1], scalar2=None, op0=ALU.mult),
                 reads=[d_wqf, d_g], writes=[d_wq])
            for i in range(2):
                o = 384 + i * 64
                S.op('dve', lambda e, kc=kc, o=o: e.tensor_scalar(out=wq_b[:, kc, o:o + 32], in0=wq_b[:, kc, o:o + 32], scalar1=-1.0, scalar2=None, op0=ALU.mult),
                     reads=[d_wq], writes=[d_wq])
            S.op('dve', lambda e, kc=kc: e.tensor_scalar(out=wkv_b[:, kc, :], in0=wkv_f[:, kc, :], scalar1=kvg[:, kc:kc + 1], scalar2=None, op0=ALU.mult),
                 reads=[d_wkvf, d_g], writes=[d_wkv])

        import os
        CUT = float(os.environ.get('MK_CUT', '99'))
        if CUT <= 1:
            S.barrier()
            return
        load_x_bf(xb[0], d_xb[0], -1, 'xb0')
        pcount = [0]

        def nextp():
            pcount[0] += 1
            return pcount[0] % 4

        for gi, g in enumerate([-1] + list(range(NG))):
            bi = gi % 2
            N = 16 if g < 0 else 512
            ntile = 1 if g < 0 else 4
            k0 = 0 if g < 0 else 16 + g * 512
            if gi + 1 <= NG:
                load_x_bf(xb[1 - bi], d_xb[1 - bi], g + 1, 'xb%d' % (1 - bi))
            S.dma('sp', lambda e, k0=k0, N=N: e.dma_start(out=cs[:, 0, 0:N], in_=cos_d[:, k0:k0 + N]), 'cs', writes=[d_cs])
            S.dma('sp', lambda e, k0=k0, N=N: e.dma_start(out=cs[:, 1, 0:N], in_=sin_d[:, k0:k0 + N]), 'cs', writes=[d_cs])
            transpose_x(xb[bi], d_xb[bi], XT, d_XT, g)
            if g >= 0 and CUT <= 2.1:
                break
            for j in range(2):
                p = nextp()
                proj_fm(pF[p], d_pF[p], win_b, 256 + j * 128, XT, d_win, d_XT, N)
                S.op('dve', lambda e, j=j, p=p: e.tensor_copy(out=ckvT[:, j, 0:N], in_=pF[p][:, 0:N]), reads=[d_pF[p]], writes=[d_ckv])
                S.op('act', lambda e, j=j, p=p: e.activation(out=sqkv[:, j, 0:N], in_=ckvT[:, j, 0:N], func=AF.Square), reads=[d_ckv], writes=[d_sqkv])
            if g >= 0 and CUT <= 2.15:
                break
            if g >= 0:
                for j in range(2):
                    p = nextp()
                    proj_fm(pF[p], d_pF[p], win_b, j * 128, XT, d_win, d_XT, N)
                    S.op('dve', lambda e, j=j, p=p: e.tensor_copy(out=cqT[:, j, 0:N], in_=pF[p][:, 0:N]), reads=[d_pF[p]], writes=[d_cq])
                    S.op('act', lambda e, j=j, p=p: e.activation(out=sqq[:, j, 0:N], in_=cqT[:, j, 0:N], func=AF.Square), reads=[d_cq], writes=[d_sqq])
            if g >= 0 and CUT <= 2.17:
                break
            p = nextp()
            proj_fm(pF[p], d_pF[p], win_b, 512, XT, d_win, d_XT, N)
            S.op('dve', lambda e, p=p: e.tensor_tensor(out=t1[:, 0:N], in0=pF[p][:, 0:N], in1=cs[:, 0, 0:N], op=ALU.mult), reads=[d_pF[p], d_cs], writes=[d_t1])
            p = nextp()
            proj_fm(pF[p], d_pF[p], win_b, 640, XT, d_win, d_XT, N)
            S.op('dve', lambda e, p=p: e.tensor_tensor(out=t2[:, 0:N], in0=pF[p][:, 0:N], in1=cs[:, 1, 0:N], op=ALU.mult), reads=[d_pF[p], d_cs], writes=[d_t2])
            S.op('dve', lambda e, k0=k0: e.tensor_tensor(out=krT[:, k0:k0 + N], in0=t1[:, 0:N], in1=t2[:, 0:N], op=ALU.add), reads=[d_t1, d_t2], writes=[d_krT])
            if g >= 0 and CUT <= 2.2:
                break
            p = nextp()
            for j in range(2):
                S.op('pe', lambda e, j=j, p=p: e.matmul(pF[p][:, 0:N], lhsT=ones_b[:, :], rhs=sqkv[:, j, 0:N], start=(j == 0), stop=(j == 1)),
                     reads=[d_sqkv, d_const], writes=[d_pF[p]])
            rsqrt_mean(rkv, d_rkv, pF[p], d_pF[p], 256.0, RMS_EPS, N=N)
            p = nextp()
            for t in range(ntile):
                nt_ = 16 if g < 0 else 128
                for j in range(2):
                    S.op('pe', lambda e, t=t, j=j, p=p, nt_=nt_: e.matmul(pF[p][0:nt_, 16 * t:16 * t + 16], lhsT=sqkv[:, j, t * 128:t * 128 + nt_], rhs=ones_b[:, 0:16],
                                                                        start=(j == 0), stop=(j == 1)),
                         reads=[d_sqkv, d_const], writes=[d_pF[p]])
            rsqrt_mean(rcol, d_rcol, pF[p], d_pF[p], 256.0, RMS_EPS, np_=(16 if g < 0 else 128), N=16 * ntile)
            if g >= 0:
                p = nextp()
                for j in range(2):
                    S.op('pe', lambda e, j=j, p=p: e.matmul(pF[p][:, 0:N], lhsT=ones_b[:, :], rhs=sqq[:, j, 0:N], start=(j == 0), stop=(j == 1)),
                         reads=[d_sqq, d_const], writes=[d_pF[p]])
                rsqrt_mean(rq, d_rq, pF[p], d_pF[p], 256.0, RMS_EPS, N=N)
            if g >= 0 and CUT <= 2.3:
                break
            for i in range(2):
                p = nextp()
                proj_fm(pF[p], d_pF[p], wkv_b, i * 128, ckvT, d_wkv, d_ckv, N, nk=2)
                S.op('dve', lambda e, i=i, p=p, k0=k0: e.tensor_tensor(out=KnT[:, i, k0:k0 + N], in0=pF[p][:, 0:N], in1=rkv[:, 0:N], op=ALU.mult),
                     reads=[d_pF[p], d_rkv], writes=[d_KnT])
            for t in range(ntile):
                nt_ = 16 if g < 0 else 128
                vt = 0 if g < 0 else 1 + g * 4 + t
                p = nextp()
                for kc in range(2):
                    S.op('pe', lambda e, t=t, kc=kc, p=p, nt_=nt_: e.matmul(pF[p][0:nt_, 0:256], lhsT=ckvT[:, kc, t * 128:t * 128 + nt_], rhs=wkv_b[:, kc, 256:512],
                                                                          start=(kc == 0), stop=(kc == 1)),
                         reads=[d_ckv, d_wkv], writes=[d_pF[p]])
                S.op('act', lambda e, t=t, p=p, nt_=nt_, vt=vt: e.activation(out=V[0:nt_, vt, :], in_=pF[p][0:nt_, 0:256], func=AF.Copy, scale=rcol[0:nt_, 16 * t:16 * t + 1]),
                     reads=[d_pF[p], d_rcol], writes=[d_V])
            if g < 0:
                if CUT <= 2:
                    break
                continue
            if CUT <= 2.4:
                break
            for i in range(2):
                p = nextp()
                proj_fm(pF[p], d_pF[p], wq_b, i * 128, cqT, d_wq, d_cq, N, nk=2)
                S.op('dve', lambda e, i=i, p=p: e.scalar_tensor_tensor(out=QnT[:, i, :], in0=pF[p][:, :], scalar=SCALE_MLA, in1=rq[:, :], op0=ALU.mult, op1=ALU.mult),
                     reads=[d_pF[p], d_rq], writes=[d_Qn])
            p = nextp()
            proj_fm(pF[p], d_pF[p], wq_b, 256, cqT, d_wq, d_cq, N, nk=2)
            S.op('dve', lambda e, p=p: e.tensor_tensor(out=t1[:, :], in0=pF[p][:, :], in1=cs[:, 0, :], op=ALU.mult), reads=[d_pF[p], d_cs], writes=[d_t1])
            p = nextp()
            proj_fm(pF[p], d_pF[p], wq_b, 384, cqT, d_wq, d_cq, N, nk=2)
            S.op('dve', lambda e, p=p: e.tensor_tensor(out=t2[:, :], in0=pF[p][:, :], in1=cs[:, 1, :], op=ALU.mult), reads=[d_pF[p], d_cs], writes=[d_t2])
            S.op('dve', lambda e: e.tensor_tensor(out=t1[:, :], in0=t1[:, :], in1=t2[:, :], op=ALU.add), reads=[d_t1, d_t2], writes=[d_t1])
            S.op('dve', lambda e: e.scalar_tensor_tensor(out=QrT[:, :], in0=t1[:, :], scalar=SCALE_MLA, in1=rq[:, :], op0=ALU.mult, op1=ALU.mult),
                 reads=[d_t1, d_rq], writes=[d_Qr])
            if CUT <= 3:
                break
            SK = 2
            tiles = []
            for i in range(2):
                blocks = [(-1, 0)] + [(kb, 0) for kb in range(4 * g)] + [(4 * g + j, j) for j in range(4)]
                for bi_, (kb, jq) in enumerate(blocks):
                    tiles.append((i, kb, jq, bi_ == 0, bi_ == len(blocks) - 1))
            pO, d_pO = pF[4], d_pF[4]
            pR, d_pR = pF[5], d_pF[5]

            def stA(ti):
                i, kb, jq, first, last = tiles[ti]
                nk_ = 16 if kb < 0 else 128
                kc0 = 0 if kb < 0 else 16 + kb * 128
                q0 = jq * 128
                pS, d_pS = pF[1 + ti % 3], d_pF[1 + ti % 3]
                pt, d_pt = PT[ti % 4], d_PT[ti % 4]
                S.op('pe', lambda e: e.matmul(pS[0:nk_, q0:512], lhsT=KnT[:, i, kc0:kc0 + nk_], rhs=QnT[:, i, q0:512], start=True, stop=False),
                     reads=[d_KnT, d_Qn], writes=[d_pS])
                S.op('pe', lambda e: e.matmul(pS[0:nk_, q0:512], lhsT=krT[i * 64:(i + 1) * 64, kc0:kc0 + nk_], rhs=QrT[i * 64:(i + 1) * 64, q0:512],
                                              start=False, stop=True), reads=[d_krT, d_Qr], writes=[d_pS])
                S.op('act', lambda e: e.activation(out=pt[0:nk_, q0:512], in_=pS[0:nk_, q0:512], func=AF.Exp), reads=[d_pS], writes=[d_pt])
                if kb >= 4 * g:
                    S.op('dve', lambda e: e.tensor_tensor(out=pt[:, q0:q0 + 128], in0=pt[:, q0:q0 + 128], in1=mle_b[:, :], op=ALU.mult),
                         reads=[d_pt, d_const], writes=[d_pt])

            def stB(ti):
                i, kb, jq, first, last = tiles[ti]
                nk_ = 16 if kb < 0 else 128
                vt = 0 if kb < 0 else 1 + kb
                q0 = jq * 128
                pt, d_pt = PT[ti % 4], d_PT[ti % 4]
                S.op('pe', lambda e: e.matmul(pO[:, q0:512], lhsT=V[0:nk_, vt, i * 128:(i + 1) * 128], rhs=pt[0:nk_, q0:512], start=first, stop=last,
                                              skip_group_check=True), reads=[d_V, d_pt], writes=[d_pO])
                if first:
                    S.op('dve', lambda e: e.memset(Racc[:, :], 0.0), writes=[d_Racc])
                S.op('dve', lambda e: e.tensor_tensor(out=Racc[0:nk_, q0:512], in0=Racc[0:nk_, q0:512], in1=pt[0:nk_, q0:512], op=ALU.add),
                     reads=[d_Racc, d_pt], writes=[d_Racc])
                if last:
                    S.op('pe', lambda e: e.matmul(pR[:, :], lhsT=ones_f[:, :], rhs=Racc[:, :], start=True, stop=True), reads=[d_const, d_Racc], writes=[d_pR])
                    S.op('dve', lambda e: e.reciprocal(out=rec[:, :], in_=pR[:, :]), reads=[d_pR], writes=[d_rec])
                    S.op('dve', lambda e: e.tensor_tensor(out=oT[i][:, :], in0=pO[:, :], in1=rec[:, :], op=ALU.mult), reads=[d_pO, d_rec], writes=[d_oT[i]])
                    h = heads[i]
                    S.dma('sp', lambda e: e.dma_start(out=omla_d[h, :, g * 512:(g + 1) * 512], in_=oT[i][:, :]), 'oT%d' % i, reads=[d_oT[i]])
            for s_ in range(len(tiles) + SK):
                if s_ < len(tiles):
                    stA(s_)
                if s_ >= SK:
                    stB(s_ - SK)
        S.barrier()

    def sb_phase(hsel):
        A = Arena(nc, PBASE, "s%d" % hsel)
        skT = A.alloc([128, 2, KT], BF16); d_skT = Dep()
        sv = A.alloc([128, NT + 1, 256], BF16); d_sv = Dep()
        win_b = A.alloc([128, 8, 768], BF16); d_win = Dep()
        xb = [A.alloc([128, 4, DM], BF16) for _ in range(2)]; d_xb = [Dep(), Dep()]
        XT = A.alloc([128, 8, 512], BF16); d_XT = Dep()
        sqT = A.alloc([128, 2, 512], BF16); d_sq = Dep()
        e32 = [A.alloc([128, 512], F32) for _ in range(3)]; d_e32 = [Dep() for _ in range(3)]
        spb = [A.alloc([128, 512], BF16) for _ in range(3)]; d_spb = [Dep() for _ in range(3)]
        wb = [A.alloc([128, 512], BF16) for _ in range(3)]; d_wb = [Dep() for _ in range(3)]
        L32 = A.alloc([128, 512], F32); d_L32 = Dep()
        Lb = [A.alloc([128, 512], BF16) for _ in range(3)]; d_Lb = [Dep() for _ in range(3)]
        oTs = [A.alloc([64, 512], F32) for _ in range(2)]; d_oTs = [Dep(), Dep()]
        c0 = 2 * hsel
        S.dma('pool', lambda e: e.dma_start(out=win_b[:, :, 0:256], in_=w_in_t[:, :, 576 + c0 * 128:576 + c0 * 128 + 256]), 'w1', writes=[d_win])
        S.dma('pool', lambda e: e.dma_start(out=win_b[:, :, 256:512], in_=w_in_t[:, :, 1088 + c0 * 128:1088 + c0 * 128 + 256]), 'w1', writes=[d_win])
        S.dma('pool', lambda e: e.dma_start(out=win_b[:, :, 512:768], in_=w_in_t[:, :, 1600 + c0 * 128:1600 + c0 * 128 + 256]), 'w1', writes=[d_win])
        load_x_bf(xb[0], d_xb[0], -1, 'xb0')
        pcount = [0]

        def nextp():
            pcount[0] += 1
            return pcount[0] % 4
        ocount = [0]
        for gi, g in enumerate([-1] + list(range(NG))):
            bi = gi % 2
            N = 16 if g < 0 else 512
            ntile = 1 if g < 0 else 4
            k0 = 0 if g < 0 else 16 + g * 512
            if gi + 1 <= NG:
                load_x_bf(xb[1 - bi], d_xb[1 - bi], g + 1, 'xb%d' % (1 - bi))
            transpose_x(xb[bi], d_xb[bi], XT, d_XT, g)
            for j in range(2):
                p = nextp()
                proj_fm(pF[p], d_pF[p], win_b, 256 + j * 128, XT, d_win, d_XT, N)
                S.op('dve', lambda e, j=j, p=p, k0=k0: e.tensor_copy(out=skT[:, j, k0:k0 + N], in_=pF[p][:, 0:N]), reads=[d_pF[p]], writes=[d_skT])
            for t in range(ntile):
                nt_ = 16 if g < 0 else 128
                vt = 0 if g < 0 else 1 + g * 4 + t
                p = nextp()
                for kc in range(8):
                    S.op('pe', lambda e, t=t, kc=kc, p=p, nt_=nt_: e.matmul(pF[p][0:nt_, 0:256], lhsT=XT[:, kc, t * 128:t * 128 + nt_], rhs=win_b[:, kc, 512:768],
                                                                          start=(kc == 0), stop=(kc == 7)),
                         reads=[d_XT, d_win], writes=[d_pF[p]])
                S.op('dve', lambda e, p=p, nt_=nt_, vt=vt: e.tensor_copy(out=sv[0:nt_, vt, :], in_=pF[p][0:nt_, 0:256]), reads=[d_pF[p]], writes=[d_sv])
            if g < 0:
                continue
            for j in range(2):
                p = nextp()
                proj_fm(pF[p], d_pF[p], win_b, j * 128, XT, d_win, d_XT, N)
                S.op('dve', lambda e, j=j, p=p: e.tensor_scalar(out=sqT[:, j, :], in0=pF[p][:, :], scalar1=0.125, scalar2=None, op0=ALU.mult),
                     reads=[d_pF[p]], writes=[d_sq])
            tiles = []
            for hh in range(4):
                blocks = [(4 * g + jq, jq) for jq in (3, 2, 1, 0)] + [(kb, 0) for kb in range(4 * g - 1, -1, -1)] + [(-1, 0)]
                for bi_, (kb, jq) in enumerate(blocks):
                    tiles.append((hh, kb, jq, bi_ == 0, bi_ == len(blocks) - 1))

            def geom(ti):
                hh, kb, jq, first, last = tiles[ti]
                nk_ = 16 if kb < 0 else 128
                kc0 = 0 if kb < 0 else 16 + kb * 128
                vt = 0 if kb < 0 else 1 + kb
                return hh, kb, jq * 128, first, last, nk_, kc0, vt, kb >= 4 * g

            def stA1(ti):
                hh, kb, q0, first, last, nk_, kc0, vt, diag = geom(ti)
                j = hh // 2
                hp = (hh % 2) * 64
                pS, d_pS = pF[ti % 4], d_pF[ti % 4]
                e_, d_e = e32[ti % 3], d_e32[ti % 3]
                if first:
                    pO, d_pO = pF[4 + hh % 2], d_pF[4 + hh % 2]
                    S.op('pe', lambda e: e.matmul(pO[0:64, :], lhsT=zeros_b[:, 0:64], rhs=zeros512[:, :], start=True, stop=False, skip_group_check=True),
                         reads=[d_const], writes=[d_pO])
                S.op('pe', lambda e: e.matmul(pS[0:nk_, q0:512], lhsT=skT[hp:hp + 64, j, kc0:kc0 + nk_], rhs=sqT[hp:hp + 64, j, q0:512],
                                              start=True, stop=False, skip_group_check=True), reads=[d_skT, d_sq], writes=[d_pS])
                S.op('act', lambda e: e.activation(out=e_[0:nk_, q0:512], in_=pS[0:nk_, q0:512], func=AF.Exp), reads=[d_pS], writes=[d_e])

            def stA2(ti):
                hh, kb, q0, first, last, nk_, kc0, vt, diag = geom(ti)
                e_, d_e = e32[ti % 3], d_e32[ti % 3]
                sp_, d_sp = spb[ti % 3], d_spb[ti % 3]
                S.op('act', lambda e: e.activation(out=sp_[0:nk_, q0:512], in_=e_[0:nk_, q0:512], func=AF.Ln, bias=1.0), reads=[d_e], writes=[d_sp])
                if diag:
                    S.op('dve', lambda e: e.tensor_tensor(out=sp_[:, q0:q0 + 128], in0=sp_[:, q0:q0 + 128], in1=mlt_b[:, :], op=ALU.mult),
                         reads=[d_sp, d_const], writes=[d_sp])
                if not last:
                    lprev = zeros512 if first else Lb[(ti - 1) % 3]
                    d_lprev = d_const if first else d_Lb[(ti - 1) % 3]
                    if q0 > 0:
                        S.op('dve', lambda e: e.tensor_copy(out=Lb[ti % 3][:, 0:q0], in_=lprev[:, 0:q0]), reads=[d_lprev], writes=[d_Lb[ti % 3]])
                    S.op('dve', lambda e: e.tensor_tensor(out=Lb[ti % 3][:, q0:512], in0=lprev[:, q0:512], in1=sp_[:, q0:512], op=ALU.add),
                         reads=[d_sp, d_lprev], writes=[d_Lb[ti % 3]])

            def stB(ti):
                hh, kb, q0, first, last, nk_, kc0, vt, diag = geom(ti)
                pS, d_pS = pF[ti % 4], d_pF[ti % 4]
                sp_, d_sp = spb[ti % 3], d_spb[ti % 3]
                w_, d_w = wb[ti % 3], d_wb[ti % 3]
                S.op('pe', lambda e: e.matmul(pS[0:nk_, q0:512], lhsT=negu_b[0:nk_, 0:nk_], rhs=sp_[0:nk_, q0:512], start=False, stop=first,
                                              skip_group_check=True), reads=[d_sp, d_const], writes=[d_pS])
                if not first:
                    lcur = (ti - 1) % 3
                    S.op('pe', lambda e: e.matmul(pS[0:nk_, q0:512], lhsT=negones_b[:, 0:nk_], rhs=Lb[lcur][:, q0:512], start=False, stop=True,
                                                  skip_group_check=True), reads=[d_Lb[lcur], d_const], writes=[d_pS])
                S.op('act', lambda e: e.activation(out=w_[0:nk_, q0:512], in_=pS[0:nk_, q0:512], func=AF.Exp), reads=[d_pS], writes=[d_w])
                if diag:
                    S.op('dve', lambda e: e.tensor_tensor(out=w_[:, q0:q0 + 128], in0=w_[:, q0:q0 + 128], in1=mlt_b[:, :], op=ALU.mult),
                         reads=[d_w, d_const], writes=[d_w])

            def stC(ti):
                hh, kb, q0, first, last, nk_, kc0, vt, diag = geom(ti)
                w_, d_w = wb[ti % 3], d_wb[ti % 3]
                pO, d_pO = pF[4 + hh % 2], d_pF[4 + hh % 2]
                S.op('pe', lambda e: e.matmul(pO[0:64, q0:512], lhsT=sv[0:nk_, vt, hh * 64:(hh + 1) * 64], rhs=w_[0:nk_, q0:512], start=False, stop=last,
                                              skip_group_check=True), reads=[d_sv, d_w], writes=[d_pO])
                if last:
                    oi = hh % 2
                    head = 4 * hsel + hh
                    S.op('dve', lambda e: e.tensor_copy(out=oTs[oi][:, :], in_=pO[0:64, :]), reads=[d_pO], writes=[d_oTs[oi]])
                    S.dma('sp', lambda e: e.dma_start(out=osb_d[head, :, g * 512:(g + 1) * 512], in_=oTs[oi][:, :]), 'oS%d' % oi, reads=[d_oTs[oi]])
            nt_all = len(tiles)
            for s_ in range(nt_all + 3):
                if s_ < nt_all:
                    stA1(s_)
                if 1 <= s_ <= nt_all:
                    stA2(s_ - 1)
                if 2 <= s_ <= nt_all + 1:
                    stB(s_ - 2)
                if s_ >= 3:
                    stC(s_ - 3)
        S.barrier()

    def merge_phase():
        A = Arena(nc, PBASE, "g")
        wo_f = A.alloc([128, 4, 512], F32); d_wof = Dep()
        wo_m = A.alloc([128, 4, DM], BF16)
        wo_s = A.alloc([64, 8, DM], BF16)
        d_wo = Dep()
        gm = A.alloc([128, 4], F32)
        gs = A.alloc([64, 8], F32)
        g1 = A.alloc([128, DM], F32)
        b1 = A.alloc([128, DM], F32)
        wr_f = A.alloc([128, 8, NE], F32)
        br_f = A.alloc([1, NE], F32)
        d_w = Dep()
        om = [A.alloc([128, 4, 512], F32) for _ in range(2)]; d_om = [Dep(), Dep()]
        os_ = [A.alloc([64, 8, 512], F32) for _ in range(2)]; d_os = [Dep(), Dep()]
        sqm = A.alloc([128, 4, 512], BF16); d_sqm = Dep()
        sqs = A.alloc([64, 8, 512], BF16); d_sqs = Dep()
        rrm = A.alloc([128, 512], F32); d_rrm = Dep()
        rrs = A.alloc([128, 512], F32); d_rrs = Dep()
        onm = A.alloc([128, 4, 512], BF16); d_onm = Dep()
        ons = A.alloc([64, 8, 512], BF16); d_ons = Dep()
        xt = [A.alloc([128, DM], F32) for _ in range(2)]; d_xt = [Dep(), Dep()]
        hp_ = A.alloc([128, DM], F32); d_hp = Dep()
        junk = A.alloc([128, DM], BF16); d_junk = Dep()
        h1 = [A.alloc([128, DM], F32) for _ in range(2)]; d_h1 = [Dep(), Dep()]
        h1b = [A.alloc([128, DM], BF16) for _ in range(2)]; d_h1b = [Dep(), Dep()]
        h1T = A.alloc([128, 8, 128], F32); d_h1T = Dep()
        st = A.alloc([128, 16], F32); d_st = Dep()
        lg = A.alloc([128, NE], F32); d_lg = Dep()
        top8 = A.alloc([128, 8], F32); d_top8 = Dep()
        idx8 = A.alloc([128, 8], U32); d_idx8 = Dep()
        idxf = A.alloc([128, 4], F32); d_idxf = Dep()
        e4 = A.alloc([128, 4], F32); d_e4 = Dep()
        maskb = A.alloc([128, NE], BF16); d_mask = Dep()
        rank = A.alloc([128, NE], F32); d_rank = Dep()
        base = A.alloc([128, NE], F32); d_base = Dep()
        oh = A.alloc([128, NE], F32); d_oh = Dep()
        oh2 = A.alloc([128, NE], F32)
        rsel = A.alloc([128, 4], F32); d_rsel = Dep()
        destf = A.alloc([128, 4], F32); d_destf = Dep()

        wo_t = w_o_d[:, :]
        S.dma('sp', lambda e: e.dma_start(out=gm[:], in_=gm_d[:, :]), 'w2', writes=[d_w])
        S.dma('sp', lambda e: e.dma_start(out=gs[:], in_=gs_d[:, :]), 'w2', writes=[d_w])
        S.dma('sp', lambda e: e.dma_start(out=g1[:], in_=ln1g_d[:, :]), 'w2', writes=[d_w])
        S.dma('sp', lambda e: e.dma_start(out=b1[:], in_=ln1b_d[:, :]), 'w2', writes=[d_w])
        S.dma('sp', lambda e: e.dma_start(out=wr_f[:], in_=wr_d[:, :].rearrange("(kc p) n -> p kc n", p=128)), 'w2', writes=[d_w])
        S.dma('sp', lambda e: e.dma_start(out=br_f[:], in_=br_d[:, :]), 'w2', writes=[d_w])
        for half in range(2):
            S.dma('sp', lambda e, half=half: e.dma_start(out=wo_f[:, :, :], in_=wo_t[0:512, half * 512:(half + 1) * 512].rearrange("(c p) n -> p c n", p=128)),
                  'w3', writes=[d_wof])
            for c in range(4):
                S.op('dve', lambda e, c=c, half=half: e.tensor_scalar(out=wo_m[:, c, half * 512:(half + 1) * 512], in0=wo_f[:, c, :], scalar1=gm[:, c:c + 1],
                                                                      scalar2=None, op0=ALU.mult), reads=[d_wof, d_w], writes=[d_wo])
            for q4 in range(2):
                S.dma('sp', lambda e, half=half, q4=q4: e.dma_start(
                    out=wo_f[0:64, :, :], in_=wo_t[512 + q4 * 256:512 + (q4 + 1) * 256, half * 512:(half + 1) * 512].rearrange("(c p) n -> p c n", p=64)),
                    'w3', writes=[d_wof])
                for c in range(4):
                    S.op('dve', lambda e, c=c, half=half, q4=q4: e.tensor_scalar(out=wo_s[:, q4 * 4 + c, half * 512:(half + 1) * 512], in0=wo_f[0:64, c, :],
                                                                              scalar1=gs[:, q4 * 4 + c:q4 * 4 + c + 1], scalar2=None, op0=ALU.mult),
                         reads=[d_wof, d_w], writes=[d_wo])
        S.op('pool', lambda e: e.memset(base[:], 0.0), writes=[d_base])

        def load_o(g, bi):
            S.dma('sp', lambda e: e.dma_start(out=om[bi][:, :, :], in_=omla_d[:, :, g * 512:(g + 1) * 512].rearrange("c p t -> p c t")), 'om%d' % bi, writes=[d_om[bi]])
            S.dma('sp', lambda e: e.dma_start(out=os_[bi][:, :, :], in_=osb_d[:, :, g * 512:(g + 1) * 512].rearrange("c p t -> p c t")), 'os%d' % bi, writes=[d_os[bi]])

        def load_xt(tt, bi):
            S.dma('sp', lambda e: e.dma_start(out=xt[bi][:, :], in_=x_d[tt * 128:(tt + 1) * 128, :]), 'xt%d' % bi, writes=[d_xt[bi]])
        load_o(0, 0)
        load_xt(0, 0)
        for g in range(NG):
            bi = g % 2
            if g + 1 < NG:
                load_o(g + 1, 1 - bi)
            for c2 in range(2):
                S.op('act', lambda e, c2=c2: e.activation(out=sqm[:, 2 * c2:2 * c2 + 2, :], in_=om[bi][:, 2 * c2:2 * c2 + 2, :], func=AF.Square), reads=[d_om[bi]], writes=[d_sqm])
            for c2 in range(4):
                S.op('act', lambda e, c2=c2: e.activation(out=sqs[:, 2 * c2:2 * c2 + 2, :], in_=os_[bi][:, 2 * c2:2 * c2 + 2, :], func=AF.Square), reads=[d_os[bi]], writes=[d_sqs])
            for c in range(4):
                S.op('pe', lambda e, c=c: e.matmul(pF[2][:, :], lhsT=ones_b[:, :], rhs=sqm[:, c, :], start=(c == 0), stop=(c == 3)), reads=[d_sqm, d_const], writes=[d_pF[2]])
            rsqrt_mean(rrm, d_rrm, pF[2], d_pF[2], 512.0, RMS_EPS)
            for c in range(8):
                S.op('pe', lambda e, c=c: e.matmul(pF[3][:, :], lhsT=ones_b[0:64, :], rhs=sqs[0:64, c, :], start=(c == 0), stop=(c == 7)), reads=[d_sqs, d_const], writes=[d_pF[3]])
            rsqrt_mean(rrs, d_rrs, pF[3], d_pF[3], 512.0, RMS_EPS)
            for c in range(4):
                S.op('dve', lambda e, c=c: e.tensor_tensor(out=onm[:, c, :], in0=om[bi][:, c, :], in1=rrm[:, :], op=ALU.mult), reads=[d_om[bi], d_rrm], writes=[d_onm])
            for c in range(8):
                S.op('dve', lambda e, c=c: e.tensor_tensor(out=ons[:, c, :], in0=os_[bi][:, c, :], in1=rrs[0:64, :], op=ALU.mult), reads=[d_os[bi], d_rrs], writes=[d_ons])
            for t in range(4):
                tt = g * 4 + t
                tb = tt % 2
                if tt + 1 < NT:
                    load_xt(tt + 1, 1 - tb)
                S.op('pool', lambda e: e.memset(st[:, :], 0.0), writes=[d_st])
                for half in range(2):
                    pa, d_pa = pF[half], d_pF[half]
                    for c in range(4):
                        S.op('pe', lambda e, c=c, t=t, half=half, pa=pa: e.matmul(pa[:, :], lhsT=onm[:, c, t * 128:(t + 1) * 128], rhs=wo_m[:, c, half * 512:(half + 1) * 512],
                                                                                 start=(c == 0), stop=False), reads=[d_onm, d_wo], writes=[d_pa])
                    for c in range(8):
                        S.op('pe', lambda e, c=c, t=t, half=half, pa=pa: e.matmul(pa[:, :], lhsT=ons[0:64, c, t * 128:(t + 1) * 128], rhs=wo_s[0:64, c, half * 512:(half + 1) * 512],
                                                                                 start=False, stop=(c == 7)), reads=[d_ons, d_wo], writes=[d_pa])
                    S.op('dve', lambda e, half=half, pa=pa, tb=tb: e.scalar_tensor_tensor(out=hp_[:, half * 512:(half + 1) * 512], in0=xt[tb][:, half * 512:(half + 1) * 512],
                                                                                        scalar=ALPHA, in1=pa[:, :], op0=ALU.mult, op1=ALU.add, accum_out=st[:, half:half + 1]),
                         reads=[d_xt[tb], d_pa, d_st], writes=[d_hp, d_st])
                layer_norm(hp_, d_hp, st, d_st, junk, d_junk, g1, b1, d_w, h1[tb], d_h1[tb])
                S.dma('sp', lambda e, tt=tt, tb=tb: e.dma_start(out=h1_d[tt * 128:(tt + 1) * 128, :], in_=h1[tb][:, :]), 'h1o%d' % tb, reads=[d_h1[tb]])
                S.op('act', lambda e, tb=tb: e.activation(out=h1b[tb][:, :], in_=h1[tb][:, :], func=AF.Copy), reads=[d_h1[tb]], writes=[d_h1b[tb]])
                for hf in range(2):
                    pX, d_pX = pF[4 + hf], d_pF[4 + hf]
                    for c in range(4):
                        cc = hf * 4 + c
                        S.op('pe', lambda e, c=c, cc=cc, pX=pX, tb=tb: e.transpose(out=pX[:, c * 128:(c + 1) * 128], in_=h1[tb][:, cc * 128:(cc + 1) * 128], identity=ident_f[:, :]),
                             reads=[d_h1[tb], d_const], writes=[d_pX])
                    S.op('act', lambda e, hf=hf, pX=pX: e.activation(out=h1T[:, hf * 4:(hf + 1) * 4, :], in_=pX[:, :].rearrange("p (c n) -> p c n", c=4), func=AF.Copy),
                         reads=[d_pX], writes=[d_h1T])
                pL, d_pL = pF[2], d_pF[2]
                for c in range(8):
                    S.op('pe', lambda e, c=c: e.matmul(pL[:, 0:NE], lhsT=h1T[:, c, :], rhs=wr_f[:, c, :], start=(c == 0), stop=False), reads=[d_h1T, d_w], writes=[d_pL])
                S.op('pe', lambda e: e.matmul(pL[:, 0:NE], lhsT=ones_f[0:1, :], rhs=br_f[0:1, :], start=False, stop=True), reads=[d_const, d_w], writes=[d_pL])
                S.op('dve', lambda e: e.tensor_copy(out=lg[:, :], in_=pL[:, 0:NE]), reads=[d_pL], writes=[d_lg])
                S.op('dve', lambda e: e.max(out=top8[:, :], in_=lg[:, :]), reads=[d_lg], writes=[d_top8])
                S.op('dve', lambda e: e.max_index(out=idx8[:, :], in_max=top8[:, :], in_values=lg[:, :]), reads=[d_lg, d_top8], writes=[d_idx8])
                S.op('dve', lambda e: e.tensor_copy(out=idxf[:, :], in_=idx8[:, 0:4]), reads=[d_idx8], writes=[d_idxf])
                S.op('dve', lambda e: e.tensor_scalar(out=st[:, 8:9], in0=top8[:, 0:1], scalar1=-1.0, scalar2=None, op0=ALU.mult), reads=[d_top8, d_st], writes=[d_st])
                S.op('act', lambda e: e.activation(out=e4[:, :], in_=top8[:, 0:4], func=AF.Exp, bias=st[:, 8:9], scale=1.0, accum_out=st[:, 9:10]),
                     reads=[d_top8, d_st], writes=[d_e4, d_st])
                S.op('dve', lambda e: e.reciprocal(out=st[:, 10:11], in_=st[:, 9:10]), reads=[d_st], writes=[d_st])
                S.op('dve', lambda e, tt=tt: e.tensor_scalar(out=gtab[:, tt, :], in0=e4[:, :], scalar1=st[:, 10:11], scalar2=None, op0=ALU.mult),
                     reads=[d_e4, d_st], writes=[d_gtab])
                S.op('dve', lambda e: e.tensor_scalar(out=maskb[:, :], in0=lg[:, :], scalar1=top8[:, 3:4], scalar2=None, op0=ALU.is_ge), reads=[d_lg, d_top8], writes=[d_mask])
                pK, d_pK = pF[3], d_pF[3]
                S.op('pe', lambda e: e.matmul(pK[:, 0:NE], lhsT=lst_b[:, :], rhs=maskb[:, :], start=True, stop=True), reads=[d_mask, d_const], writes=[d_pK])
                S.op('pe', lambda e: e.matmul(pK[:, NE:2 * NE], lhsT=ones_b[:, :], rhs=maskb[:, :], start=True, stop=True), reads=[d_mask, d_const], writes=[d_pK])
                S.op('dve', lambda e: e.tensor_tensor(out=rank[:, :], in0=pK[:, 0:NE], in1=base[:, :], op=ALU.add), reads=[d_pK, d_base], writes=[d_rank])
                S.op('dve', lambda e: e.tensor_tensor(out=base[:, :], in0=pK[:, NE:2 * NE], in1=base[:, :], op=ALU.add), reads=[d_pK, d_base], writes=[d_base])
                S.op('pool', lambda e: e.memset(rsel[:, :], 0.0), writes=[d_rsel])
                for k in range(4):
                    S.op('dve', lambda e, k=k: e.tensor_scalar(out=oh[:, :], in0=iota_f[:, :], scalar1=idxf[:, k:k + 1], scalar2=None, op0=ALU.is_equal),
                         reads=[d_const, d_idxf], writes=[d_oh])
                    S.op('dve', lambda e, k=k: e.scalar_tensor_tensor(out=oh2[:, :], in0=oh[:, :], scalar=1.0, in1=rank[:, :], op0=ALU.mult, op1=ALU.mult,
                                                                      accum_out=rsel[:, k:k + 1]), reads=[d_oh, d_rank, d_rsel], writes=[d_rsel, d_oh])
                S.op('dve', lambda e: e.tensor_scalar(out=rsel[:, :], in0=rsel[:, :], scalar1=float(CAP - 1), scalar2=None, op0=ALU.min), reads=[d_rsel], writes=[d_rsel])
                S.op('dve', lambda e: e.scalar_tensor_tensor(out=destf[:, :], in0=idxf[:, :], scalar=float(CAP), in1=rsel[:, :], op0=ALU.mult, op1=ALU.add),
                     reads=[d_idxf, d_rsel], writes=[d_destf])
                S.op('dve', lambda e, tt=tt: e.tensor_copy(out=dtab[:, tt, :], in_=destf[:, :]), reads=[d_destf], writes=[d_dtab])
                if debug:
                    S.dma('sp', lambda e, tt=tt: e.dma_start(out=dbg_d[tt * 128:(tt + 1) * 128, 0:4], in_=destf[:, :]), 'dbg%d' % tb, reads=[d_destf])
                    S.dma('sp', lambda e, tt=tt: e.dma_start(out=dbg_d[tt * 128:(tt + 1) * 128, 4:8], in_=gtab[:, tt, :]), 'dbg%d' % tb, reads=[d_gtab])
                S.wait_dma('pool', 'sc%d' % (1 - tb))
                for k in range(4):
                    S.dma('pool', lambda e, tt=tt, k=k, tb=tb: e.indirect_dma_start(
                        out=xbuf_d[:, :], out_offset=bass.IndirectOffsetOnAxis(ap=dtab[:, tt, k:k + 1], axis=0), in_=h1b[tb][:, :], in_offset=None), 'sc%d' % tb, reads=[d_h1b[tb], d_dtab, d_xbufz])
        S.barrier()

    def layer_norm(hp_, d_hp, st, d_st, junk, d_junk, gt, bt, d_gb, out, d_out):
        S.op('dve', lambda e: e.tensor_tensor(out=st[:, 2:3], in0=st[:, 0:1], in1=st[:, 1:2], op=ALU.add), reads=[d_st], writes=[d_st])
        S.op('dve', lambda e: e.tensor_scalar(out=st[:, 2:3], in0=st[:, 2:3], scalar1=-1.0 / DM, scalar2=None, op0=ALU.mult), reads=[d_st], writes=[d_st])
        S.op('act', lambda e: e.activation(out=junk[:, :], in_=hp_[:, :], func=AF.Square, bias=st[:, 2:3], scale=1.0, accum_out=st[:, 3:4]),
             reads=[d_hp, d_st], writes=[d_junk, d_st])
        S.op('act', lambda e: e.activation(out=st[:, 4:5], in_=st[:, 3:4], func=AF.Sqrt, bias=float(LN_EPS), scale=1.0 / DM), reads=[d_st], writes=[d_st])
        S.op('dve', lambda e: e.reciprocal(out=st[:, 4:5], in_=st[:, 4:5]), reads=[d_st], writes=[d_st])
        S.op('dve', lambda e: e.tensor_tensor(out=st[:, 5:6], in0=st[:, 2:3], in1=st[:, 4:5], op=ALU.mult), reads=[d_st], writes=[d_st])
        S.op('act', lambda e: e.activation(out=out[:, :], in_=hp_[:, :], func=AF.Identity, bias=st[:, 5:6], scale=st[:, 4:5]),
             reads=[d_hp, d_st], writes=[d_out])
        S.op('dve', lambda e: e.tensor_tensor(out=out[:, :], in0=out[:, :], in1=gt[:, :], op=ALU.mult), reads=[d_out, d_gb], writes=[d_out])
        S.op('dve', lambda e: e.tensor_tensor(out=out[:, :], in0=out[:, :], in1=bt[:, :], op=ALU.add), reads=[d_out, d_gb], writes=[d_out])

    def moe_phase():
        A = Arena(nc, PBASE, "e")
        wg_b = [A.alloc([128, 8, DM], BF16) for _ in range(2)]
        wu_b = [A.alloc([128, 8, DM], BF16) for _ in range(2)]
        wd_b = [A.alloc([128, 8, DM], BF16) for _ in range(2)]
        d_wg = [Dep(), Dep()]; d_wu = [Dep(), Dep()]; d_wd = [Dep(), Dep()]
        bgu = A.alloc([128, NE, 16], F32); d_bgu = Dep()
        bd_f = [A.alloc([1, DM], F32) for _ in range(2)]; d_bdf = [Dep(), Dep()]
        bdb = [A.alloc([128, DM], F32) for _ in range(2)]; d_bdb = [Dep(), Dep()]
        xs = [A.alloc([128, 4, DM], BF16) for _ in range(2)]; d_xs = [Dep(), Dep()]
        XsT = A.alloc([128, 8, 512], BF16); d_XsT = Dep()
        g32 = [A.alloc([128, 512], F32) for _ in range(2)]; d_g32 = [Dep(), Dep()]
        s32 = [A.alloc([128, 512], F32) for _ in range(2)]; d_s32 = [Dep(), Dep()]
        u32 = [A.alloc([128, 512], F32) for _ in range(2)]; d_u32 = [Dep(), Dep()]
        aT = [A.alloc([128, 8, 512], BF16) for _ in range(2)]; d_aT = [Dep(), Dep()]
        ys = [A.alloc([128, DM], F32) for _ in range(2)]; d_ys = [Dep(), Dep()]
        S.dma('sp', lambda e: e.dma_start(out=bgu[:], in_=bgu_d[:, :, :]), 'w2', writes=[d_bgu])

        def load_w(ex, bi):
            for (wt, src, dd, key) in ((wg_b, wg_d, d_wg, 'we'), (wu_b, wu_d, d_wg, 'we'), (wd_b, wd_d, d_wg, 'we')):
                for hf in range(2):
                    S.dma('pool', lambda e, wt=wt, src=src, hf=hf: e.dma_start(out=wt[bi][:, hf * 4:(hf + 1) * 4, :],
                                                                                in_=src[ex, hf * 512:(hf + 1) * 512, :].rearrange("(kc p) f -> p kc f", p=128)),
                          '%s%d' % (key, bi), writes=[dd[bi]])
            S.dma('sp', lambda e: e.dma_start(out=bd_f[bi][:, :], in_=bd_d[ex:ex + 1, :]), 'bd%d' % bi, writes=[d_bdf[bi]])
        groups = []
        off = 0
        while off < CAP:
            n = min(512, CAP - off)
            groups.append((off, n))
            off += n
        load_w(0, 0)
        gcount = 0
        ycount = 0
        for ex in range(NE):
            wi = ex % 2
            if ex + 1 < NE:
                load_w(ex + 1, 1 - wi)
            for half in range(2):
                S.op('pe', lambda e, half=half: e.matmul(pF[5][:, :], lhsT=ones_f[0:1, :], rhs=bd_f[wi][0:1, half * 512:(half + 1) * 512], start=True, stop=True),
                     reads=[d_const, d_bdf[wi]], writes=[d_pF[5]])
                S.op('act', lambda e, half=half: e.activation(out=bdb[wi][:, half * 512:(half + 1) * 512], in_=pF[5][:, :], func=AF.Copy), reads=[d_pF[5]], writes=[d_bdb[wi]])
            for (soff, N) in groups:
                xi = gcount % 2
                gcount += 1
                ntile = N // 128
                row0 = ex * CAP + soff
                S.dma('sp', lambda e, xi=xi, row0=row0, ntile=ntile, N=N: e.dma_start(out=xs[xi][:, 0:ntile, :],
                                                                                     in_=xbuf_d[row0:row0 + N, :].rearrange("(t p) d -> p t d", p=128)),
                      'xs%d' % xi, writes=[d_xs[xi]])
                for t in range(ntile):
                    pi = t % 2
                    for c in range(8):
                        S.op('pe', lambda e, t=t, c=c, pi=pi, xi=xi: e.transpose(out=pT[pi][:, c * 128:(c + 1) * 128], in_=xs[xi][:, t, c * 128:(c + 1) * 128], identity=ident_b[:, :]),
                             reads=[d_xs[xi], d_const], writes=[d_pT[pi]])
                    S.op('dve', lambda e, t=t, pi=pi: e.tensor_copy(out=XsT[:, :, t * 128:(t + 1) * 128], in_=pT[pi][:, :].rearrange("p (c n) -> p c n", c=8)),
                         reads=[d_pT[pi]], writes=[d_XsT])
                ai = gcount % 2
                for fc in range(8):
                    fi = fc % 2
                    for kc in range(8):
                        S.op('pe', lambda e, fc=fc, kc=kc, N=N: e.matmul(pF[0][:, 0:N], lhsT=wg_b[wi][:, kc, fc * 128:(fc + 1) * 128], rhs=XsT[:, kc, 0:N], start=(kc == 0), stop=(kc == 7)),
                             reads=[d_wg[wi], d_XsT], writes=[d_pF[0]])
                    for kc in range(8):
                        S.op('pe', lambda e, fc=fc, kc=kc, N=N: e.matmul(pF[1][:, 0:N], lhsT=wu_b[wi][:, kc, fc * 128:(fc + 1) * 128], rhs=XsT[:, kc, 0:N], start=(kc == 0), stop=(kc == 7)),
                             reads=[d_wg[wi], d_XsT], writes=[d_pF[1]])
                    S.op('dve', lambda e, fc=fc, fi=fi, N=N, ex=ex: e.tensor_scalar(out=g32[fi][:, 0:N], in0=pF[0][:, 0:N], scalar1=bgu[:, ex, fc:fc + 1], scalar2=7.0, op0=ALU.add, op1=ALU.min),
                         reads=[d_pF[0], d_bgu], writes=[d_g32[fi]])
                    S.op('act', lambda e, fi=fi, N=N: e.activation(out=s32[fi][:, 0:N], in_=g32[fi][:, 0:N], func=AF.Silu, scale=1.702), reads=[d_g32[fi]], writes=[d_s32[fi]])
                    S.op('dve', lambda e, fc=fc, fi=fi, N=N, ex=ex: e.tensor_scalar(out=u32[fi][:, 0:N], in0=pF[1][:, 0:N], scalar1=bgu[:, ex, 8 + fc:9 + fc], scalar2=7.0, op0=ALU.add, op1=ALU.min),
                         reads=[d_pF[1], d_bgu], writes=[d_u32[fi]])
                    S.op('dve', lambda e, fi=fi, N=N: e.tensor_scalar(out=u32[fi][:, 0:N], in0=u32[fi][:, 0:N], scalar1=-7.0, scalar2=1.0, op0=ALU.max, op1=ALU.add),
                         reads=[d_u32[fi]], writes=[d_u32[fi]])
                    S.op('dve', lambda e, fc=fc, fi=fi, N=N, ai=ai: e.scalar_tensor_tensor(out=aT[ai][:, fc, 0:N], in0=s32[fi][:, 0:N], scalar=1.0 / 1.702, in1=u32[fi][:, 0:N],
                                                                                          op0=ALU.mult, op1=ALU.mult),
                         reads=[d_s32[fi], d_u32[fi]], writes=[d_aT[ai]])
                for t in range(ntile):
                    yi = ycount % 2
                    ycount += 1
                    for half in range(2):
                        pY, d_pY = pF[2 + half], d_pF[2 + half]
                        for fc in range(8):
                            S.op('pe', lambda e, t=t, fc=fc, half=half, pY=pY, ai=ai: e.matmul(pY[:, :], lhsT=aT[ai][:, fc, t * 128:(t + 1) * 128], rhs=wd_b[wi][:, fc, half * 512:(half + 1) * 512],
                                                                                              start=(fc == 0), stop=(fc == 7)), reads=[d_aT[ai], d_wg[wi]], writes=[d_pY])
                        S.op('dve', lambda e, half=half, pY=pY, yi=yi: e.tensor_tensor(out=ys[yi][:, half * 512:(half + 1) * 512], in0=pY[:, :], in1=bdb[wi][:, half * 512:(half + 1) * 512], op=ALU.add),
                             reads=[d_pY, d_bdb[wi]], writes=[d_ys[yi]])
                    r0 = row0 + t * 128
                    S.dma('sp', lambda e, yi=yi, r0=r0: e.dma_start(out=ybuf_d[r0:r0 + 128, :], in_=ys[yi][:, :]), 'yo%d' % yi, reads=[d_ys[yi]])
        S.barrier()

    def comb_phase():
        A = Arena(nc, PBASE, "f")
        g2 = A.alloc([128, DM], F32)
        b2 = A.alloc([128, DM], F32)
        d_gb = Dep()
        yk = [[A.alloc([128, DM], F32) for _ in range(4)] for _ in range(2)]
        d_yk = [[Dep() for _ in range(4)] for _ in range(2)]
        hh = [A.alloc([128, DM], F32) for _ in range(2)]; d_hh = [Dep(), Dep()]
        acc = A.alloc([128, DM], F32); d_acc = Dep()
        junk = A.alloc([128, DM], BF16); d_junk = Dep()
        st = A.alloc([128, 16], F32); d_st = Dep()
        ot = [A.alloc([128, DM], F32) for _ in range(2)]; d_ot = [Dep(), Dep()]
        S.dma('sp', lambda e: e.dma_start(out=g2[:], in_=ln2g_d[:, :]), 'w2', writes=[d_gb])
        S.dma('sp', lambda e: e.dma_start(out=b2[:], in_=ln2b_d[:, :]), 'w2', writes=[d_gb])

        def load(tt, bi):
            S.dma('sp', lambda e: e.dma_start(out=hh[bi][:, :], in_=h1_d[tt * 128:(tt + 1) * 128, :]), 'hh%d' % bi, writes=[d_hh[bi]])
            S.wait_dma('pool', 'yk%d' % (1 - bi))
            for k in range(4):
                S.dma('pool', lambda e, k=k: e.indirect_dma_start(out=yk[bi][k][:, :], out_offset=None, in_=ybuf_d[:, :],
                                                                  in_offset=bass.IndirectOffsetOnAxis(ap=dtab[:, tt, k:k + 1], axis=0)),
                      'yk%d' % bi, reads=[d_dtab], writes=[d_yk[bi][0]])
        load(0, 0)
        for tt in range(NT):
            bi = tt % 2
            if tt + 1 < NT:
                load(tt + 1, 1 - bi)
            S.op('pool', lambda e: e.memset(st[:, :], 0.0), writes=[d_st])
            S.op('act', lambda e: e.activation(out=acc[:, :], in_=hh[bi][:, :], func=AF.Copy, scale=ALPHA), reads=[d_hh[bi]], writes=[d_acc])
            for k in range(3):
                eng = 'dve'
                S.op(eng, lambda e, k=k: e.scalar_tensor_tensor(out=acc[:, :], in0=yk[bi][k][:, :], scalar=gtab[:, tt, k:k + 1], in1=acc[:, :], op0=ALU.mult, op1=ALU.add),
                     reads=[d_yk[bi][0], d_gtab, d_acc], writes=[d_acc])
            S.op('dve', lambda e: e.scalar_tensor_tensor(out=acc[:, :], in0=yk[bi][3][:, :], scalar=gtab[:, tt, 3:4], in1=acc[:, :], op0=ALU.mult, op1=ALU.add,
                                                          accum_out=st[:, 0:1]), reads=[d_yk[bi][0], d_gtab, d_acc, d_st], writes=[d_acc, d_st])
            layer_norm(acc, d_acc, st, d_st, junk, d_junk, g2, b2, d_gb, ot[bi], d_ot[bi])
            S.dma('sp', lambda e, tt=tt: e.dma_start(out=out_d[tt * 128:(tt + 1) * 128, :], in_=ot[bi][:, :]), 'out%d' % bi, reads=[d_ot[bi]])

    for ph in phases:
        if ph == 'mla0':
            mla_phase(0)
        elif ph == 'mla1':
            mla_phase(1)
        elif ph == 'sb0':
            sb_phase(0)
        elif ph == 'sb1':
            sb_phase(1)
        elif ph == 'merge':
            merge_phase()
        elif ph == 'moe':
            moe_phase()
        elif ph == 'comb':
            comb_phase()
    S.barrier()

    from contextlib import ExitStack
    with ExitStack() as es:
        sems = {}
        for k in S.semkeys:
            nm = "s_" + "_".join(str(a) for a in k)
            sems[k] = es.enter_context(nc.semaphore(nm))
        with nc.Block() as block:
            S.emit(block, sems)
    return nc, S


def rope_tables(npos):
    half = 32
    freqs = (np.float32(10000.0) ** (-(np.arange(half, dtype=np.float32) * np.float32(2.0)) / np.float32(64))).astype(np.float32)
    ang = (np.arange(npos, dtype=np.float32)[:, None] * freqs[None, :]).astype(np.float32)
    c = np.cos(ang.astype(np.float64)).astype(np.float32).T
    s = np.sin(ang.astype(np.float64)).astype(np.float32).T
    return np.ascontiguousarray(np.tile(c, (4, 1))), np.ascontiguousarray(np.tile(s, (4, 1)))


def make_consts(T):
    k = np.arange(128)
    cos2, sin2 = rope_tables(T + 16)
    return {
        "ident": np.eye(128, dtype=np.float32),
        "mle": (k[:, None] <= k[None, :]).astype(np.float32),
        "mlt": (k[:, None] < k[None, :]).astype(np.float32),
        "negu": -(k[:, None] >= k[None, :]).astype(np.float32),
        "lst": (k[:, None] < k[None, :]).astype(np.float32),
        "iota": np.tile(np.arange(NE, dtype=np.float32)[None, :], (128, 1)),
        "cos2": cos2, "sin2": sin2,
    }


def make_shared(inp):
    f = lambda a: np.ascontiguousarray(a, dtype=np.float32)
    d = {}
    d["meta"] = f(inp["meta_tokens"])
    d["w_in"] = f(inp["w_in"][0])
    d["qg"] = f(inp["q_norm_g"][0].reshape(2, 128).T)
    d["w_uq"] = f(inp["w_uq"][0])
    d["kvg"] = f(inp["kv_norm_g"][0].reshape(2, 128).T)
    d["w_ukv"] = f(inp["w_ukv"][0])
    d["gm"] = f(inp["mla_out_g"][0].reshape(4, 128).T)
    d["gs"] = f(inp["sb_out_g"][0].reshape(8, 64).T)
    d["w_o"] = f(inp["w_o"][0])
    d["ln1g"] = f(np.tile(inp["ln1_g"][0][None, :], (128, 1)))
    d["ln1b"] = f(np.tile(inp["ln1_b"][0][None, :], (128, 1)))
    d["wr"] = f(inp["w_router"][0])
    d["br"] = f(inp["b_router"][0][None, :])
    d["wg"] = f(inp["w_gate"][0])
    d["wu"] = f(inp["w_up"][0])
    d["wd"] = f(inp["w_down"][0])
    bg = np.asarray(inp["b_gate"][0]).reshape(NE, 8, 128).transpose(2, 0, 1)
    bu = np.asarray(inp["b_up"][0]).reshape(NE, 8, 128).transpose(2, 0, 1)
    d["bgu"] = f(np.concatenate([bg, bu], axis=2))
    d["bd"] = f(inp["b_down"][0])
    d["ln2g"] = f(np.tile(inp["ln2_g"][0][None, :], (128, 1)))
    d["ln2b"] = f(np.tile(inp["ln2_b"][0][None, :], (128, 1)))
    return d


CAP_FULL = 1280


def kernel(**inputs):
    x = np.asarray(inputs["x"])
    B, L, _ = x.shape
    NG = L // 512
    nc, _ = build(NG, CAP_FULL)
    shared = make_shared(inputs)
    shared.update(make_consts(L))
    in_maps = []
    for b in range(B):
        m = dict(shared)
        m["x"] = np.ascontiguousarray(x[b], dtype=np.float32)
        in_maps.append(m)
    res = run_bass_kernel_spmd(nc, in_maps, core_ids=list(range(B)))
    return np.stack([np.asarray(r["out"]) for r in res.results], axis=0).astype(np.float32)
```

```python
import numpy as np
import concourse.bass as bass
import concourse.mybir as mybir
from concourse.bass_utils import run_bass_kernel_spmd

F32 = mybir.dt.float32
BF16 = mybir.dt.bfloat16
I32 = mybir.dt.int32
U32 = mybir.dt.uint32
ALU = mybir.AluOpType
AF = mybir.ActivationFunctionType

ENGS = ['pe', 'act', 'dve', 'pool', 'sp']
EPOCH = 30000
SETUP_KEYS = ('const', 'w1', 'w2')
DM = 1024
NE = 32
SCALE_MLA = float(192 ** -0.5)
ALPHA = float(2.0 ** 0.25)
LN_EPS = 1e-5
RMS_EPS = 1e-6
SB_BASE = 24576
SB_END = 229344


class Dep:
    __slots__ = ('w', 'r', 'x')

    def __init__(self, excl=False):
        self.w = None
        self.r = {}
        self.x = excl


class _Rec:
    def __getattr__(self, name):
        def f(*a, **k):
            return (name, a, k)
        return f


_REC = _Rec()


class Sched:
    def __init__(self):
        self.prog = {e: [] for e in ENGS}
        self.cnt = {e: 0 for e in ENGS}
        self.epoch = {e: 0 for e in ENGS}
        self.waited = {e: {} for e in ENGS}
        self.dma_cnt = {}
        self.semkeys = []
        for e in ENGS:
            if e != 'sp':
                self.semkeys.append((e, 0))

    def _need(self, eng, reads, writes):
        need = {}

        def add(d):
            if d is None:
                return
            k, v = d
            if need.get(k, 0) < v:
                need[k] = v
        for b in reads:
            add(b.w)
            if b.x:
                for k_, d in b.r.items():
                    if k_[0] != eng:
                        add(d)
        for b in writes:
            add(b.w)
            for d in b.r.values():
                add(d)
        wl = self.waited[eng]
        for k, v in need.items():
            if eng == 'pe' and k[0] == 'pe':
                continue
            if k[0] == 'dma' and k[1] in SETUP_KEYS:
                v = self.dma_cnt[k]
            if wl.get(k, 0) >= v:
                continue
            wl[k] = v
            self.prog[eng].append(('wait', k, v))

    def op(self, eng, fn, reads=(), writes=()):
        self._need(eng, reads, writes)
        if self.cnt[eng] >= EPOCH:
            self.epoch[eng] += 1
            self.cnt[eng] = 0
            self.semkeys.append((eng, self.epoch[eng]))
        self.cnt[eng] += 1
        key = (eng, self.epoch[eng])
        tok = (key, self.cnt[eng])
        self.prog[eng].append(('op', fn(_REC), key))
        for b in reads:
            b.r[key] = tok
        for b in writes:
            b.w = tok
            b.r = {}

    def dma(self, q, fn, key, reads=(), writes=(), n=1):
        self._need(q, reads, writes)
        k = ('dma', key, 'sw' if q == 'pool' else 'hw')
        if k not in self.dma_cnt:
            self.dma_cnt[k] = 0
            self.semkeys.append(k)
        self.dma_cnt[k] += 16 * n
        tok = (k, self.dma_cnt[k])
        self.prog[q].append(('dma', fn(_REC), k))
        for b in reads:
            b.r[k] = tok
        for b in writes:
            b.w = tok
            b.r = {}

    def wait_dma(self, eng, key, q='pool'):
        k = ('dma', key, 'sw' if q == 'pool' else 'hw')
        v = self.dma_cnt.get(k, 0)
        if v and self.waited[eng].get(k, 0) < v:
            self.waited[eng][k] = v
            self.prog[eng].append(('wait', k, v))

    def barrier(self):
        cur = {}
        for e in ENGS:
            if e != 'sp' and self.cnt[e] > 0:
                cur[(e, self.epoch[e])] = self.cnt[e]
        for k, v in self.dma_cnt.items():
            cur[k] = v
        for e in ENGS:
            wl = self.waited[e]
            for k, v in cur.items():
                if e == 'pe' and k[0] == 'pe':
                    continue
                if wl.get(k, 0) >= v:
                    continue
                wl[k] = v
                self.prog[e].append(('wait', k, v))

    def emit(self, block, sems):
        handles = {'pe': 'tensor', 'act': 'scalar', 'dve': 'vector', 'pool': 'gpsimd', 'sp': 'sync'}

        def run(eng_name):
            def body(e):
                for item in self.prog[eng_name]:
                    if item[0] == 'wait':
                        e.wait_ge(sems[item[1]], item[2])
                    elif item[0] == 'op':
                        nm, a, k = item[1]
                        getattr(e, nm)(*a, **k).then_inc(sems[item[2]], 1)
                    else:
                        nm, a, k = item[1]
                        try:
                            getattr(e, nm)(*a, **k).then_inc(sems[item[2]], 16)
                        except Exception:
                            print("DMA FAIL", eng_name, nm, item[2], k)
                            raise
            return body
        for en in ENGS:
            getattr(block, handles[en])(run(en))


class Arena:
    def __init__(self, nc, base, tag):
        self.nc = nc
        self.off = base
        self.tag = tag
        self.n = 0

    def alloc(self, shape, dt):
        sz = int(np.prod(shape[1:])) * (2 if dt == BF16 else 4)
        off = (self.off + 31) // 32 * 32
        self.n += 1
        t = self.nc.alloc_sbuf_tensor_at("%s_%d" % (self.tag, self.n), list(shape), dt, offset=off)
        self.off = off + sz
        assert self.off <= SB_END, (self.tag, self.off)
        return t


def build(NG, CAP, debug=False, phases=('mla0', 'mla1', 'sb0', 'sb1', 'merge', 'moe', 'comb')):
    T = NG * 512
    NT = NG * 4
    KT = T + 16
    NSLOT = NE * CAP
    nc = bass.Bass("TRN2", target_bir_lowering=False)
    S = Sched()

    def din(name, shape, dt=F32):
        return nc.dram_tensor(name, list(shape), dt, kind="ExternalInput")

    def dscratch(name, shape, dt=F32, dbg_out=True):
        return nc.dram_tensor(name, list(shape), dt, kind="ExternalOutput" if (debug and dbg_out) else "Internal")

    x_d = din("x", [T, DM])
    meta_d = din("meta", [16, DM])
    w_in_d = din("w_in", [DM, 2112])
    qg_d = din("qg", [128, 2])
    w_uq_d = din("w_uq", [256, 768])
    kvg_d = din("kvg", [128, 2])
    w_ukv_d = din("w_ukv", [256, 1024])
    gm_d = din("gm", [128, 4])
    gs_d = din("gs", [64, 8])
    w_o_d = din("w_o", [DM, DM])
    ln1g_d = din("ln1g", [128, DM])
    ln1b_d = din("ln1b", [128, DM])
    wr_d = din("wr", [DM, NE])
    br_d = din("br", [1, NE])
    if 'moe' in phases:
        wg_d = din("wg", [NE, DM, DM])
        wu_d = din("wu", [NE, DM, DM])
        wd_d = din("wd", [NE, DM, DM])
    bgu_d = din("bgu", [128, NE, 16])
    bd_d = din("bd", [NE, DM])
    ln2g_d = din("ln2g", [128, DM])
    ln2b_d = din("ln2b", [128, DM])
    ident_d = din("ident", [128, 128])
    mle_d = din("mle", [128, 128])
    mlt_d = din("mlt", [128, 128])
    negu_d = din("negu", [128, 128])
    lst_d = din("lst", [128, 128])
    iota_d = din("iota", [128, NE])
    cos_d = din("cos2", [128, KT])
    sin_d = din("sin2", [128, KT])
    out_d = nc.dram_tensor("out", [T, DM], F32, kind="ExternalOutput")

    omla_d = dscratch("omla", [4, 128, T])
    osb_d = dscratch("osb", [8, 64, T])
    h1_d = dscratch("h1", [T, DM])
    xbuf_d = dscratch("xbuf", [NSLOT + 128, DM], BF16, dbg_out=False)
    ybuf_d = dscratch("ybuf", [NSLOT, DM], dbg_out=False)
    dbg_d = dscratch("dbg", [T, 16]) if debug else None

    AP_ = Arena(nc, SB_BASE, "c")
    ident_b = AP_.alloc([128, 128], BF16)
    ident_f = AP_.alloc([128, 128], F32)
    mle_b = AP_.alloc([128, 128], BF16)
    mlt_b = AP_.alloc([128, 128], BF16)
    negu_b = AP_.alloc([128, 128], BF16)
    lst_b = AP_.alloc([128, 128], BF16)
    ones_b = AP_.alloc([128, 128], BF16)
    negones_b = AP_.alloc([128, 128], BF16)
    zeros_b = AP_.alloc([128, 128], BF16)
    ones_f = AP_.alloc([128, 128], F32)
    zeros512 = AP_.alloc([128, 512], BF16)
    iota_f = AP_.alloc([128, NE], F32)
    dtab = AP_.alloc([128, NT, 4], I32)
    gtab = AP_.alloc([128, NT, 4], F32)
    d_const = Dep()
    d_dtab = Dep()
    d_gtab = Dep()
    PBASE = AP_.off

    pTa = nc.alloc_psum_tensor("pTa", [128, 1024], BF16)
    pTb = nc.alloc_psum_tensor("pTb", [128, 1024], BF16)
    pF = [nc.alloc_psum_tensor("pF%d" % i, [128, 512], F32) for i in range(6)]
    d_pT = [Dep(True), Dep(True)]
    d_pF = [Dep(True) for _ in range(6)]
    pT = [pTa, pTb]

    def cdma(dst, src, cast):
        q = 'pool' if cast else 'sp'
        S.dma(q, lambda e: e.dma_start(out=dst, in_=src), 'const', writes=[d_const])

    cdma(ident_b[:], ident_d[:, :], True)
    cdma(ident_f[:], ident_d[:, :], False)
    cdma(mle_b[:], mle_d[:, :], True)
    cdma(mlt_b[:], mlt_d[:, :], True)
    cdma(negu_b[:], negu_d[:, :], True)
    cdma(lst_b[:], lst_d[:, :], True)
    cdma(iota_f[:], iota_d[:, :], False)
    S.op('pool', lambda e: e.memset(ones_b[:], 1.0), writes=[d_const])
    S.op('pool', lambda e: e.memset(negones_b[:], -1.0), writes=[d_const])
    S.op('pool', lambda e: e.memset(zeros_b[:], 0.0), writes=[d_const])
    S.op('pool', lambda e: e.memset(zeros512[:], 0.0), writes=[d_const])
    S.op('pool', lambda e: e.memset(ones_f[:], 1.0), writes=[d_const])

    d_xbufz = Dep()
    if 'merge' in phases:
        ztile = AP_.alloc([128, 4096], BF16)
        PBASE = AP_.off
        S.op('pool', lambda e: e.memset(ztile[:], 0.0), writes=[d_const])
        nz = (NSLOT + 128) // 512
        xz = xbuf_d[0:nz * 512, :].rearrange("(i p j) d -> i p (j d)", p=128, j=4)
        for i in range(nz):
            S.dma('sp', lambda e, i=i: e.dma_start(out=xz[i], in_=ztile[:]), 'xz', reads=[d_const], writes=[d_xbufz])
        rem = (NSLOT + 128) - nz * 512
        if rem:
            S.dma('sp', lambda e: e.dma_start(out=xbuf_d[nz * 512:nz * 512 + rem, :], in_=ztile[0:rem, 0:DM]), 'xz',
                  reads=[d_const], writes=[d_xbufz])

    x_t = x_d[:, :].rearrange("(g t p) d -> g p t d", p=128, t=4)
    w_in_t = w_in_d[:, :].rearrange("(kc p) c -> p kc c", p=128)

    def load_x_bf(xb, d_xb, g, key):
        if g < 0:
            S.dma('pool', lambda e: e.dma_start(out=xb[0:16, 0, :], in_=meta_d[:, :]), key, writes=[d_xb])
        else:
            S.dma('pool', lambda e: e.dma_start(out=xb[:, :, :], in_=x_t[g]), key, writes=[d_xb])

    def transpose_x(xb, d_xb, XT, d_XT, g):
        ntile = 1 if g < 0 else 4
        np_ = 16 if g < 0 else 128
        for t in range(ntile):
            pi = t % 2
            for c in range(8):
                S.op('pe', lambda e, t=t, c=c, pi=pi: e.transpose(out=pT[pi][:, c * 128:c * 128 + np_],
                                                                 in_=xb[0:np_, t, c * 128:(c + 1) * 128],
                                                                 identity=ident_b[0:np_, 0:np_]),
                     reads=[d_xb, d_const], writes=[d_pT[pi]])
            src = pT[pi][:, :].rearrange("p (c n) -> p c n", c=8)[:, :, 0:np_]
            S.op('dve', lambda e, t=t, src=src: e.tensor_copy(out=XT[:, :, t * 128:t * 128 + np_], in_=src),
                 reads=[d_pT[pi]], writes=[d_XT])

    def proj_fm(pbank, d_pb, wt, col0, XT, d_w, d_XT, N, nk=8, kp=128):
        for kc in range(nk):
            S.op('pe', lambda e, kc=kc: e.matmul(pbank[:, 0:N], lhsT=wt[0:kp, kc, col0:col0 + 128], rhs=XT[0:kp, kc, 0:N],
                                                 start=(kc == 0), stop=(kc == nk - 1)),
                 reads=[d_w, d_XT], writes=[d_pb])

    def rsqrt_mean(dst, d_dst, src_ps, d_src, n, eps, np_=128, N=512):
        S.op('act', lambda e: e.activation(out=dst[0:np_, 0:N], in_=src_ps[0:np_, 0:N], func=AF.Sqrt, bias=float(eps), scale=1.0 / n),
             reads=[d_src], writes=[d_dst])
        S.op('dve', lambda e: e.reciprocal(out=dst[0:np_, 0:N], in_=dst[0:np_, 0:N]), reads=[d_dst], writes=[d_dst])

    def mla_phase(hsel):
        A = Arena(nc, PBASE, "m%d" % hsel)
        heads = (2 * hsel, 2 * hsel + 1)
        KnT = A.alloc([128, 2, KT], BF16); d_KnT = Dep()
        krT = A.alloc([128, KT], BF16); d_krT = Dep()
        V = A.alloc([128, NT + 1, 256], BF16); d_V = Dep()
        win_b = A.alloc([128, 8, 768], BF16); d_win = Dep()
        wq_f = A.alloc([128, 2, 512], F32); d_wqf = Dep()
        wq_b = A.alloc([128, 2, 512], BF16); d_wq = Dep()
        wkv_f = A.alloc([128, 2, 512], F32); d_wkvf = Dep()
        wkv_b = A.alloc([128, 2, 512], BF16); d_wkv = Dep()
        qg = A.alloc([128, 2], F32)
        kvg = A.alloc([128, 2], F32)
        d_g = Dep()
        xb = [A.alloc([128, 4, DM], BF16) for _ in range(2)]; d_xb = [Dep(), Dep()]
        XT = A.alloc([128, 8, 512], BF16); d_XT = Dep()
        cqT = A.alloc([128, 2, 512], BF16); d_cq = Dep()
        ckvT = A.alloc([128, 2, 512], BF16); d_ckv = Dep()
        sqq = A.alloc([128, 2, 512], BF16); d_sqq = Dep()
        sqkv = A.alloc([128, 2, 512], BF16); d_sqkv = Dep()
        rq = A.alloc([128, 512], F32); d_rq = Dep()
        rkv = A.alloc([128, 512], F32); d_rkv = Dep()
        rcol = A.alloc([128, 64], F32); d_rcol = Dep()
        cs = A.alloc([128, 2, 512], F32); d_cs = Dep()
        t1 = A.alloc([128, 512], F32); d_t1 = Dep()
        t2 = A.alloc([128, 512], F32); d_t2 = Dep()
        QnT = A.alloc([128, 2, 512], BF16); d_Qn = Dep()
        QrT = A.alloc([128, 512], BF16); d_Qr = Dep()
        PT = [A.alloc([128, 512], BF16) for _ in range(4)]; d_PT = [Dep() for _ in range(4)]
        rec = A.alloc([128, 512], F32); d_rec = Dep()
        Racc = A.alloc([128, 512], F32); d_Racc = Dep()
        oT = [A.alloc([128, 512], F32) for _ in range(2)]; d_oT = [Dep(), Dep()]

        S.dma('pool', lambda e: e.dma_start(out=win_b[:, :, 0:576], in_=w_in_t[:, :, 0:576]), 'w1', writes=[d_win])
        S.dma('pool', lambda e: e.dma_start(out=win_b[:, :, 576:640], in_=w_in_t[:, :, 512:576]), 'w1', writes=[d_win])
        for dup in range(2):
            o = 640 + dup * 64
            S.dma('pool', lambda e, o=o: e.dma_start(out=win_b[:, :, o:o + 32], in_=w_in_t[:, :, 544:576]), 'w1', writes=[d_win])
            S.dma('pool', lambda e, o=o: e.dma_start(out=win_b[:, :, o + 32:o + 64], in_=w_in_t[:, :, 512:544]), 'w1', writes=[d_win])
        for dup in range(2):
            o = 640 + dup * 64
            S.op('pool', lambda e, o=o: e.tensor_scalar(out=win_b[:, :, o:o + 32], in0=win_b[:, :, o:o + 32], scalar1=-1.0, scalar2=None,
                                                        op0=ALU.mult), reads=[d_win], writes=[d_win])
        S.dma('sp', lambda e: e.dma_start(out=qg[:], in_=qg_d[:, :]), 'w2', writes=[d_g])
        S.dma('sp', lambda e: e.dma_start(out=kvg[:], in_=kvg_d[:, :]), 'w2', writes=[d_g])
        wuq_t = w_uq_d[:, :].rearrange("(kc p) c -> p kc c", p=128)
        wukv_t = w_ukv_d[:, :].rearrange("(kc p) c -> p kc c", p=128)
        hA, hB = heads
        S.dma('sp', lambda e: e.dma_start(out=wq_f[:, :, 0:128], in_=wuq_t[:, :, hA * 192:hA * 192 + 128]), 'w2', writes=[d_wqf])
        S.dma('sp', lambda e: e.dma_start(out=wq_f[:, :, 128:256], in_=wuq_t[:, :, hB * 192:hB * 192 + 128]), 'w2', writes=[d_wqf])
        for i, h in enumerate(heads):
            r0 = h * 192 + 128
            S.dma('sp', lambda e, i=i, r0=r0: e.dma_start(out=wq_f[:, :, 256 + i * 64:320 + i * 64], in_=wuq_t[:, :, r0:r0 + 64]), 'w2', writes=[d_wqf])
            S.dma('sp', lambda e, i=i, r0=r0: e.dma_start(out=wq_f[:, :, 384 + i * 64:416 + i * 64], in_=wuq_t[:, :, r0 + 32:r0 + 64]), 'w2', writes=[d_wqf])
            S.dma('sp', lambda e, i=i, r0=r0: e.dma_start(out=wq_f[:, :, 416 + i * 64:448 + i * 64], in_=wuq_t[:, :, r0:r0 + 32]), 'w2', writes=[d_wqf])
        for i, h in enumerate(heads):
            S.dma('sp', lambda e, i=i, h=h: e.dma_start(out=wkv_f[:, :, i * 128:(i + 1) * 128], in_=wukv_t[:, :, h * 256:h * 256 + 128]), 'w2', writes=[d_wkvf])
            S.dma('sp', lambda e, i=i, h=h: e.dma_start(out=wkv_f[:, :, 256 + i * 128:256 + (i + 1) * 128], in_=wukv_t[:, :, h * 256 + 128:h * 256 + 256]), 'w2', writes=[d_wkvf])
        for kc in range(2):
            S.op('dve', lambda e, kc=kc: e.tensor_scalar(out=wq_b[:, kc, :], in0=wq_f[:, kc, :], scalar1=qg[:, kc:kc + 1], scalar2=None, op0=ALU.mult),
                 reads=[d_wqf, d_g], writes=[d_wq])
            for i in range(2):
                o = 384 + i * 64
                S.op('dve', lambda e, kc=kc, o=o: e.tensor_scalar(out=wq_b[:, kc, o:o + 32], in0=wq_b[:, kc, o:o + 32], scalar1=-1.0, scalar2=None, op0=ALU.mult),
                     reads=[d_wq], writes=[d_wq])
            S.op('dve', lambda e, kc=kc: e.tensor_scalar(out=wkv_b[:, kc, :], in0=wkv_f[:, kc, :], scalar1=kvg[:, kc:kc + 1], scalar2=None, op0=ALU.mult),
                 reads=[d_wkvf, d_g], writes=[d_wkv])

        import os
        CUT = float(os.environ.get('MK_CUT', '99'))
        if CUT <= 1:
            S.barrier()
            return
        load_x_bf(xb[0], d_xb[0], -1, 'xb0')
        pcount = [0]

        def nextp():
            pcount[0] += 1
            return pcount[0] % 4

        for gi, g in enumerate([-1] + list(range(NG))):
            bi = gi % 2
            N = 16 if g < 0 else 512
            ntile = 1 if g < 0 else 4
            k0 = 0 if g < 0 else 16 + g * 512
            if gi + 1 <= NG:
                load_x_bf(xb[1 - bi], d_xb[1 - bi], g + 1, 'xb%d' % (1 - bi))
            S.dma('sp', lambda e, k0=k0, N=N: e.dma_start(out=cs[:, 0, 0:N], in_=cos_d[:, k0:k0 + N]), 'cs', writes=[d_cs])
            S.dma('sp', lambda e, k0=k0, N=N: e.dma_start(out=cs[:, 1, 0:N], in_=sin_d[:, k0:k0 + N]), 'cs', writes=[d_cs])
            transpose_x(xb[bi], d_xb[bi], XT, d_XT, g)
            if g >= 0 and CUT <= 2.1:
                break
            for j in range(2):
                p = nextp()
                proj_fm(pF[p], d_pF[p], win_b, 256 + j * 128, XT, d_win, d_XT, N)
                S.op('dve', lambda e, j=j, p=p: e.tensor_copy(out=ckvT[:, j, 0:N], in_=pF[p][:, 0:N]), reads=[d_pF[p]], writes=[d_ckv])
                S.op('act', lambda e, j=j, p=p: e.activation(out=sqkv[:, j, 0:N], in_=ckvT[:, j, 0:N], func=AF.Square), reads=[d_ckv], writes=[d_sqkv])
            if g >= 0 and CUT <= 2.15:
                break
            if g >= 0:
                for j in range(2):
                    p = nextp()
                    proj_fm(pF[p], d_pF[p], win_b, j * 128, XT, d_win, d_XT, N)
                    S.op('dve', lambda e, j=j, p=p: e.tensor_copy(out=cqT[:, j, 0:N], in_=pF[p][:, 0:N]), reads=[d_pF[p]], writes=[d_cq])
                    S.op('act', lambda e, j=j, p=p: e.activation(out=sqq[:, j, 0:N], in_=cqT[:, j, 0:N], func=AF.Square), reads=[d_cq], writes=[d_sqq])
            if g >= 0 and CUT <= 2.17:
                break
            p = nextp()
            proj_fm(pF[p], d_pF[p], win_b, 512, XT, d_win, d_XT, N)
            S.op('dve', lambda e, p=p: e.tensor_tensor(out=t1[:, 0:N], in0=pF[p][:, 0:N], in1=cs[:, 0, 0:N], op=ALU.mult), reads=[d_pF[p], d_cs], writes=[d_t1])
            p = nextp()
            proj_fm(pF[p], d_pF[p], win_b, 640, XT, d_win, d_XT, N)
            S.op('dve', lambda e, p=p: e.tensor_tensor(out=t2[:, 0:N], in0=pF[p][:, 0:N], in1=cs[:, 1, 0:N], op=ALU.mult), reads=[d_pF[p], d_cs], writes=[d_t2])
            S.op('dve', lambda e, k0=k0: e.tensor_tensor(out=krT[:, k0:k0 + N], in0=t1[:, 0:N], in1=t2[:, 0:N], op=ALU.add), reads=[d_t1, d_t2], writes=[d_krT])
            if g >= 0 and CUT <= 2.2:
                break
            p = nextp()
            for j in range(2):
                S.op('pe', lambda e, j=j, p=p: e.matmul(pF[p][:, 0:N], lhsT=ones_b[:, :], rhs=sqkv[:, j, 0:N], start=(j == 0), stop=(j == 1)),
                     reads=[d_sqkv, d_const], writes=[d_pF[p]])
            rsqrt_mean(rkv, d_rkv, pF[p], d_pF[p], 256.0, RMS_EPS, N=N)
            p = nextp()
            for t in range(ntile):
                nt_ = 16 if g < 0 else 128
                for j in range(2):
                    S.op('pe', lambda e, t=t, j=j, p=p, nt_=nt_: e.matmul(pF[p][0:nt_, 16 * t:16 * t + 16], lhsT=sqkv[:, j, t * 128:t * 128 + nt_], rhs=ones_b[:, 0:16],
                                                                        start=(j == 0), stop=(j == 1)),
                         reads=[d_sqkv, d_const], writes=[d_pF[p]])
            rsqrt_mean(rcol, d_rcol, pF[p], d_pF[p], 256.0, RMS_EPS, np_=(16 if g < 0 else 128), N=16 * ntile)
            if g >= 0:
                p = nextp()
                for j in range(2):
                    S.op('pe', lambda e, j=j, p=p: e.matmul(pF[p][:, 0:N], lhsT=ones_b[:, :], rhs=sqq[:, j, 0:N], start=(j == 0), stop=(j == 1)),
                         reads=[d_sqq, d_const], writes=[d_pF[p]])
                rsqrt_mean(rq, d_rq, pF[p], d_pF[p], 256.0, RMS_EPS, N=N)
            if g >= 0 and CUT <= 2.3:
                break
            for i in range(2):
                p = nextp()
                proj_fm(pF[p], d_pF[p], wkv_b, i * 128, ckvT, d_wkv, d_ckv, N, nk=2)
                S.op('dve', lambda e, i=i, p=p, k0=k0: e.tensor_tensor(out=KnT[:, i, k0:k0 + N], in0=pF[p][:, 0:N], in1=rkv[:, 0:N], op=ALU.mult),
                     reads=[d_pF[p], d_rkv], writes=[d_KnT])
            for t in range(ntile):
                nt_ = 16 if g < 0 else 128
                vt = 0 if g < 0 else 1 + g * 4 + t
                p = nextp()
                for kc in range(2):
                    S.op('pe', lambda e, t=t, kc=kc, p=p, nt_=nt_: e.matmul(pF[p][0:nt_, 0:256], lhsT=ckvT[:, kc, t * 128:t * 128 + nt_], rhs=wkv_b[:, kc, 256:512],
                                                                          start=(kc == 0), stop=(kc == 1)),
                         reads=[d_ckv, d_wkv], writes=[d_pF[p]])
                S.op('act', lambda e, t=t, p=p, nt_=nt_, vt=vt: e.activation(out=V[0:nt_, vt, :], in_=pF[p][0:nt_, 0:256], func=AF.Copy, scale=rcol[0:nt_, 16 * t:16 * t + 1]),
                     reads=[d_pF[p], d_rcol], writes=[d_V])
            if g < 0:
                if CUT <= 2:
                    break
                continue
            if CUT <= 2.4:
                break
            for i in range(2):
                p = nextp()
                proj_fm(pF[p], d_pF[p], wq_b, i * 128, cqT, d_wq, d_cq, N, nk=2)
                S.op('dve', lambda e, i=i, p=p: e.scalar_tensor_tensor(out=QnT[:, i, :], in0=pF[p][:, :], scalar=SCALE_MLA, in1=rq[:, :], op0=ALU.mult, op1=ALU.mult),
                     reads=[d_pF[p], d_rq], writes=[d_Qn])
            p = nextp()
            proj_fm(pF[p], d_pF[p], wq_b, 256, cqT, d_wq, d_cq, N, nk=2)
            S.op('dve', lambda e, p=p: e.tensor_tensor(out=t1[:, :], in0=pF[p][:, :], in1=cs[:, 0, :], op=ALU.mult), reads=[d_pF[p], d_cs], writes=[d_t1])
            p = nextp()
            proj_fm(pF[p], d_pF[p], wq_b, 384, cqT, d_wq, d_cq, N, nk=2)
            S.op('dve', lambda e, p=p: e.tensor_tensor(out=t2[:, :], in0=pF[p][:, :], in1=cs[:, 1, :], op=ALU.mult), reads=[d_pF[p], d_cs], writes=[d_t2])
            S.op('dve', lambda e: e.tensor_tensor(out=t1[:, :], in0=t1[:, :], in1=t2[:, :], op=ALU.add), reads=[d_t1, d_t2], writes=[d_t1])
            S.op('dve', lambda e: e.scalar_tensor_tensor(out=QrT[:, :], in0=t1[:, :], scalar=SCALE_MLA, in1=rq[:, :], op0=ALU.mult, op1=ALU.mult),
                 reads=[d_t1, d_rq], writes=[d_Qr])
            if CUT <= 3:
                break
            SK = 2
            tiles = []
            for i in range(2):
                blocks = [(-1, 0)] + [(kb, 0) for kb in range(4 * g)] + [(4 * g + j, j) for j in range(4)]
                for bi_, (kb, jq) in enumerate(blocks):
                    tiles.append((i, kb, jq, bi_ == 0, bi_ == len(blocks) - 1))
            pO, d_pO = pF[4], d_pF[4]
            pR, d_pR = pF[5], d_pF[5]

            def stA(ti):
                i, kb, jq, first, last = tiles[ti]
                nk_ = 16 if kb < 0 else 128
                kc0 = 0 if kb < 0 else 16 + kb * 128
                q0 = jq * 128
                pS, d_pS = pF[1 + ti % 3], d_pF[1 + ti % 3]
                pt, d_pt = PT[ti % 4], d_PT[ti % 4]
                S.op('pe', lambda e: e.matmul(pS[0:nk_, q0:512], lhsT=KnT[:, i, kc0:kc0 + nk_], rhs=QnT[:, i, q0:512], start=True, stop=False),
                     reads=[d_KnT, d_Qn], writes=[d_pS])
                S.op('pe', lambda e: e.matmul(pS[0:nk_, q0:512], lhsT=krT[i * 64:(i + 1) * 64, kc0:kc0 + nk_], rhs=QrT[i * 64:(i + 1) * 64, q0:512],
                                              start=False, stop=True), reads=[d_krT, d_Qr], writes=[d_pS])
                S.op('act', lambda e: e.activation(out=pt[0:nk_, q0:512], in_=pS[0:nk_, q0:512], func=AF.Exp), reads=[d_pS], writes=[d_pt])
                if kb >= 4 * g:
                    S.op('dve', lambda e: e.tensor_tensor(out=pt[:, q0:q0 + 128], in0=pt[:, q0:q0 + 128], in1=mle_b[:, :], op=ALU.mult),
                         reads=[d_pt, d_const], writes=[d_pt])

            def stB(ti):
                i, kb, jq, first, last = tiles[ti]
                nk_ = 16 if kb < 0 else 128
                vt = 0 if kb < 0 else 1 + kb
                q0 = jq * 128
                pt, d_pt = PT[ti % 4], d_PT[ti % 4]
                S.op('pe', lambda e: e.matmul(pO[:, q0:512], lhsT=V[0:nk_, vt, i * 128:(i + 1) * 128], rhs=pt[0:nk_, q0:512], start=first, stop=last,
                                              skip_group_check=True), reads=[d_V, d_pt], writes=[d_pO])
                S.op('pe', lambda e: e.matmul(pR[:, q0:512], lhsT=ones_b[0:nk_, :], rhs=pt[0:nk_, q0:512], start=first, stop=last,
                                              skip_group_check=True), reads=[d_const, d_pt], writes=[d_pR])
                if last:
                    S.op('dve', lambda e: e.reciprocal(out=rec[:, :], in_=pR[:, :]), reads=[d_pR], writes=[d_rec])
                    S.op('dve', lambda e: e.tensor_tensor(out=oT[i][:, :], in0=pO[:, :], in1=rec[:, :], op=ALU.mult), reads=[d_pO, d_rec], writes=[d_oT[i]])
                    h = heads[i]
                    S.dma('sp', lambda e: e.dma_start(out=omla_d[h, :, g * 512:(g + 1) * 512], in_=oT[i][:, :]), 'oT%d' % i, reads=[d_oT[i]])
            for s_ in range(len(tiles) + SK):
                if s_ < len(tiles):
                    stA(s_)
                if s_ >= SK:
                    stB(s_ - SK)
        S.barrier()

    def sb_phase(hsel):
        A = Arena(nc, PBASE, "s%d" % hsel)
        skT = A.alloc([128, 2, KT], BF16); d_skT = Dep()
        sv = A.alloc([128, NT + 1, 256], BF16); d_sv = Dep()
        win_b = A.alloc([128, 8, 768], BF16); d_win = Dep()
        xb = [A.alloc([128, 4, DM], BF16) for _ in range(2)]; d_xb = [Dep(), Dep()]
        XT = A.alloc([128, 8, 512], BF16); d_XT = Dep()
        sqT = A.alloc([128, 2, 512], BF16); d_sq = Dep()
        e32 = [A.alloc([128, 512], F32) for _ in range(3)]; d_e32 = [Dep() for _ in range(3)]
        spb = [A.alloc([128, 512], BF16) for _ in range(3)]; d_spb = [Dep() for _ in range(3)]
        wb = [A.alloc([128, 512], BF16) for _ in range(3)]; d_wb = [Dep() for _ in range(3)]
        L32 = A.alloc([128, 512], F32); d_L32 = Dep()
        Lb = [A.alloc([128, 512], BF16) for _ in range(3)]; d_Lb = [Dep() for _ in range(3)]
        oTs = [A.alloc([64, 512], F32) for _ in range(2)]; d_oTs = [Dep(), Dep()]
        c0 = 2 * hsel
        S.dma('pool', lambda e: e.dma_start(out=win_b[:, :, 0:256], in_=w_in_t[:, :, 576 + c0 * 128:576 + c0 * 128 + 256]), 'w1', writes=[d_win])
        S.dma('pool', lambda e: e.dma_start(out=win_b[:, :, 256:512], in_=w_in_t[:, :, 1088 + c0 * 128:1088 + c0 * 128 + 256]), 'w1', writes=[d_win])
        S.dma('pool', lambda e: e.dma_start(out=win_b[:, :, 512:768], in_=w_in_t[:, :, 1600 + c0 * 128:1600 + c0 * 128 + 256]), 'w1', writes=[d_win])
        load_x_bf(xb[0], d_xb[0], -1, 'xb0')
        pcount = [0]

        def nextp():
            pcount[0] += 1
            return pcount[0] % 4
        ocount = [0]
        for gi, g in enumerate([-1] + list(range(NG))):
            bi = gi % 2
            N = 16 if g < 0 else 512
            ntile = 1 if g < 0 else 4
            k0 = 0 if g < 0 else 16 + g * 512
            if gi + 1 <= NG:
                load_x_bf(xb[1 - bi], d_xb[1 - bi], g + 1, 'xb%d' % (1 - bi))
            transpose_x(xb[bi], d_xb[bi], XT, d_XT, g)
            for j in range(2):
                p = nextp()
                proj_fm(pF[p], d_pF[p], win_b, 256 + j * 128, XT, d_win, d_XT, N)
                S.op('dve', lambda e, j=j, p=p, k0=k0: e.tensor_copy(out=skT[:, j, k0:k0 + N], in_=pF[p][:, 0:N]), reads=[d_pF[p]], writes=[d_skT])
            for t in range(ntile):
                nt_ = 16 if g < 0 else 128
                vt = 0 if g < 0 else 1 + g * 4 + t
                p = nextp()
                for kc in range(8):
                    S.op('pe', lambda e, t=t, kc=kc, p=p, nt_=nt_: e.matmul(pF[p][0:nt_, 0:256], lhsT=XT[:, kc, t * 128:t * 128 + nt_], rhs=win_b[:, kc, 512:768],
                                                                          start=(kc == 0), stop=(kc == 7)),
                         reads=[d_XT, d_win], writes=[d_pF[p]])
                S.op('dve', lambda e, p=p, nt_=nt_, vt=vt: e.tensor_copy(out=sv[0:nt_, vt, :], in_=pF[p][0:nt_, 0:256]), reads=[d_pF[p]], writes=[d_sv])
            if g < 0:
                continue
            for j in range(2):
                p = nextp()
                proj_fm(pF[p], d_pF[p], win_b, j * 128, XT, d_win, d_XT, N)
                S.op('dve', lambda e, j=j, p=p: e.tensor_scalar(out=sqT[:, j, :], in0=pF[p][:, :], scalar1=0.125, scalar2=None, op0=ALU.mult),
                     reads=[d_pF[p]], writes=[d_sq])
            tiles = []
            for hh in range(4):
                blocks = [(4 * g + jq, jq) for jq in (3, 2, 1, 0)] + [(kb, 0) for kb in range(4 * g - 1, -1, -1)] + [(-1, 0)]
                for bi_, (kb, jq) in enumerate(blocks):
                    tiles.append((hh, kb, jq, bi_ == 0, bi_ == len(blocks) - 1))

            def geom(ti):
                hh, kb, jq, first, last = tiles[ti]
                nk_ = 16 if kb < 0 else 128
                kc0 = 0 if kb < 0 else 16 + kb * 128
                vt = 0 if kb < 0 else 1 + kb
                return hh, kb, jq * 128, first, last, nk_, kc0, vt, kb >= 4 * g

            def stA1(ti):
                hh, kb, q0, first, last, nk_, kc0, vt, diag = geom(ti)
                j = hh // 2
                hp = (hh % 2) * 64
                pS, d_pS = pF[ti % 4], d_pF[ti % 4]
                e_, d_e = e32[ti % 3], d_e32[ti % 3]
                if first:
                    pO, d_pO = pF[4 + hh % 2], d_pF[4 + hh % 2]
                    S.op('pe', lambda e: e.matmul(pO[0:64, :], lhsT=zeros_b[:, 0:64], rhs=zeros512[:, :], start=True, stop=False, skip_group_check=True),
                         reads=[d_const], writes=[d_pO])
                S.op('pe', lambda e: e.matmul(pS[0:nk_, q0:512], lhsT=skT[hp:hp + 64, j, kc0:kc0 + nk_], rhs=sqT[hp:hp + 64, j, q0:512],
                                              start=True, stop=False, skip_group_check=True), reads=[d_skT, d_sq], writes=[d_pS])
                S.op('act', lambda e: e.activation(out=e_[0:nk_, q0:512], in_=pS[0:nk_, q0:512], func=AF.Exp), reads=[d_pS], writes=[d_e])

            def stA2(ti):
                hh, kb, q0, first, last, nk_, kc0, vt, diag = geom(ti)
                e_, d_e = e32[ti % 3], d_e32[ti % 3]
                sp_, d_sp = spb[ti % 3], d_spb[ti % 3]
                S.op('act', lambda e: e.activation(out=sp_[0:nk_, q0:512], in_=e_[0:nk_, q0:512], func=AF.Ln, bias=1.0), reads=[d_e], writes=[d_sp])
                if diag:
                    S.op('dve', lambda e: e.tensor_tensor(out=sp_[:, q0:q0 + 128], in0=sp_[:, q0:q0 + 128], in1=mlt_b[:, :], op=ALU.mult),
                         reads=[d_sp, d_const], writes=[d_sp])
                if not last:
                    lprev = zeros512 if first else Lb[(ti - 1) % 3]
                    d_lprev = d_const if first else d_Lb[(ti - 1) % 3]
                    if q0 > 0:
                        S.op('dve', lambda e: e.tensor_copy(out=Lb[ti % 3][:, 0:q0], in_=lprev[:, 0:q0]), reads=[d_lprev], writes=[d_Lb[ti % 3]])
                    S.op('dve', lambda e: e.tensor_tensor(out=Lb[ti % 3][:, q0:512], in0=lprev[:, q0:512], in1=sp_[:, q0:512], op=ALU.add),
                         reads=[d_sp, d_lprev], writes=[d_Lb[ti % 3]])

            def stB(ti):
                hh, kb, q0, first, last, nk_, kc0, vt, diag = geom(ti)
                pS, d_pS = pF[ti % 4], d_pF[ti % 4]
                sp_, d_sp = spb[ti % 3], d_spb[ti % 3]
                w_, d_w = wb[ti % 3], d_wb[ti % 3]
                S.op('pe', lambda e: e.matmul(pS[0:nk_, q0:512], lhsT=negu_b[0:nk_, 0:nk_], rhs=sp_[0:nk_, q0:512], start=False, stop=first,
                                              skip_group_check=True), reads=[d_sp, d_const], writes=[d_pS])
                if not first:
                    lcur = (ti - 1) % 3
                    S.op('pe', lambda e: e.matmul(pS[0:nk_, q0:512], lhsT=negones_b[:, 0:nk_], rhs=Lb[lcur][:, q0:512], start=False, stop=True,
                                                  skip_group_check=True), reads=[d_Lb[lcur], d_const], writes=[d_pS])
                S.op('act', lambda e: e.activation(out=w_[0:nk_, q0:512], in_=pS[0:nk_, q0:512], func=AF.Exp), reads=[d_pS], writes=[d_w])
                if diag:
                    S.op('dve', lambda e: e.tensor_tensor(out=w_[:, q0:q0 + 128], in0=w_[:, q0:q0 + 128], in1=mlt_b[:, :], op=ALU.mult),
                         reads=[d_w, d_const], writes=[d_w])

            def stC(ti):
                hh, kb, q0, first, last, nk_, kc0, vt, diag = geom(ti)
                w_, d_w = wb[ti % 3], d_wb[ti % 3]
                pO, d_pO = pF[4 + hh % 2], d_pF[4 + hh % 2]
                S.op('pe', lambda e: e.matmul(pO[0:64, q0:512], lhsT=sv[0:nk_, vt, hh * 64:(hh + 1) * 64], rhs=w_[0:nk_, q0:512], start=False, stop=last,
                                              skip_group_check=True), reads=[d_sv, d_w], writes=[d_pO])
                if last:
                    oi = hh % 2
                    head = 4 * hsel + hh
                    S.op('dve', lambda e: e.tensor_copy(out=oTs[oi][:, :], in_=pO[0:64, :]), reads=[d_pO], writes=[d_oTs[oi]])
                    S.dma('sp', lambda e: e.dma_start(out=osb_d[head, :, g * 512:(g + 1) * 512], in_=oTs[oi][:, :]), 'oS%d' % oi, reads=[d_oTs[oi]])
            nt_all = len(tiles)
            for s_ in range(nt_all + 3):
                if s_ < nt_all:
                    stA1(s_)
                if 1 <= s_ <= nt_all:
                    stA2(s_ - 1)
                if 2 <= s_ <= nt_all + 1:
                    stB(s_ - 2)
                if s_ >= 3:
                    stC(s_ - 3)
        S.barrier()

    def merge_phase():
        A = Arena(nc, PBASE, "g")
        wo_f = A.alloc([128, 4, 512], F32); d_wof = Dep()
        wo_m = A.alloc([128, 4, DM], BF16)
        wo_s = A.alloc([64, 8, DM], BF16)
        d_wo = Dep()
        gm = A.alloc([128, 4], F32)
        gs = A.alloc([64, 8], F32)
        g1 = A.alloc([128, DM], F32)
        b1 = A.alloc([128, DM], F32)
        wr_f = A.alloc([128, 8, NE], F32)
        br_f = A.alloc([1, NE], F32)
        d_w = Dep()
        om = [A.alloc([128, 4, 512], F32) for _ in range(2)]; d_om = [Dep(), Dep()]
        os_ = [A.alloc([64, 8, 512], F32) for _ in range(2)]; d_os = [Dep(), Dep()]
        sqm = A.alloc([128, 4, 512], BF16); d_sqm = Dep()
        sqs = A.alloc([64, 8, 512], BF16); d_sqs = Dep()
        rrm = A.alloc([128, 512], F32); d_rrm = Dep()
        rrs = A.alloc([128, 512], F32); d_rrs = Dep()
        onm = A.alloc([128, 4, 512], BF16); d_onm = Dep()
        ons = A.alloc([64, 8, 512], BF16); d_ons = Dep()
        xt = [A.alloc([128, DM], F32) for _ in range(2)]; d_xt = [Dep(), Dep()]
        hp_ = A.alloc([128, DM], F32); d_hp = Dep()
        junk = A.alloc([128, DM], BF16); d_junk = Dep()
        h1 = [A.alloc([128, DM], F32) for _ in range(2)]; d_h1 = [Dep(), Dep()]
        h1b = [A.alloc([128, DM], BF16) for _ in range(2)]; d_h1b = [Dep(), Dep()]
        h1T = A.alloc([128, 8, 128], F32); d_h1T = Dep()
        st = A.alloc([128, 16], F32); d_st = Dep()
        lg = A.alloc([128, NE], F32); d_lg = Dep()
        top8 = A.alloc([128, 8], F32); d_top8 = Dep()
        idx8 = A.alloc([128, 8], U32); d_idx8 = Dep()
        idxf = A.alloc([128, 4], F32); d_idxf = Dep()
        e4 = A.alloc([128, 4], F32); d_e4 = Dep()
        maskb = A.alloc([128, NE], BF16); d_mask = Dep()
        rank = A.alloc([128, NE], F32); d_rank = Dep()
        base = A.alloc([128, NE], F32); d_base = Dep()
        oh = A.alloc([128, NE], F32); d_oh = Dep()
        oh2 = A.alloc([128, NE], F32)
        rsel = A.alloc([128, 4], F32); d_rsel = Dep()
        destf = A.alloc([128, 4], F32); d_destf = Dep()

        wo_t = w_o_d[:, :]
        S.dma('sp', lambda e: e.dma_start(out=gm[:], in_=gm_d[:, :]), 'w2', writes=[d_w])
        S.dma('sp', lambda e: e.dma_start(out=gs[:], in_=gs_d[:, :]), 'w2', writes=[d_w])
        S.dma('sp', lambda e: e.dma_start(out=g1[:], in_=ln1g_d[:, :]), 'w2', writes=[d_w])
        S.dma('sp', lambda e: e.dma_start(out=b1[:], in_=ln1b_d[:, :]), 'w2', writes=[d_w])
        S.dma('sp', lambda e: e.dma_start(out=wr_f[:], in_=wr_d[:, :].rearrange("(kc p) n -> p kc n", p=128)), 'w2', writes=[d_w])
        S.dma('sp', lambda e: e.dma_start(out=br_f[:], in_=br_d[:, :]), 'w2', writes=[d_w])
        for half in range(2):
            S.dma('sp', lambda e, half=half: e.dma_start(out=wo_f[:, :, :], in_=wo_t[0:512, half * 512:(half + 1) * 512].rearrange("(c p) n -> p c n", p=128)),
                  'w3', writes=[d_wof])
            for c in range(4):
                S.op('dve', lambda e, c=c, half=half: e.tensor_scalar(out=wo_m[:, c, half * 512:(half + 1) * 512], in0=wo_f[:, c, :], scalar1=gm[:, c:c + 1],
                                                                      scalar2=None, op0=ALU.mult), reads=[d_wof, d_w], writes=[d_wo])
            for q4 in range(2):
                S.dma('sp', lambda e, half=half, q4=q4: e.dma_start(
                    out=wo_f[0:64, :, :], in_=wo_t[512 + q4 * 256:512 + (q4 + 1) * 256, half * 512:(half + 1) * 512].rearrange("(c p) n -> p c n", p=64)),
                    'w3', writes=[d_wof])
                for c in range(4):
                    S.op('dve', lambda e, c=c, half=half, q4=q4: e.tensor_scalar(out=wo_s[:, q4 * 4 + c, half * 512:(half + 1) * 512], in0=wo_f[0:64, c, :],
                                                                              scalar1=gs[:, q4 * 4 + c:q4 * 4 + c + 1], scalar2=None, op0=ALU.mult),
                         reads=[d_wof, d_w], writes=[d_wo])
        S.op('pool', lambda e: e.memset(base[:], 0.0), writes=[d_base])

        def load_o(g, bi):
            S.dma('sp', lambda e: e.dma_start(out=om[bi][:, :, :], in_=omla_d[:, :, g * 512:(g + 1) * 512].rearrange("c p t -> p c t")), 'om%d' % bi, writes=[d_om[bi]])
            S.dma('sp', lambda e: e.dma_start(out=os_[bi][:, :, :], in_=osb_d[:, :, g * 512:(g + 1) * 512].rearrange("c p t -> p c t")), 'os%d' % bi, writes=[d_os[bi]])

        def load_xt(tt, bi):
            S.dma('sp', lambda e: e.dma_start(out=xt[bi][:, :], in_=x_d[tt * 128:(tt + 1) * 128, :]), 'xt%d' % bi, writes=[d_xt[bi]])
        load_o(0, 0)
        load_xt(0, 0)
        for g in range(NG):
            bi = g % 2
            if g + 1 < NG:
                load_o(g + 1, 1 - bi)
            for c2 in range(2):
                S.op('act', lambda e, c2=c2: e.activation(out=sqm[:, 2 * c2:2 * c2 + 2, :], in_=om[bi][:, 2 * c2:2 * c2 + 2, :], func=AF.Square), reads=[d_om[bi]], writes=[d_sqm])
            for c2 in range(4):
                S.op('act', lambda e, c2=c2: e.activation(out=sqs[:, 2 * c2:2 * c2 + 2, :], in_=os_[bi][:, 2 * c2:2 * c2 + 2, :], func=AF.Square), reads=[d_os[bi]], writes=[d_sqs])
            for c in range(4):
                S.op('pe', lambda e, c=c: e.matmul(pF[2][:, :], lhsT=ones_b[:, :], rhs=sqm[:, c, :], start=(c == 0), stop=(c == 3)), reads=[d_sqm, d_const], writes=[d_pF[2]])
            rsqrt_mean(rrm, d_rrm, pF[2], d_pF[2], 512.0, RMS_EPS)
            for c in range(8):
                S.op('pe', lambda e, c=c: e.matmul(pF[3][:, :], lhsT=ones_b[0:64, :], rhs=sqs[0:64, c, :], start=(c == 0), stop=(c == 7)), reads=[d_sqs, d_const], writes=[d_pF[3]])
            rsqrt_mean(rrs, d_rrs, pF[3], d_pF[3], 512.0, RMS_EPS)
            for c in range(4):
                S.op('dve', lambda e, c=c: e.tensor_tensor(out=onm[:, c, :], in0=om[bi][:, c, :], in1=rrm[:, :], op=ALU.mult), reads=[d_om[bi], d_rrm], writes=[d_onm])
            for c in range(8):
                S.op('dve', lambda e, c=c: e.tensor_tensor(out=ons[:, c, :], in0=os_[bi][:, c, :], in1=rrs[0:64, :], op=ALU.mult), reads=[d_os[bi], d_rrs], writes=[d_ons])
            for t in range(4):
                tt = g * 4 + t
                tb = tt % 2
                if tt + 1 < NT:
                    load_xt(tt + 1, 1 - tb)
                S.op('pool', lambda e: e.memset(st[:, :], 0.0), writes=[d_st])
                for half in range(2):
                    pa, d_pa = pF[half], d_pF[half]
                    for c in range(4):
                        S.op('pe', lambda e, c=c, t=t, half=half, pa=pa: e.matmul(pa[:, :], lhsT=onm[:, c, t * 128:(t + 1) * 128], rhs=wo_m[:, c, half * 512:(half + 1) * 512],
                                                                                 start=(c == 0), stop=False), reads=[d_onm, d_wo], writes=[d_pa])
                    for c in range(8):
                        S.op('pe', lambda e, c=c, t=t, half=half, pa=pa: e.matmul(pa[:, :], lhsT=ons[0:64, c, t * 128:(t + 1) * 128], rhs=wo_s[0:64, c, half * 512:(half + 1) * 512],
                                                                                 start=False, stop=(c == 7)), reads=[d_ons, d_wo], writes=[d_pa])
                    S.op('dve', lambda e, half=half, pa=pa, tb=tb: e.scalar_tensor_tensor(out=hp_[:, half * 512:(half + 1) * 512], in0=xt[tb][:, half * 512:(half + 1) * 512],
                                                                                        scalar=ALPHA, in1=pa[:, :], op0=ALU.mult, op1=ALU.add, accum_out=st[:, half:half + 1]),
                         reads=[d_xt[tb], d_pa, d_st], writes=[d_hp, d_st])
                layer_norm(hp_, d_hp, st, d_st, junk, d_junk, g1, b1, d_w, h1[tb], d_h1[tb])
                S.dma('sp', lambda e, tt=tt, tb=tb: e.dma_start(out=h1_d[tt * 128:(tt + 1) * 128, :], in_=h1[tb][:, :]), 'h1o%d' % tb, reads=[d_h1[tb]])
                S.op('act', lambda e, tb=tb: e.activation(out=h1b[tb][:, :], in_=h1[tb][:, :], func=AF.Copy), reads=[d_h1[tb]], writes=[d_h1b[tb]])
                for hf in range(2):
                    pX, d_pX = pF[4 + hf], d_pF[4 + hf]
                    for c in range(4):
                        cc = hf * 4 + c
                        S.op('pe', lambda e, c=c, cc=cc, pX=pX, tb=tb: e.transpose(out=pX[:, c * 128:(c + 1) * 128], in_=h1[tb][:, cc * 128:(cc + 1) * 128], identity=ident_f[:, :]),
                             reads=[d_h1[tb], d_const], writes=[d_pX])
                    S.op('act', lambda e, hf=hf, pX=pX: e.activation(out=h1T[:, hf * 4:(hf + 1) * 4, :], in_=pX[:, :].rearrange("p (c n) -> p c n", c=4), func=AF.Copy),
                         reads=[d_pX], writes=[d_h1T])
                pL, d_pL = pF[2], d_pF[2]
                for c in range(8):
                    S.op('pe', lambda e, c=c: e.matmul(pL[:, 0:NE], lhsT=h1T[:, c, :], rhs=wr_f[:, c, :], start=(c == 0), stop=False), reads=[d_h1T, d_w], writes=[d_pL])
                S.op('pe', lambda e: e.matmul(pL[:, 0:NE], lhsT=ones_f[0:1, :], rhs=br_f[0:1, :], start=False, stop=True), reads=[d_const, d_w], writes=[d_pL])
                S.op('dve', lambda e: e.tensor_copy(out=lg[:, :], in_=pL[:, 0:NE]), reads=[d_pL], writes=[d_lg])
                S.op('dve', lambda e: e.max(out=top8[:, :], in_=lg[:, :]), reads=[d_lg], writes=[d_top8])
                S.op('dve', lambda e: e.max_index(out=idx8[:, :], in_max=top8[:, :], in_values=lg[:, :]), reads=[d_lg, d_top8], writes=[d_idx8])
                S.op('dve', lambda e: e.tensor_copy(out=idxf[:, :], in_=idx8[:, 0:4]), reads=[d_idx8], writes=[d_idxf])
                S.op('dve', lambda e: e.tensor_scalar(out=st[:, 8:9], in0=top8[:, 0:1], scalar1=-1.0, scalar2=None, op0=ALU.mult), reads=[d_top8, d_st], writes=[d_st])
                S.op('act', lambda e: e.activation(out=e4[:, :], in_=top8[:, 0:4], func=AF.Exp, bias=st[:, 8:9], scale=1.0, accum_out=st[:, 9:10]),
                     reads=[d_top8, d_st], writes=[d_e4, d_st])
                S.op('dve', lambda e: e.reciprocal(out=st[:, 10:11], in_=st[:, 9:10]), reads=[d_st], writes=[d_st])
                S.op('dve', lambda e, tt=tt: e.tensor_scalar(out=gtab[:, tt, :], in0=e4[:, :], scalar1=st[:, 10:11], scalar2=None, op0=ALU.mult),
                     reads=[d_e4, d_st], writes=[d_gtab])
                S.op('dve', lambda e: e.tensor_scalar(out=maskb[:, :], in0=lg[:, :], scalar1=top8[:, 3:4], scalar2=None, op0=ALU.is_ge), reads=[d_lg, d_top8], writes=[d_mask])
                pK, d_pK = pF[3], d_pF[3]
                S.op('pe', lambda e: e.matmul(pK[:, 0:NE], lhsT=lst_b[:, :], rhs=maskb[:, :], start=True, stop=True), reads=[d_mask, d_const], writes=[d_pK])
                S.op('pe', lambda e: e.matmul(pK[:, NE:2 * NE], lhsT=ones_b[:, :], rhs=maskb[:, :], start=True, stop=True), reads=[d_mask, d_const], writes=[d_pK])
                S.op('dve', lambda e: e.tensor_tensor(out=rank[:, :], in0=pK[:, 0:NE], in1=base[:, :], op=ALU.add), reads=[d_pK, d_base], writes=[d_rank])
                S.op('dve', lambda e: e.tensor_tensor(out=base[:, :], in0=pK[:, NE:2 * NE], in1=base[:, :], op=ALU.add), reads=[d_pK, d_base], writes=[d_base])
                S.op('pool', lambda e: e.memset(rsel[:, :], 0.0), writes=[d_rsel])
                for k in range(4):
                    S.op('dve', lambda e, k=k: e.tensor_scalar(out=oh[:, :], in0=iota_f[:, :], scalar1=idxf[:, k:k + 1], scalar2=None, op0=ALU.is_equal),
                         reads=[d_const, d_idxf], writes=[d_oh])
                    S.op('dve', lambda e, k=k: e.scalar_tensor_tensor(out=oh2[:, :], in0=oh[:, :], scalar=1.0, in1=rank[:, :], op0=ALU.mult, op1=ALU.mult,
                                                                      accum_out=rsel[:, k:k + 1]), reads=[d_oh, d_rank, d_rsel], writes=[d_rsel, d_oh])
                S.op('dve', lambda e: e.tensor_scalar(out=rsel[:, :], in0=rsel[:, :], scalar1=float(CAP - 1), scalar2=None, op0=ALU.min), reads=[d_rsel], writes=[d_rsel])
                S.op('dve', lambda e: e.scalar_tensor_tensor(out=destf[:, :], in0=idxf[:, :], scalar=float(CAP), in1=rsel[:, :], op0=ALU.mult, op1=ALU.add),
                     reads=[d_idxf, d_rsel], writes=[d_destf])
                S.op('dve', lambda e, tt=tt: e.tensor_copy(out=dtab[:, tt, :], in_=destf[:, :]), reads=[d_destf], writes=[d_dtab])
                if debug:
                    S.dma('sp', lambda e, tt=tt: e.dma_start(out=dbg_d[tt * 128:(tt + 1) * 128, 0:4], in_=destf[:, :]), 'dbg%d' % tb, reads=[d_destf])
                    S.dma('sp', lambda e, tt=tt: e.dma_start(out=dbg_d[tt * 128:(tt + 1) * 128, 4:8], in_=gtab[:, tt, :]), 'dbg%d' % tb, reads=[d_gtab])
                S.wait_dma('pool', 'sc%d' % (1 - tb))
                for k in range(4):
                    S.dma('pool', lambda e, tt=tt, k=k, tb=tb: e.indirect_dma_start(
                        out=xbuf_d[:, :], out_offset=bass.IndirectOffsetOnAxis(ap=dtab[:, tt, k:k + 1], axis=0), in_=h1b[tb][:, :], in_offset=None), 'sc%d' % tb, reads=[d_h1b[tb], d_dtab, d_xbufz])
        S.barrier()

    def layer_norm(hp_, d_hp, st, d_st, junk, d_junk, gt, bt, d_gb, out, d_out):
        S.op('dve', lambda e: e.tensor_tensor(out=st[:, 2:3], in0=st[:, 0:1], in1=st[:, 1:2], op=ALU.add), reads=[d_st], writes=[d_st])
        S.op('dve', lambda e: e.tensor_scalar(out=st[:, 2:3], in0=st[:, 2:3], scalar1=-1.0 / DM, scalar2=None, op0=ALU.mult), reads=[d_st], writes=[d_st])
        S.op('act', lambda e: e.activation(out=junk[:, :], in_=hp_[:, :], func=AF.Square, bias=st[:, 2:3], scale=1.0, accum_out=st[:, 3:4]),
             reads=[d_hp, d_st], writes=[d_junk, d_st])
        S.op('act', lambda e: e.activation(out=st[:, 4:5], in_=st[:, 3:4], func=AF.Sqrt, bias=float(LN_EPS), scale=1.0 / DM), reads=[d_st], writes=[d_st])
        S.op('dve', lambda e: e.reciprocal(out=st[:, 4:5], in_=st[:, 4:5]), reads=[d_st], writes=[d_st])
        S.op('dve', lambda e: e.tensor_tensor(out=st[:, 5:6], in0=st[:, 2:3], in1=st[:, 4:5], op=ALU.mult), reads=[d_st], writes=[d_st])
        S.op('act', lambda e: e.activation(out=out[:, :], in_=hp_[:, :], func=AF.Identity, bias=st[:, 5:6], scale=st[:, 4:5]),
             reads=[d_hp, d_st], writes=[d_out])
        S.op('dve', lambda e: e.tensor_tensor(out=out[:, :], in0=out[:, :], in1=gt[:, :], op=ALU.mult), reads=[d_out, d_gb], writes=[d_out])
        S.op('dve', lambda e: e.tensor_tensor(out=out[:, :], in0=out[:, :], in1=bt[:, :], op=ALU.add), reads=[d_out, d_gb], writes=[d_out])

    def moe_phase():
        A = Arena(nc, PBASE, "e")
        wg_b = [A.alloc([128, 8, DM], BF16) for _ in range(2)]
        wu_b = [A.alloc([128, 8, DM], BF16) for _ in range(2)]
        wd_b = [A.alloc([128, 8, DM], BF16) for _ in range(2)]
        d_wg = [Dep(), Dep()]; d_wu = [Dep(), Dep()]; d_wd = [Dep(), Dep()]
        bgu = A.alloc([128, NE, 16], F32); d_bgu = Dep()
        bd_f = [A.alloc([1, DM], F32) for _ in range(2)]; d_bdf = [Dep(), Dep()]
        bdb = [A.alloc([128, DM], F32) for _ in range(2)]; d_bdb = [Dep(), Dep()]
        xs = [A.alloc([128, 4, DM], BF16) for _ in range(2)]; d_xs = [Dep(), Dep()]
        XsT = A.alloc([128, 8, 512], BF16); d_XsT = Dep()
        g32 = [A.alloc([128, 512], F32) for _ in range(2)]; d_g32 = [Dep(), Dep()]
        s32 = [A.alloc([128, 512], F32) for _ in range(2)]; d_s32 = [Dep(), Dep()]
        u32 = [A.alloc([128, 512], F32) for _ in range(2)]; d_u32 = [Dep(), Dep()]
        aT = [A.alloc([128, 8, 512], BF16) for _ in range(2)]; d_aT = [Dep(), Dep()]
        ys = [A.alloc([128, DM], F32) for _ in range(2)]; d_ys = [Dep(), Dep()]
        S.dma('sp', lambda e: e.dma_start(out=bgu[:], in_=bgu_d[:, :, :]), 'w2', writes=[d_bgu])

        def load_w(ex, bi):
            for (wt, src, dd, key) in ((wg_b, wg_d, d_wg, 'we'), (wu_b, wu_d, d_wg, 'we'), (wd_b, wd_d, d_wg, 'we')):
                for hf in range(2):
                    S.dma('pool', lambda e, wt=wt, src=src, hf=hf: e.dma_start(out=wt[bi][:, hf * 4:(hf + 1) * 4, :],
                                                                                in_=src[ex, hf * 512:(hf + 1) * 512, :].rearrange("(kc p) f -> p kc f", p=128)),
                          '%s%d' % (key, bi), writes=[dd[bi]])
            S.dma('sp', lambda e: e.dma_start(out=bd_f[bi][:, :], in_=bd_d[ex:ex + 1, :]), 'bd%d' % bi, writes=[d_bdf[bi]])
        groups = []
        off = 0
        while off < CAP:
            n = min(512, CAP - off)
            groups.append((off, n))
            off += n
        load_w(0, 0)
        gcount = 0
        ycount = 0
        for ex in range(NE):
            wi = ex % 2
            if ex + 1 < NE:
                load_w(ex + 1, 1 - wi)
            for half in range(2):
                S.op('pe', lambda e, half=half: e.matmul(pF[5][:, :], lhsT=ones_f[0:1, :], rhs=bd_f[wi][0:1, half * 512:(half + 1) * 512], start=True, stop=True),
                     reads=[d_const, d_bdf[wi]], writes=[d_pF[5]])
                S.op('act', lambda e, half=half: e.activation(out=bdb[wi][:, half * 512:(half + 1) * 512], in_=pF[5][:, :], func=AF.Copy), reads=[d_pF[5]], writes=[d_bdb[wi]])
            for (soff, N) in groups:
                xi = gcount % 2
                gcount += 1
                ntile = N // 128
                row0 = ex * CAP + soff
                S.dma('sp', lambda e, xi=xi, row0=row0, ntile=ntile, N=N: e.dma_start(out=xs[xi][:, 0:ntile, :],
                                                                                     in_=xbuf_d[row0:row0 + N, :].rearrange("(t p) d -> p t d", p=128)),
                      'xs%d' % xi, writes=[d_xs[xi]])
                for t in range(ntile):
                    pi = t % 2
                    for c in range(8):
                        S.op('pe', lambda e, t=t, c=c, pi=pi, xi=xi: e.transpose(out=pT[pi][:, c * 128:(c + 1) * 128], in_=xs[xi][:, t, c * 128:(c + 1) * 128], identity=ident_b[:, :]),
                             reads=[d_xs[xi], d_const], writes=[d_pT[pi]])
                    S.op('dve', lambda e, t=t, pi=pi: e.tensor_copy(out=XsT[:, :, t * 128:(t + 1) * 128], in_=pT[pi][:, :].rearrange("p (c n) -> p c n", c=8)),
                         reads=[d_pT[pi]], writes=[d_XsT])
                ai = gcount % 2
                for fc in range(8):
                    fi = fc % 2
                    for kc in range(8):
                        S.op('pe', lambda e, fc=fc, kc=kc, N=N: e.matmul(pF[0][:, 0:N], lhsT=wg_b[wi][:, kc, fc * 128:(fc + 1) * 128], rhs=XsT[:, kc, 0:N], start=(kc == 0), stop=(kc == 7)),
                             reads=[d_wg[wi], d_XsT], writes=[d_pF[0]])
                    for kc in range(8):
                        S.op('pe', lambda e, fc=fc, kc=kc, N=N: e.matmul(pF[1][:, 0:N], lhsT=wu_b[wi][:, kc, fc * 128:(fc + 1) * 128], rhs=XsT[:, kc, 0:N], start=(kc == 0), stop=(kc == 7)),
                             reads=[d_wg[wi], d_XsT], writes=[d_pF[1]])
                    S.op('dve', lambda e, fc=fc, fi=fi, N=N, ex=ex: e.tensor_scalar(out=g32[fi][:, 0:N], in0=pF[0][:, 0:N], scalar1=bgu[:, ex, fc:fc + 1], scalar2=7.0, op0=ALU.add, op1=ALU.min),
                         reads=[d_pF[0], d_bgu], writes=[d_g32[fi]])
                    S.op('act', lambda e, fi=fi, N=N: e.activation(out=s32[fi][:, 0:N], in_=g32[fi][:, 0:N], func=AF.Silu, scale=1.702), reads=[d_g32[fi]], writes=[d_s32[fi]])
                    S.op('dve', lambda e, fc=fc, fi=fi, N=N, ex=ex: e.tensor_scalar(out=u32[fi][:, 0:N], in0=pF[1][:, 0:N], scalar1=bgu[:, ex, 8 + fc:9 + fc], scalar2=7.0, op0=ALU.add, op1=ALU.min),
                         reads=[d_pF[1], d_bgu], writes=[d_u32[fi]])
                    S.op('dve', lambda e, fi=fi, N=N: e.tensor_scalar(out=u32[fi][:, 0:N], in0=u32[fi][:, 0:N], scalar1=-7.0, scalar2=1.0, op0=ALU.max, op1=ALU.add),
                         reads=[d_u32[fi]], writes=[d_u32[fi]])
                    S.op('dve', lambda e, fc=fc, fi=fi, N=N, ai=ai: e.scalar_tensor_tensor(out=aT[ai][:, fc, 0:N], in0=s32[fi][:, 0:N], scalar=1.0 / 1.702, in1=u32[fi][:, 0:N],
                                                                                          op0=ALU.mult, op1=ALU.mult),
                         reads=[d_s32[fi], d_u32[fi]], writes=[d_aT[ai]])
                for t in range(ntile):
                    yi = ycount % 2
                    ycount += 1
                    for half in range(2):
                        pY, d_pY = pF[2 + half], d_pF[2 + half]
                        for fc in range(8):
                            S.op('pe', lambda e, t=t, fc=fc, half=half, pY=pY, ai=ai: e.matmul(pY[:, :], lhsT=aT[ai][:, fc, t * 128:(t + 1) * 128], rhs=wd_b[wi][:, fc, half * 512:(half + 1) * 512],
                                                                                              start=(fc == 0), stop=(fc == 7)), reads=[d_aT[ai], d_wg[wi]], writes=[d_pY])
                        S.op('dve', lambda e, half=half, pY=pY, yi=yi: e.tensor_tensor(out=ys[yi][:, half * 512:(half + 1) * 512], in0=pY[:, :], in1=bdb[wi][:, half * 512:(half + 1) * 512], op=ALU.add),
                             reads=[d_pY, d_bdb[wi]], writes=[d_ys[yi]])
                    r0 = row0 + t * 128
                    S.dma('sp', lambda e, yi=yi, r0=r0: e.dma_start(out=ybuf_d[r0:r0 + 128, :], in_=ys[yi][:, :]), 'yo%d' % yi, reads=[d_ys[yi]])
        S.barrier()

    def comb_phase():
        A = Arena(nc, PBASE, "f")
        g2 = A.alloc([128, DM], F32)
        b2 = A.alloc([128, DM], F32)
        d_gb = Dep()
        yk = [[A.alloc([128, DM], F32) for _ in range(4)] for _ in range(2)]
        d_yk = [[Dep() for _ in range(4)] for _ in range(2)]
        hh = [A.alloc([128, DM], F32) for _ in range(2)]; d_hh = [Dep(), Dep()]
        acc = A.alloc([128, DM], F32); d_acc = Dep()
        junk = A.alloc([128, DM], BF16); d_junk = Dep()
        st = A.alloc([128, 16], F32); d_st = Dep()
        ot = [A.alloc([128, DM], F32) for _ in range(2)]; d_ot = [Dep(), Dep()]
        S.dma('sp', lambda e: e.dma_start(out=g2[:], in_=ln2g_d[:, :]), 'w2', writes=[d_gb])
        S.dma('sp', lambda e: e.dma_start(out=b2[:], in_=ln2b_d[:, :]), 'w2', writes=[d_gb])

        def load(tt, bi):
            S.dma('sp', lambda e: e.dma_start(out=hh[bi][:, :], in_=h1_d[tt * 128:(tt + 1) * 128, :]), 'hh%d' % bi, writes=[d_hh[bi]])
            S.wait_dma('pool', 'yk%d' % (1 - bi))
            for k in range(4):
                S.dma('pool', lambda e, k=k: e.indirect_dma_start(out=yk[bi][k][:, :], out_offset=None, in_=ybuf_d[:, :],
                                                                  in_offset=bass.IndirectOffsetOnAxis(ap=dtab[:, tt, k:k + 1], axis=0)),
                      'yk%d' % bi, reads=[d_dtab], writes=[d_yk[bi][0]])
        load(0, 0)
        for tt in range(NT):
            bi = tt % 2
            if tt + 1 < NT:
                load(tt + 1, 1 - bi)
            S.op('pool', lambda e: e.memset(st[:, :], 0.0), writes=[d_st])
            S.op('act', lambda e: e.activation(out=acc[:, :], in_=hh[bi][:, :], func=AF.Copy, scale=ALPHA), reads=[d_hh[bi]], writes=[d_acc])
            for k in range(3):
                eng = 'dve'
                S.op(eng, lambda e, k=k: e.scalar_tensor_tensor(out=acc[:, :], in0=yk[bi][k][:, :], scalar=gtab[:, tt, k:k + 1], in1=acc[:, :], op0=ALU.mult, op1=ALU.add),
                     reads=[d_yk[bi][0], d_gtab, d_acc], writes=[d_acc])
            S.op('dve', lambda e: e.scalar_tensor_tensor(out=acc[:, :], in0=yk[bi][3][:, :], scalar=gtab[:, tt, 3:4], in1=acc[:, :], op0=ALU.mult, op1=ALU.add,
                                                          accum_out=st[:, 0:1]), reads=[d_yk[bi][0], d_gtab, d_acc, d_st], writes=[d_acc, d_st])
            layer_norm(acc, d_acc, st, d_st, junk, d_junk, g2, b2, d_gb, ot[bi], d_ot[bi])
            S.dma('sp', lambda e, tt=tt: e.dma_start(out=out_d[tt * 128:(tt + 1) * 128, :], in_=ot[bi][:, :]), 'out%d' % bi, reads=[d_ot[bi]])

    for ph in phases:
        if ph == 'mla0':
            mla_phase(0)
        elif ph == 'mla1':
            mla_phase(1)
        elif ph == 'sb0':
            sb_phase(0)
        elif ph == 'sb1':
            sb_phase(1)
        elif ph == 'merge':
            merge_phase()
        elif ph == 'moe':
            moe_phase()
        elif ph == 'comb':
            comb_phase()
    S.barrier()

    from contextlib import ExitStack
    with ExitStack() as es:
        sems = {}
        for k in S.semkeys:
            nm = "s_" + "_".join(str(a) for a in k)
            sems[k] = es.enter_context(nc.semaphore(nm))
        with nc.Block() as block:
            S.emit(block, sems)
    return nc, S


def rope_tables(npos):
    half = 32
    freqs = (np.float32(10000.0) ** (-(np.arange(half, dtype=np.float32) * np.float32(2.0)) / np.float32(64))).astype(np.float32)
    ang = (np.arange(npos, dtype=np.float32)[:, None] * freqs[None, :]).astype(np.float32)
    c = np.cos(ang.astype(np.float64)).astype(np.float32).T
    s = np.sin(ang.astype(np.float64)).astype(np.float32).T
    return np.ascontiguousarray(np.tile(c, (4, 1))), np.ascontiguousarray(np.tile(s, (4, 1)))


def make_consts(T):
    k = np.arange(128)
    cos2, sin2 = rope_tables(T + 16)
    return {
        "ident": np.eye(128, dtype=np.float32),
        "mle": (k[:, None] <= k[None, :]).astype(np.float32),
        "mlt": (k[:, None] < k[None, :]).astype(np.float32),
        "negu": -(k[:, None] >= k[None, :]).astype(np.float32),
        "lst": (k[:, None] < k[None, :]).astype(np.float32),
        "iota": np.tile(np.arange(NE, dtype=np.float32)[None, :], (128, 1)),
        "cos2": cos2, "sin2": sin2,
    }


def make_shared(inp):
    f = lambda a: np.ascontiguousarray(a, dtype=np.float32)
    d = {}
    d["meta"] = f(inp["meta_tokens"])
    d["w_in"] = f(inp["w_in"][0])
    d["qg"] = f(inp["q_norm_g"][0].reshape(2, 128).T)
    d["w_uq"] = f(inp["w_uq"][0])
    d["kvg"] = f(inp["kv_norm_g"][0].reshape(2, 128).T)
    d["w_ukv"] = f(inp["w_ukv"][0])
    d["gm"] = f(inp["mla_out_g"][0].reshape(4, 128).T)
    d["gs"] = f(inp["sb_out_g"][0].reshape(8, 64).T)
    d["w_o"] = f(inp["w_o"][0])
    d["ln1g"] = f(np.tile(inp["ln1_g"][0][None, :], (128, 1)))
    d["ln1b"] = f(np.tile(inp["ln1_b"][0][None, :], (128, 1)))
    d["wr"] = f(inp["w_router"][0])
    d["br"] = f(inp["b_router"][0][None, :])
    d["wg"] = f(inp["w_gate"][0])
    d["wu"] = f(inp["w_up"][0])
    d["wd"] = f(inp["w_down"][0])
    bg = np.asarray(inp["b_gate"][0]).reshape(NE, 8, 128).transpose(2, 0, 1)
    bu = np.asarray(inp["b_up"][0]).reshape(NE, 8, 128).transpose(2, 0, 1)
    d["bgu"] = f(np.concatenate([bg, bu], axis=2))
    d["bd"] = f(inp["b_down"][0])
    d["ln2g"] = f(np.tile(inp["ln2_g"][0][None, :], (128, 1)))
    d["ln2b"] = f(np.tile(inp["ln2_b"][0][None, :], (128, 1)))
    return d


CAP_FULL = 1280


def kernel(**inputs):
    x = np.asarray(inputs["x"])
    B, L, _ = x.shape
    NG = L // 512
    nc, _ = build(NG, CAP_FULL)
    shared = make_shared(inputs)
    shared.update(make_consts(L))
    in_maps = []
    for b in range(B):
        m = dict(shared)
        m["x"] = np.ascontiguousarray(x[b], dtype=np.float32)
        in_maps.append(m)
    res = run_bass_kernel_spmd(nc, in_maps, core_ids=list(range(B)))
    return np.stack([np.asarray(r["out"]) for r in res.results], axis=0).astype(np.float32)
```

```python
import numpy as np
import concourse.bass as bass
import concourse.mybir as mybir
from concourse.bass_utils import run_bass_kernel_spmd

F32 = mybir.dt.float32
BF16 = mybir.dt.bfloat16
I32 = mybir.dt.int32
U32 = mybir.dt.uint32
ALU = mybir.AluOpType
AF = mybir.ActivationFunctionType

ENGS = ['pe', 'act', 'dve', 'pool', 'sp']
EPOCH = 30000
SETUP_KEYS = ('const', 'w1', 'w2')
DM = 1024
NE = 32
SCALE_MLA = float(192 ** -0.5)
ALPHA = float(2.0 ** 0.25)
LN_EPS = 1e-5
RMS_EPS = 1e-6
SB_BASE = 24576
SB_END = 229344


class Dep:
    __slots__ = ('w', 'r', 'x')

    def __init__(self, excl=False):
        self.w = None
        self.r = {}
        self.x = excl


class _Rec:
    def __getattr__(self, name):
        def f(*a, **k):
            return (name, a, k)
        return f


_REC = _Rec()


class Sched:
    def __init__(self):
        self.prog = {e: [] for e in ENGS}
        self.cnt = {e: 0 for e in ENGS}
        self.epoch = {e: 0 for e in ENGS}
        self.waited = {e: {} for e in ENGS}
        self.dma_cnt = {}
        self.semkeys = []
        for e in ENGS:
            if e != 'sp':
                self.semkeys.append((e, 0))

    def _need(self, eng, reads, writes):
        need = {}

        def add(d):
            if d is None:
                return
            k, v = d
            if need.get(k, 0) < v:
                need[k] = v
        for b in reads:
            add(b.w)
            if b.x:
                for k_, d in b.r.items():
                    if k_[0] != eng:
                        add(d)
        for b in writes:
            add(b.w)
            for d in b.r.values():
                add(d)
        wl = self.waited[eng]
        for k, v in need.items():
            if eng == 'pe' and k[0] == 'pe':
                continue
            if k[0] == 'dma' and k[1] in SETUP_KEYS:
                v = self.dma_cnt[k]
            if wl.get(k, 0) >= v:
                continue
            wl[k] = v
            self.prog[eng].append(('wait', k, v))

    def op(self, eng, fn, reads=(), writes=()):
        self._need(eng, reads, writes)
        if self.cnt[eng] >= EPOCH:
            self.epoch[eng] += 1
            self.cnt[eng] = 0
            self.semkeys.append((eng, self.epoch[eng]))
        self.cnt[eng] += 1
        key = (eng, self.epoch[eng])
        tok = (key, self.cnt[eng])
        self.prog[eng].append(('op', fn(_REC), key))
        for b in reads:
            b.r[key] = tok
        for b in writes:
            b.w = tok
            b.r = {}

    def dma(self, q, fn, key, reads=(), writes=(), n=1):
        self._need(q, reads, writes)
        k = ('dma', key, 'sw' if q == 'pool' else 'hw')
        if k not in self.dma_cnt:
            self.dma_cnt[k] = 0
            self.semkeys.append(k)
        self.dma_cnt[k] += 16 * n
        tok = (k, self.dma_cnt[k])
        self.prog[q].append(('dma', fn(_REC), k))
        for b in reads:
            b.r[k] = tok
        for b in writes:
            b.w = tok
            b.r = {}

    def wait_dma(self, eng, key, q='pool'):
        k = ('dma', key, 'sw' if q == 'pool' else 'hw')
        v = self.dma_cnt.get(k, 0)
        if v and self.waited[eng].get(k, 0) < v:
            self.waited[eng][k] = v
            self.prog[eng].append(('wait', k, v))

    def barrier(self):
        cur = {}
        for e in ENGS:
            if e != 'sp' and self.cnt[e] > 0:
                cur[(e, self.epoch[e])] = self.cnt[e]
        for k, v in self.dma_cnt.items():
            cur[k] = v
        for e in ENGS:
            wl = self.waited[e]
            for k, v in cur.items():
                if e == 'pe' and k[0] == 'pe':
                    continue
                if wl.get(k, 0) >= v:
                    continue
                wl[k] = v
                self.prog[e].append(('wait', k, v))

    def emit(self, block, sems):
        handles = {'pe': 'tensor', 'act': 'scalar', 'dve': 'vector', 'pool': 'gpsimd', 'sp': 'sync'}

        def run(eng_name):
            def body(e):
                for item in self.prog[eng_name]:
                    if item[0] == 'wait':
                        e.wait_ge(sems[item[1]], item[2])
                    elif item[0] == 'op':
                        nm, a, k = item[1]
                        getattr(e, nm)(*a, **k).then_inc(sems[item[2]], 1)
                    else:
                        nm, a, k = item[1]
                        try:
                            getattr(e, nm)(*a, **k).then_inc(sems[item[2]], 16)
                        except Exception:
                            print("DMA FAIL", eng_name, nm, item[2], k)
                            raise
            return body
        for en in ENGS:
            getattr(block, handles[en])(run(en))


class Arena:
    def __init__(self, nc, base, tag):
        self.nc = nc
        self.off = base
        self.tag = tag
        self.n = 0

    def alloc(self, shape, dt):
        sz = int(np.prod(shape[1:])) * (2 if dt == BF16 else 4)
        off = (self.off + 31) // 32 * 32
        self.n += 1
        t = self.nc.alloc_sbuf_tensor_at("%s_%d" % (self.tag, self.n), list(shape), dt, offset=off)
        self.off = off + sz
        assert self.off <= SB_END, (self.tag, self.off)
        return t


def build(NG, CAP, debug=False, phases=('mla0', 'mla1', 'sb0', 'sb1', 'merge', 'moe', 'comb')):
    T = NG * 512
    NT = NG * 4
    KT = T + 16
    NSLOT = NE * CAP
    nc = bass.Bass("TRN2", target_bir_lowering=False)
    S = Sched()

    def din(name, shape, dt=F32):
        return nc.dram_tensor(name, list(shape), dt, kind="ExternalInput")

    def dscratch(name, shape, dt=F32, dbg_out=True):
        return nc.dram_tensor(name, list(shape), dt, kind="ExternalOutput" if (debug and dbg_out) else "Internal")

    x_d = din("x", [T, DM])
    meta_d = din("meta", [16, DM])
    w_in_d = din("w_in", [DM, 2112])
    qg_d = din("qg", [128, 2])
    w_uq_d = din("w_uq", [256, 768])
    kvg_d = din("kvg", [128, 2])
    w_ukv_d = din("w_ukv", [256, 1024])
    gm_d = din("gm", [128, 4])
    gs_d = din("gs", [64, 8])
    w_o_d = din("w_o", [DM, DM])
    ln1g_d = din("ln1g", [128, DM])
    ln1b_d = din("ln1b", [128, DM])
    wr_d = din("wr", [DM, NE])
    br_d = din("br", [1, NE])
    if 'moe' in phases:
        wg_d = din("wg", [NE, DM, DM])
        wu_d = din("wu", [NE, DM, DM])
        wd_d = din("wd", [NE, DM, DM])
    bgu_d = din("bgu", [128, NE, 16])
    bd_d = din("bd", [NE, DM])
    ln2g_d = din("ln2g", [128, DM])
    ln2b_d = din("ln2b", [128, DM])
    ident_d = din("ident", [128, 128])
    mle_d = din("mle", [128, 128])
    mlt_d = din("mlt", [128, 128])
    negu_d = din("negu", [128, 128])
    lst_d = din("lst", [128, 128])
    iota_d = din("iota", [128, NE])
    cos_d = din("cos2", [128, KT])
    sin_d = din("sin2", [128, KT])
    out_d = nc.dram_tensor("out", [T, DM], F32, kind="ExternalOutput")

    omla_d = dscratch("omla", [4, 128, T])
    osb_d = dscratch("osb", [8, 64, T])
    h1_d = dscratch("h1", [T, DM])
    xbuf_d = dscratch("xbuf", [NSLOT + 128, DM], BF16, dbg_out=False)
    ybuf_d = dscratch("ybuf", [NSLOT, DM], dbg_out=False)
    dbg_d = dscratch("dbg", [T, 16]) if debug else None

    AP_ = Arena(nc, SB_BASE, "c")
    ident_b = AP_.alloc([128, 128], BF16)
    ident_f = AP_.alloc([128, 128], F32)
    mle_b = AP_.alloc([128, 128], BF16)
    mlt_b = AP_.alloc([128, 128], BF16)
    negu_b = AP_.alloc([128, 128], BF16)
    lst_b = AP_.alloc([128, 128], BF16)
    ones_b = AP_.alloc([128, 128], BF16)
    negones_b = AP_.alloc([128, 128], BF16)
    zeros_b = AP_.alloc([128, 128], BF16)
    ones_f = AP_.alloc([128, 128], F32)
    zeros512 = AP_.alloc([128, 512], BF16)
    iota_f = AP_.alloc([128, NE], F32)
    dtab = AP_.alloc([128, NT, 4], I32)
    gtab = AP_.alloc([128, NT, 4], F32)
    d_const = Dep()
    d_dtab = Dep()
    d_gtab = Dep()
    PBASE = AP_.off

    pTa = nc.alloc_psum_tensor("pTa", [128, 1024], BF16)
    pTb = nc.alloc_psum_tensor("pTb", [128, 1024], BF16)
    pF = [nc.alloc_psum_tensor("pF%d" % i, [128, 512], F32) for i in range(6)]
    d_pT = [Dep(True), Dep(True)]
    d_pF = [Dep(True) for _ in range(6)]
    pT = [pTa, pTb]

    def cdma(dst, src, cast):
        q = 'pool' if cast else 'sp'
        S.dma(q, lambda e: e.dma_start(out=dst, in_=src), 'const', writes=[d_const])

    cdma(ident_b[:], ident_d[:, :], True)
    cdma(ident_f[:], ident_d[:, :], False)
    cdma(mle_b[:], mle_d[:, :], True)
    cdma(mlt_b[:], mlt_d[:, :], True)
    cdma(negu_b[:], negu_d[:, :], True)
    cdma(lst_b[:], lst_d[:, :], True)
    cdma(iota_f[:], iota_d[:, :], False)
    S.op('pool', lambda e: e.memset(ones_b[:], 1.0), writes=[d_const])
    S.op('pool', lambda e: e.memset(negones_b[:], -1.0), writes=[d_const])
    S.op('pool', lambda e: e.memset(zeros_b[:], 0.0), writes=[d_const])
    S.op('pool', lambda e: e.memset(zeros512[:], 0.0), writes=[d_const])
    S.op('pool', lambda e: e.memset(ones_f[:], 1.0), writes=[d_const])

    d_xbufz = Dep()
    if 'merge' in phases:
        ztile = AP_.alloc([128, 4096], BF16)
        PBASE = AP_.off
        S.op('pool', lambda e: e.memset(ztile[:], 0.0), writes=[d_const])
        nz = (NSLOT + 128) // 512
        xz = xbuf_d[0:nz * 512, :].rearrange("(i p j) d -> i p (j d)", p=128, j=4)
        for i in range(nz):
            S.dma('sp', lambda e, i=i: e.dma_start(out=xz[i], in_=ztile[:]), 'xz', reads=[d_const], writes=[d_xbufz])
        rem = (NSLOT + 128) - nz * 512
        if rem:
            S.dma('sp', lambda e: e.dma_start(out=xbuf_d[nz * 512:nz * 512 + rem, :], in_=ztile[0:rem, 0:DM]), 'xz',
                  reads=[d_const], writes=[d_xbufz])

    x_t = x_d[:, :].rearrange("(g t p) d -> g p t d", p=128, t=4)
    w_in_t = w_in_d[:, :].rearrange("(kc p) c -> p kc c", p=128)

    def load_x_bf(xb, d_xb, g, key):
        if g < 0:
            S.dma('pool', lambda e: e.dma_start(out=xb[0:16, 0, :], in_=meta_d[:, :]), key, writes=[d_xb])
        else:
            S.dma('pool', lambda e: e.dma_start(out=xb[:, :, :], in_=x_t[g]), key, writes=[d_xb])

    def transpose_x(xb, d_xb, XT, d_XT, g):
        ntile = 1 if g < 0 else 4
        np_ = 16 if g < 0 else 128
        for t in range(ntile):
            pi = t % 2
            for c in range(8):
                S.op('pe', lambda e, t=t, c=c, pi=pi: e.transpose(out=pT[pi][:, c * 128:c * 128 + np_],
                                                                 in_=xb[0:np_, t, c * 128:(c + 1) * 128],
                                                                 identity=ident_b[0:np_, 0:np_]),
                     reads=[d_xb, d_const], writes=[d_pT[pi]])
            src = pT[pi][:, :].rearrange("p (c n) -> p c n", c=8)[:, :, 0:np_]
            S.op('dve', lambda e, t=t, src=src: e.tensor_copy(out=XT[:, :, t * 128:t * 128 + np_], in_=src),
                 reads=[d_pT[pi]], writes=[d_XT])

    def proj_fm(pbank, d_pb, wt, col0, XT, d_w, d_XT, N, nk=8, kp=128):
        for kc in range(nk):
            S.op('pe', lambda e, kc=kc: e.matmul(pbank[:, 0:N], lhsT=wt[0:kp, kc, col0:col0 + 128], rhs=XT[0:kp, kc, 0:N],
                                                 start=(kc == 0), stop=(kc == nk - 1)),
                 reads=[d_w, d_XT], writes=[d_pb])

    def rsqrt_mean(dst, d_dst, src_ps, d_src, n, eps, np_=128, N=512):
        S.op('act', lambda e: e.activation(out=dst[0:np_, 0:N], in_=src_ps[0:np_, 0:N], func=AF.Sqrt, bias=float(eps), scale=1.0 / n),
             reads=[d_src], writes=[d_dst])
        S.op('dve', lambda e: e.reciprocal(out=dst[0:np_, 0:N], in_=dst[0:np_, 0:N]), reads=[d_dst], writes=[d_dst])

    def mla_phase(hsel):
        A = Arena(nc, PBASE, "m%d" % hsel)
        heads = (2 * hsel, 2 * hsel + 1)
        KnT = A.alloc([128, 2, KT], BF16); d_KnT = Dep()
        krT = A.alloc([128, KT], BF16); d_krT = Dep()
        V = A.alloc([128, NT + 1, 256], BF16); d_V = Dep()
        win_b = A.alloc([128, 8, 768], BF16); d_win = Dep()
        wq_f = A.alloc([128, 2, 512], F32); d_wqf = Dep()
        wq_b = A.alloc([128, 2, 512], BF16); d_wq = Dep()
        wkv_f = A.alloc([128, 2, 512], F32); d_wkvf = Dep()
        wkv_b = A.alloc([128, 2, 512], BF16); d_wkv = Dep()
        qg = A.alloc([128, 2], F32)
        kvg = A.alloc([128, 2], F32)
        d_g = Dep()
        xb = [A.alloc([128, 4, DM], BF16) for _ in range(2)]; d_xb = [Dep(), Dep()]
        XT = A.alloc([128, 8, 512], BF16); d_XT = Dep()
        cqT = A.alloc([128, 2, 512], BF16); d_cq = Dep()
        ckvT = A.alloc([128, 2, 512], BF16); d_ckv = Dep()
        sqq = A.alloc([128, 2, 512], BF16); d_sqq = Dep()
        sqkv = A.alloc([128, 2, 512], BF16); d_sqkv = Dep()
        rq = A.alloc([128, 512], F32); d_rq = Dep()
        rkv = A.alloc([128, 512], F32); d_rkv = Dep()
        rcol = A.alloc([128, 64], F32); d_rcol = Dep()
        cs = A.alloc([128, 2, 512], F32); d_cs = Dep()
        t1 = A.alloc([128, 512], F32); d_t1 = Dep()
        t2 = A.alloc([128, 512], F32); d_t2 = Dep()
        QnT = A.alloc([128, 2, 512], BF16); d_Qn = Dep()
        QrT = A.alloc([128, 512], BF16); d_Qr = Dep()
        PT = [A.alloc([128, 512], BF16) for _ in range(5)]; d_PT = [Dep() for _ in range(5)]
        rec = A.alloc([128, 512], F32); d_rec = Dep()
        Racc = A.alloc([128, 512], F32); d_Racc = Dep()
        oT = [A.alloc([128, 512], F32) for _ in range(2)]; d_oT = [Dep(), Dep()]

        S.dma('pool', lambda e: e.dma_start(out=win_b[:, :, 0:576], in_=w_in_t[:, :, 0:576]), 'w1', writes=[d_win])
        S.dma('pool', lambda e: e.dma_start(out=win_b[:, :, 576:640], in_=w_in_t[:, :, 512:576]), 'w1', writes=[d_win])
        for dup in range(2):
            o = 640 + dup * 64
            S.dma('pool', lambda e, o=o: e.dma_start(out=win_b[:, :, o:o + 32], in_=w_in_t[:, :, 544:576]), 'w1', writes=[d_win])
            S.dma('pool', lambda e, o=o: e.dma_start(out=win_b[:, :, o + 32:o + 64], in_=w_in_t[:, :, 512:544]), 'w1', writes=[d_win])
        for dup in range(2):
            o = 640 + dup * 64
            S.op('pool', lambda e, o=o: e.tensor_scalar(out=win_b[:, :, o:o + 32], in0=win_b[:, :, o:o + 32], scalar1=-1.0, scalar2=None,
                                                        op0=ALU.mult), reads=[d_win], writes=[d_win])
        S.dma('sp', lambda e: e.dma_start(out=qg[:], in_=qg_d[:, :]), 'w2', writes=[d_g])
        S.dma('sp', lambda e: e.dma_start(out=kvg[:], in_=kvg_d[:, :]), 'w2', writes=[d_g])
        wuq_t = w_uq_d[:, :].rearrange("(kc p) c -> p kc c", p=128)
        wukv_t = w_ukv_d[:, :].rearrange("(kc p) c -> p kc c", p=128)
        hA, hB = heads
        S.dma('sp', lambda e: e.dma_start(out=wq_f[:, :, 0:128], in_=wuq_t[:, :, hA * 192:hA * 192 + 128]), 'w2', writes=[d_wqf])
        S.dma('sp', lambda e: e.dma_start(out=wq_f[:, :, 128:256], in_=wuq_t[:, :, hB * 192:hB * 192 + 128]), 'w2', writes=[d_wqf])
        for i, h in enumerate(heads):
            r0 = h * 192 + 128
            S.dma('sp', lambda e, i=i, r0=r0: e.dma_start(out=wq_f[:, :, 256 + i * 64:320 + i * 64], in_=wuq_t[:, :, r0:r0 + 64]), 'w2', writes=[d_wqf])
            S.dma('sp', lambda e, i=i, r0=r0: e.dma_start(out=wq_f[:, :, 384 + i * 64:416 + i * 64], in_=wuq_t[:, :, r0 + 32:r0 + 64]), 'w2', writes=[d_wqf])
            S.dma('sp', lambda e, i=i, r0=r0: e.dma_start(out=wq_f[:, :, 416 + i * 64:448 + i * 64], in_=wuq_t[:, :, r0:r0 + 32]), 'w2', writes=[d_wqf])
        for i, h in enumerate(heads):
            S.dma('sp', lambda e, i=i, h=h: e.dma_start(out=wkv_f[:, :, i * 128:(i + 1) * 128], in_=wukv_t[:, :, h * 256:h * 256 + 128]), 'w2', writes=[d_wkvf])
            S.dma('sp', lambda e, i=i, h=h: e.dma_start(out=wkv_f[:, :, 256 + i * 128:256 + (i + 1) * 128], in_=wukv_t[:, :, h * 256 + 128:h * 256 + 256]), 'w2', writes=[d_wkvf])
        for kc in range(2):
            S.op('dve', lambda e, kc=kc: e.tensor_scalar(out=wq_b[:, kc, :], in0=wq_f[:, kc, :], scalar1=qg[:, kc:kc + 1], scalar2=None, op0=ALU.mult),
                 reads=[d_wqf, d_g], writes=[d_wq])
            for i in range(2):
                o = 384 + i * 64
                S.op('dve', lambda e, kc=kc, o=o: e.tensor_scalar(out=wq_b[:, kc, o:o + 32], in0=wq_b[:, kc, o:o + 32], scalar1=-1.0, scalar2=None, op0=ALU.mult),
                     reads=[d_wq], writes=[d_wq])
            S.op('dve', lambda e, kc=kc: e.tensor_scalar(out=wkv_b[:, kc, :], in0=wkv_f[:, kc, :], scalar1=kvg[:, kc:kc + 1], scalar2=None, op0=ALU.mult),
                 reads=[d_wkvf, d_g], writes=[d_wkv])

        import os
        CUT = float(os.environ.get('MK_CUT', '99'))
        if CUT <= 1:
            S.barrier()
            return
        load_x_bf(xb[0], d_xb[0], -1, 'xb0')
        pcount = [0]

        def nextp():
            pcount[0] += 1
            return pcount[0] % 4

        for gi, g in enumerate([-1] + list(range(NG))):
            bi = gi % 2
            N = 16 if g < 0 else 512
            ntile = 1 if g < 0 else 4
            k0 = 0 if g < 0 else 16 + g * 512
            if gi + 1 <= NG:
                load_x_bf(xb[1 - bi], d_xb[1 - bi], g + 1, 'xb%d' % (1 - bi))
            S.dma('sp', lambda e, k0=k0, N=N: e.dma_start(out=cs[:, 0, 0:N], in_=cos_d[:, k0:k0 + N]), 'cs', writes=[d_cs])
            S.dma('sp', lambda e, k0=k0, N=N: e.dma_start(out=cs[:, 1, 0:N], in_=sin_d[:, k0:k0 + N]), 'cs', writes=[d_cs])
            transpose_x(xb[bi], d_xb[bi], XT, d_XT, g)
            if g >= 0 and CUT <= 2.1:
                break
            for j in range(2):
                p = nextp()
                proj_fm(pF[p], d_pF[p], win_b, 256 + j * 128, XT, d_win, d_XT, N)
                S.op('dve', lambda e, j=j, p=p: e.tensor_copy(out=ckvT[:, j, 0:N], in_=pF[p][:, 0:N]), reads=[d_pF[p]], writes=[d_ckv])
                S.op('act', lambda e, j=j, p=p: e.activation(out=sqkv[:, j, 0:N], in_=ckvT[:, j, 0:N], func=AF.Square), reads=[d_ckv], writes=[d_sqkv])
            if g >= 0 and CUT <= 2.15:
                break
            if g >= 0:
                for j in range(2):
                    p = nextp()
                    proj_fm(pF[p], d_pF[p], win_b, j * 128, XT, d_win, d_XT, N)
                    S.op('dve', lambda e, j=j, p=p: e.tensor_copy(out=cqT[:, j, 0:N], in_=pF[p][:, 0:N]), reads=[d_pF[p]], writes=[d_cq])
                    S.op('act', lambda e, j=j, p=p: e.activation(out=sqq[:, j, 0:N], in_=cqT[:, j, 0:N], func=AF.Square), reads=[d_cq], writes=[d_sqq])
            if g >= 0 and CUT <= 2.17:
                break
            p = nextp()
            proj_fm(pF[p], d_pF[p], win_b, 512, XT, d_win, d_XT, N)
            S.op('dve', lambda e, p=p: e.tensor_tensor(out=t1[:, 0:N], in0=pF[p][:, 0:N], in1=cs[:, 0, 0:N], op=ALU.mult), reads=[d_pF[p], d_cs], writes=[d_t1])
            p = nextp()
            proj_fm(pF[p], d_pF[p], win_b, 640, XT, d_win, d_XT, N)
            S.op('dve', lambda e, p=p: e.tensor_tensor(out=t2[:, 0:N], in0=pF[p][:, 0:N], in1=cs[:, 1, 0:N], op=ALU.mult), reads=[d_pF[p], d_cs], writes=[d_t2])
            S.op('dve', lambda e, k0=k0: e.tensor_tensor(out=krT[:, k0:k0 + N], in0=t1[:, 0:N], in1=t2[:, 0:N], op=ALU.add), reads=[d_t1, d_t2], writes=[d_krT])
            if g >= 0 and CUT <= 2.2:
                break
            p = nextp()
            for j in range(2):
                S.op('pe', lambda e, j=j, p=p: e.matmul(pF[p][:, 0:N], lhsT=ones_b[:, :], rhs=sqkv[:, j, 0:N], start=(j == 0), stop=(j == 1)),
                     reads=[d_sqkv, d_const], writes=[d_pF[p]])
            rsqrt_mean(rkv, d_rkv, pF[p], d_pF[p], 256.0, RMS_EPS, N=N)
            p = nextp()
            for t in range(ntile):
                nt_ = 16 if g < 0 else 128
                for j in range(2):
                    S.op('pe', lambda e, t=t, j=j, p=p, nt_=nt_: e.matmul(pF[p][0:nt_, 16 * t:16 * t + 16], lhsT=sqkv[:, j, t * 128:t * 128 + nt_], rhs=ones_b[:, 0:16],
                                                                        start=(j == 0), stop=(j == 1)),
                         reads=[d_sqkv, d_const], writes=[d_pF[p]])
            rsqrt_mean(rcol, d_rcol, pF[p], d_pF[p], 256.0, RMS_EPS, np_=(16 if g < 0 else 128), N=16 * ntile)
            if g >= 0:
                p = nextp()
                for j in range(2):
                    S.op('pe', lambda e, j=j, p=p: e.matmul(pF[p][:, 0:N], lhsT=ones_b[:, :], rhs=sqq[:, j, 0:N], start=(j == 0), stop=(j == 1)),
                         reads=[d_sqq, d_const], writes=[d_pF[p]])
                rsqrt_mean(rq, d_rq, pF[p], d_pF[p], 256.0, RMS_EPS, N=N)
            if g >= 0 and CUT <= 2.3:
                break
            for i in range(2):
                p = nextp()
                proj_fm(pF[p], d_pF[p], wkv_b, i * 128, ckvT, d_wkv, d_ckv, N, nk=2)
                S.op('dve', lambda e, i=i, p=p, k0=k0: e.tensor_tensor(out=KnT[:, i, k0:k0 + N], in0=pF[p][:, 0:N], in1=rkv[:, 0:N], op=ALU.mult),
                     reads=[d_pF[p], d_rkv], writes=[d_KnT])
            for t in range(ntile):
                nt_ = 16 if g < 0 else 128
                vt = 0 if g < 0 else 1 + g * 4 + t
                p = nextp()
                for kc in range(2):
                    S.op('pe', lambda e, t=t, kc=kc, p=p, nt_=nt_: e.matmul(pF[p][0:nt_, 0:256], lhsT=ckvT[:, kc, t * 128:t * 128 + nt_], rhs=wkv_b[:, kc, 256:512],
                                                                          start=(kc == 0), stop=(kc == 1)),
                         reads=[d_ckv, d_wkv], writes=[d_pF[p]])
                S.op('act', lambda e, t=t, p=p, nt_=nt_, vt=vt: e.activation(out=V[0:nt_, vt, :], in_=pF[p][0:nt_, 0:256], func=AF.Copy, scale=rcol[0:nt_, 16 * t:16 * t + 1]),
                     reads=[d_pF[p], d_rcol], writes=[d_V])
            if g < 0:
                if CUT <= 2:
                    break
                continue
            if CUT <= 2.4:
                break
            for i in range(2):
                p = nextp()
                proj_fm(pF[p], d_pF[p], wq_b, i * 128, cqT, d_wq, d_cq, N, nk=2)
                S.op('dve', lambda e, i=i, p=p: e.scalar_tensor_tensor(out=QnT[:, i, :], in0=pF[p][:, :], scalar=SCALE_MLA, in1=rq[:, :], op0=ALU.mult, op1=ALU.mult),
                     reads=[d_pF[p], d_rq], writes=[d_Qn])
            p = nextp()
            proj_fm(pF[p], d_pF[p], wq_b, 256, cqT, d_wq, d_cq, N, nk=2)
            S.op('dve', lambda e, p=p: e.tensor_tensor(out=t1[:, :], in0=pF[p][:, :], in1=cs[:, 0, :], op=ALU.mult), reads=[d_pF[p], d_cs], writes=[d_t1])
            p = nextp()
            proj_fm(pF[p], d_pF[p], wq_b, 384, cqT, d_wq, d_cq, N, nk=2)
            S.op('dve', lambda e, p=p: e.tensor_tensor(out=t2[:, :], in0=pF[p][:, :], in1=cs[:, 1, :], op=ALU.mult), reads=[d_pF[p], d_cs], writes=[d_t2])
            S.op('dve', lambda e: e.tensor_tensor(out=t1[:, :], in0=t1[:, :], in1=t2[:, :], op=ALU.add), reads=[d_t1, d_t2], writes=[d_t1])
            S.op('dve', lambda e: e.scalar_tensor_tensor(out=QrT[:, :], in0=t1[:, :], scalar=SCALE_MLA, in1=rq[:, :], op0=ALU.mult, op1=ALU.mult),
                 reads=[d_t1, d_rq], writes=[d_Qr])
            if CUT <= 3:
                break
            SK = 3
            tiles = []
            for i in range(2):
                blocks = [(-1, 0)] + [(kb, 0) for kb in range(4 * g)] + [(4 * g + j, j) for j in range(4)]
                for bi_, (kb, jq) in enumerate(blocks):
                    tiles.append((i, kb, jq, bi_ == 0, bi_ == len(blocks) - 1))
            pO, d_pO = pF[4], d_pF[4]
            pR, d_pR = pF[5], d_pF[5]

            def stA(ti):
                i, kb, jq, first, last = tiles[ti]
                nk_ = 16 if kb < 0 else 128
                kc0 = 0 if kb < 0 else 16 + kb * 128
                q0 = jq * 128
                pS, d_pS = pF[ti % 4], d_pF[ti % 4]
                pt, d_pt = PT[ti % 5], d_PT[ti % 5]
                S.op('pe', lambda e: e.matmul(pS[0:nk_, q0:512], lhsT=KnT[:, i, kc0:kc0 + nk_], rhs=QnT[:, i, q0:512], start=True, stop=False),
                     reads=[d_KnT, d_Qn], writes=[d_pS])
                S.op('pe', lambda e: e.matmul(pS[0:nk_, q0:512], lhsT=krT[i * 64:(i + 1) * 64, kc0:kc0 + nk_], rhs=QrT[i * 64:(i + 1) * 64, q0:512],
                                              start=False, stop=True), reads=[d_krT, d_Qr], writes=[d_pS])
                S.op('act', lambda e: e.activation(out=pt[0:nk_, q0:512], in_=pS[0:nk_, q0:512], func=AF.Exp), reads=[d_pS], writes=[d_pt])
                if kb >= 4 * g:
                    S.op('dve', lambda e: e.tensor_tensor(out=pt[:, q0:q0 + 128], in0=pt[:, q0:q0 + 128], in1=mle_b[:, :], op=ALU.mult),
                         reads=[d_pt, d_const], writes=[d_pt])

            def stB(ti):
                i, kb, jq, first, last = tiles[ti]
                nk_ = 16 if kb < 0 else 128
                vt = 0 if kb < 0 else 1 + kb
                q0 = jq * 128
                pt, d_pt = PT[ti % 5], d_PT[ti % 5]
                S.op('pe', lambda e: e.matmul(pO[:, q0:512], lhsT=V[0:nk_, vt, i * 128:(i + 1) * 128], rhs=pt[0:nk_, q0:512], start=first, stop=last,
                                              skip_group_check=True), reads=[d_V, d_pt], writes=[d_pO])
                S.op('pe', lambda e: e.matmul(pR[:, q0:512], lhsT=ones_b[0:nk_, :], rhs=pt[0:nk_, q0:512], start=first, stop=last,
                                              skip_group_check=True), reads=[d_const, d_pt], writes=[d_pR])
                if last:
                    S.op('dve', lambda e: e.reciprocal(out=rec[:, :], in_=pR[:, :]), reads=[d_pR], writes=[d_rec])
                    S.op('dve', lambda e: e.tensor_tensor(out=oT[i][:, :], in0=pO[:, :], in1=rec[:, :], op=ALU.mult), reads=[d_pO, d_rec], writes=[d_oT[i]])
                    h = heads[i]
                    S.dma('sp', lambda e: e.dma_start(out=omla_d[h, :, g * 512:(g + 1) * 512], in_=oT[i][:, :]), 'oT%d' % i, reads=[d_oT[i]])
            for s_ in range(len(tiles) + SK):
                if s_ < len(tiles):
                    stA(s_)
                if s_ >= SK:
                    stB(s_ - SK)
        S.barrier()

    def sb_phase(hsel):
        A = Arena(nc, PBASE, "s%d" % hsel)
        skT = A.alloc([128, 2, KT], BF16); d_skT = Dep()
        sv = A.alloc([128, NT + 1, 256], BF16); d_sv = Dep()
        win_b = A.alloc([128, 8, 768], BF16); d_win = Dep()
        xb = [A.alloc([128, 4, DM], BF16) for _ in range(2)]; d_xb = [Dep(), Dep()]
        XT = A.alloc([128, 8, 512], BF16); d_XT = Dep()
        sqT = A.alloc([128, 2, 512], BF16); d_sq = Dep()
        e32 = [A.alloc([128, 512], F32) for _ in range(3)]; d_e32 = [Dep() for _ in range(3)]
        spb = [A.alloc([128, 512], BF16) for _ in range(3)]; d_spb = [Dep() for _ in range(3)]
        wb = [A.alloc([128, 512], BF16) for _ in range(3)]; d_wb = [Dep() for _ in range(3)]
        L32 = A.alloc([128, 512], F32); d_L32 = Dep()
        Lb = [A.alloc([128, 512], BF16) for _ in range(3)]; d_Lb = [Dep() for _ in range(3)]
        oTs = [A.alloc([64, 512], F32) for _ in range(2)]; d_oTs = [Dep(), Dep()]
        c0 = 2 * hsel
        S.dma('pool', lambda e: e.dma_start(out=win_b[:, :, 0:256], in_=w_in_t[:, :, 576 + c0 * 128:576 + c0 * 128 + 256]), 'w1', writes=[d_win])
        S.dma('pool', lambda e: e.dma_start(out=win_b[:, :, 256:512], in_=w_in_t[:, :, 1088 + c0 * 128:1088 + c0 * 128 + 256]), 'w1', writes=[d_win])
        S.dma('pool', lambda e: e.dma_start(out=win_b[:, :, 512:768], in_=w_in_t[:, :, 1600 + c0 * 128:1600 + c0 * 128 + 256]), 'w1', writes=[d_win])
        load_x_bf(xb[0], d_xb[0], -1, 'xb0')
        pcount = [0]

        def nextp():
            pcount[0] += 1
            return pcount[0] % 4
        ocount = [0]
        for gi, g in enumerate([-1] + list(range(NG))):
            bi = gi % 2
            N = 16 if g < 0 else 512
            ntile = 1 if g < 0 else 4
            k0 = 0 if g < 0 else 16 + g * 512
            if gi + 1 <= NG:
                load_x_bf(xb[1 - bi], d_xb[1 - bi], g + 1, 'xb%d' % (1 - bi))
            transpose_x(xb[bi], d_xb[bi], XT, d_XT, g)
            for j in range(2):
                p = nextp()
                proj_fm(pF[p], d_pF[p], win_b, 256 + j * 128, XT, d_win, d_XT, N)
                S.op('dve', lambda e, j=j, p=p, k0=k0: e.tensor_copy(out=skT[:, j, k0:k0 + N], in_=pF[p][:, 0:N]), reads=[d_pF[p]], writes=[d_skT])
            for t in range(ntile):
                nt_ = 16 if g < 0 else 128
                vt = 0 if g < 0 else 1 + g * 4 + t
                p = nextp()
                for kc in range(8):
                    S.op('pe', lambda e, t=t, kc=kc, p=p, nt_=nt_: e.matmul(pF[p][0:nt_, 0:256], lhsT=XT[:, kc, t * 128:t * 128 + nt_], rhs=win_b[:, kc, 512:768],
                                                                          start=(kc == 0), stop=(kc == 7)),
                         reads=[d_XT, d_win], writes=[d_pF[p]])
                S.op('dve', lambda e, p=p, nt_=nt_, vt=vt: e.tensor_copy(out=sv[0:nt_, vt, :], in_=pF[p][0:nt_, 0:256]), reads=[d_pF[p]], writes=[d_sv])
            if g < 0:
                continue
            for j in range(2):
                p = nextp()
                proj_fm(pF[p], d_pF[p], win_b, j * 128, XT, d_win, d_XT, N)
                S.op('dve', lambda e, j=j, p=p: e.tensor_scalar(out=sqT[:, j, :], in0=pF[p][:, :], scalar1=0.125, scalar2=None, op0=ALU.mult),
                     reads=[d_pF[p]], writes=[d_sq])
            tiles = []
            for hh in range(4):
                blocks = [(4 * g + jq, jq) for jq in (3, 2, 1, 0)] + [(kb, 0) for kb in range(4 * g - 1, -1, -1)] + [(-1, 0)]
                for bi_, (kb, jq) in enumerate(blocks):
                    tiles.append((hh, kb, jq, bi_ == 0, bi_ == len(blocks) - 1))

            def geom(ti):
                hh, kb, jq, first, last = tiles[ti]
                nk_ = 16 if kb < 0 else 128
                kc0 = 0 if kb < 0 else 16 + kb * 128
                vt = 0 if kb < 0 else 1 + kb
                return hh, kb, jq * 128, first, last, nk_, kc0, vt, kb >= 4 * g

            def stA1(ti):
                hh, kb, q0, first, last, nk_, kc0, vt, diag = geom(ti)
                j = hh // 2
                hp = (hh % 2) * 64
                pS, d_pS = pF[ti % 4], d_pF[ti % 4]
                e_, d_e = e32[ti % 3], d_e32[ti % 3]
                if first:
                    pO, d_pO = pF[4 + hh % 2], d_pF[4 + hh % 2]
                    S.op('pe', lambda e: e.matmul(pO[0:64, :], lhsT=zeros_b[:, 0:64], rhs=zeros512[:, :], start=True, stop=False, skip_group_check=True),
                         reads=[d_const], writes=[d_pO])
                S.op('pe', lambda e: e.matmul(pS[0:nk_, q0:512], lhsT=skT[hp:hp + 64, j, kc0:kc0 + nk_], rhs=sqT[hp:hp + 64, j, q0:512],
                                              start=True, stop=False, skip_group_check=True), reads=[d_skT, d_sq], writes=[d_pS])
                S.op('act', lambda e: e.activation(out=e_[0:nk_, q0:512], in_=pS[0:nk_, q0:512], func=AF.Exp), reads=[d_pS], writes=[d_e])

            def stA2(ti):
                hh, kb, q0, first, last, nk_, kc0, vt, diag = geom(ti)
                e_, d_e = e32[ti % 3], d_e32[ti % 3]
                sp_, d_sp = spb[ti % 3], d_spb[ti % 3]
                S.op('act', lambda e: e.activation(out=sp_[0:nk_, q0:512], in_=e_[0:nk_, q0:512], func=AF.Ln, bias=1.0), reads=[d_e], writes=[d_sp])
                if diag:
                    S.op('dve', lambda e: e.tensor_tensor(out=sp_[:, q0:q0 + 128], in0=sp_[:, q0:q0 + 128], in1=mlt_b[:, :], op=ALU.mult),
                         reads=[d_sp, d_const], writes=[d_sp])
                if not last:
                    lprev = zeros512 if first else Lb[(ti - 1) % 3]
                    d_lprev = d_const if first else d_Lb[(ti - 1) % 3]
                    if q0 > 0:
                        S.op('dve', lambda e: e.tensor_copy(out=Lb[ti % 3][:, 0:q0], in_=lprev[:, 0:q0]), reads=[d_lprev], writes=[d_Lb[ti % 3]])
                    S.op('dve', lambda e: e.tensor_tensor(out=Lb[ti % 3][:, q0:512], in0=lprev[:, q0:512], in1=sp_[:, q0:512], op=ALU.add),
                         reads=[d_sp, d_lprev], writes=[d_Lb[ti % 3]])

            def stB(ti):
                hh, kb, q0, first, last, nk_, kc0, vt, diag = geom(ti)
                pS, d_pS = pF[ti % 4], d_pF[ti % 4]
                sp_, d_sp = spb[ti % 3], d_spb[ti % 3]
                w_, d_w = wb[ti % 3], d_wb[ti % 3]
                S.op('pe', lambda e: e.matmul(pS[0:nk_, q0:512], lhsT=negu_b[0:nk_, 0:nk_], rhs=sp_[0:nk_, q0:512], start=False, stop=first,
                                              skip_group_check=True), reads=[d_sp, d_const], writes=[d_pS])
                if not first:
                    lcur = (ti - 1) % 3
                    S.op('pe', lambda e: e.matmul(pS[0:nk_, q0:512], lhsT=negones_b[:, 0:nk_], rhs=Lb[lcur][:, q0:512], start=False, stop=True,
                                                  skip_group_check=True), reads=[d_Lb[lcur], d_const], writes=[d_pS])
                S.op('act', lambda e: e.activation(out=w_[0:nk_, q0:512], in_=pS[0:nk_, q0:512], func=AF.Exp), reads=[d_pS], writes=[d_w])
                if diag:
                    S.op('dve', lambda e: e.tensor_tensor(out=w_[:, q0:q0 + 128], in0=w_[:, q0:q0 + 128], in1=mlt_b[:, :], op=ALU.mult),
                         reads=[d_w, d_const], writes=[d_w])

            def stC(ti):
                hh, kb, q0, first, last, nk_, kc0, vt, diag = geom(ti)
                w_, d_w = wb[ti % 3], d_wb[ti % 3]
                pO, d_pO = pF[4 + hh % 2], d_pF[4 + hh % 2]
                S.op('pe', lambda e: e.matmul(pO[0:64, q0:512], lhsT=sv[0:nk_, vt, hh * 64:(hh + 1) * 64], rhs=w_[0:nk_, q0:512], start=False, stop=last,
                                              skip_group_check=True), reads=[d_sv, d_w], writes=[d_pO])
                if last:
                    oi = hh % 2
                    head = 4 * hsel + hh
                    S.op('dve', lambda e: e.tensor_copy(out=oTs[oi][:, :], in_=pO[0:64, :]), reads=[d_pO], writes=[d_oTs[oi]])
                    S.dma('sp', lambda e: e.dma_start(out=osb_d[head, :, g * 512:(g + 1) * 512], in_=oTs[oi][:, :]), 'oS%d' % oi, reads=[d_oTs[oi]])
            nt_all = len(tiles)
            for s_ in range(nt_all + 3):
                if s_ < nt_all:
                    stA1(s_)
                if 1 <= s_ <= nt_all:
                    stA2(s_ - 1)
                if 2 <= s_ <= nt_all + 1:
                    stB(s_ - 2)
                if s_ >= 3:
                    stC(s_ - 3)
        S.barrier()

    def merge_phase():
        A = Arena(nc, PBASE, "g")
        wo_f = A.alloc([128, 4, 512], F32); d_wof = Dep()
        wo_m = A.alloc([128, 4, DM], BF16)
        wo_s = A.alloc([64, 8, DM], BF16)
        d_wo = Dep()
        gm = A.alloc([128, 4], F32)
        gs = A.alloc([64, 8], F32)
        g1 = A.alloc([128, DM], F32)
        b1 = A.alloc([128, DM], F32)
        wr_f = A.alloc([128, 8, NE], F32)
        br_f = A.alloc([1, NE], F32)
        d_w = Dep()
        om = [A.alloc([128, 4, 512], F32) for _ in range(2)]; d_om = [Dep(), Dep()]
        os_ = [A.alloc([64, 8, 512], F32) for _ in range(2)]; d_os = [Dep(), Dep()]
        sqm = A.alloc([128, 4, 512], BF16); d_sqm = Dep()
        sqs = A.alloc([64, 8, 512], BF16); d_sqs = Dep()
        rrm = A.alloc([128, 512], F32); d_rrm = Dep()
        rrs = A.alloc([128, 512], F32); d_rrs = Dep()
        onm = A.alloc([128, 4, 512], BF16); d_onm = Dep()
        ons = A.alloc([64, 8, 512], BF16); d_ons = Dep()
        xt = [A.alloc([128, DM], F32) for _ in range(2)]; d_xt = [Dep(), Dep()]
        hp_l = [A.alloc([128, DM], F32) for _ in range(2)]; d_hp_l = [Dep(), Dep()]
        junk_l = [A.alloc([128, DM], BF16) for _ in range(2)]; d_junk_l = [Dep(), Dep()]
        h1 = [A.alloc([128, DM], F32) for _ in range(2)]; d_h1 = [Dep(), Dep()]
        h1b = [A.alloc([128, DM], BF16) for _ in range(2)]; d_h1b = [Dep(), Dep()]
        h1T = A.alloc([128, 8, 128], F32); d_h1T = Dep()
        st_l = [A.alloc([128, 16], F32) for _ in range(2)]; d_st_l = [Dep(), Dep()]
        lg = A.alloc([128, NE], F32); d_lg = Dep()
        top8 = A.alloc([128, 8], F32); d_top8 = Dep()
        idx8 = A.alloc([128, 8], U32); d_idx8 = Dep()
        idxf = A.alloc([128, 4], F32); d_idxf = Dep()
        e4 = A.alloc([128, 4], F32); d_e4 = Dep()
        maskb = A.alloc([128, NE], BF16); d_mask = Dep()
        rank = A.alloc([128, NE], F32); d_rank = Dep()
        base = A.alloc([128, NE], F32); d_base = Dep()
        oh = A.alloc([128, NE], F32); d_oh = Dep()
        oh2 = A.alloc([128, NE], F32)
        rsel = A.alloc([128, 4], F32); d_rsel = Dep()
        destf = A.alloc([128, 4], F32); d_destf = Dep()

        wo_t = w_o_d[:, :]
        S.dma('sp', lambda e: e.dma_start(out=gm[:], in_=gm_d[:, :]), 'w2', writes=[d_w])
        S.dma('sp', lambda e: e.dma_start(out=gs[:], in_=gs_d[:, :]), 'w2', writes=[d_w])
        S.dma('sp', lambda e: e.dma_start(out=g1[:], in_=ln1g_d[:, :]), 'w2', writes=[d_w])
        S.dma('sp', lambda e: e.dma_start(out=b1[:], in_=ln1b_d[:, :]), 'w2', writes=[d_w])
        S.dma('sp', lambda e: e.dma_start(out=wr_f[:], in_=wr_d[:, :].rearrange("(kc p) n -> p kc n", p=128)), 'w2', writes=[d_w])
        S.dma('sp', lambda e: e.dma_start(out=br_f[:], in_=br_d[:, :]), 'w2', writes=[d_w])
        for half in range(2):
            S.dma('sp', lambda e, half=half: e.dma_start(out=wo_f[:, :, :], in_=wo_t[0:512, half * 512:(half + 1) * 512].rearrange("(c p) n -> p c n", p=128)),
                  'w3', writes=[d_wof])
            for c in range(4):
                S.op('dve', lambda e, c=c, half=half: e.tensor_scalar(out=wo_m[:, c, half * 512:(half + 1) * 512], in0=wo_f[:, c, :], scalar1=gm[:, c:c + 1],
                                                                      scalar2=None, op0=ALU.mult), reads=[d_wof, d_w], writes=[d_wo])
            for q4 in range(2):
                S.dma('sp', lambda e, half=half, q4=q4: e.dma_start(
                    out=wo_f[0:64, :, :], in_=wo_t[512 + q4 * 256:512 + (q4 + 1) * 256, half * 512:(half + 1) * 512].rearrange("(c p) n -> p c n", p=64)),
                    'w3', writes=[d_wof])
                for c in range(4):
                    S.op('dve', lambda e, c=c, half=half, q4=q4: e.tensor_scalar(out=wo_s[:, q4 * 4 + c, half * 512:(half + 1) * 512], in0=wo_f[0:64, c, :],
                                                                              scalar1=gs[:, q4 * 4 + c:q4 * 4 + c + 1], scalar2=None, op0=ALU.mult),
                         reads=[d_wof, d_w], writes=[d_wo])
        S.op('pool', lambda e: e.memset(base[:], 0.0), writes=[d_base])

        def load_o(g, bi):
            S.dma('sp', lambda e: e.dma_start(out=om[bi][:, :, :], in_=omla_d[:, :, g * 512:(g + 1) * 512].rearrange("c p t -> p c t")), 'om%d' % bi, writes=[d_om[bi]])
            S.dma('sp', lambda e: e.dma_start(out=os_[bi][:, :, :], in_=osb_d[:, :, g * 512:(g + 1) * 512].rearrange("c p t -> p c t")), 'os%d' % bi, writes=[d_os[bi]])

        def load_xt(tt, bi):
            S.dma('sp', lambda e: e.dma_start(out=xt[bi][:, :], in_=x_d[tt * 128:(tt + 1) * 128, :]), 'xt%d' % bi, writes=[d_xt[bi]])
        load_o(0, 0)
        load_xt(0, 0)
        for g in range(NG):
            bi = g % 2
            if g + 1 < NG:
                load_o(g + 1, 1 - bi)
            for c2 in range(2):
                S.op('act', lambda e, c2=c2: e.activation(out=sqm[:, 2 * c2:2 * c2 + 2, :], in_=om[bi][:, 2 * c2:2 * c2 + 2, :], func=AF.Square), reads=[d_om[bi]], writes=[d_sqm])
            for c2 in range(4):
                S.op('act', lambda e, c2=c2: e.activation(out=sqs[:, 2 * c2:2 * c2 + 2, :], in_=os_[bi][:, 2 * c2:2 * c2 + 2, :], func=AF.Square), reads=[d_os[bi]], writes=[d_sqs])
            for c in range(4):
                S.op('pe', lambda e, c=c: e.matmul(pF[2][:, :], lhsT=ones_b[:, :], rhs=sqm[:, c, :], start=(c == 0), stop=(c == 3)), reads=[d_sqm, d_const], writes=[d_pF[2]])
            rsqrt_mean(rrm, d_rrm, pF[2], d_pF[2], 512.0, RMS_EPS)
            for c in range(8):
                S.op('pe', lambda e, c=c: e.matmul(pF[3][:, :], lhsT=ones_b[0:64, :], rhs=sqs[0:64, c, :], start=(c == 0), stop=(c == 7)), reads=[d_sqs, d_const], writes=[d_pF[3]])
            rsqrt_mean(rrs, d_rrs, pF[3], d_pF[3], 512.0, RMS_EPS)
            for c in range(4):
                S.op('dve', lambda e, c=c: e.tensor_tensor(out=onm[:, c, :], in0=om[bi][:, c, :], in1=rrm[:, :], op=ALU.mult), reads=[d_om[bi], d_rrm], writes=[d_onm])
            for c in range(8):
                S.op('dve', lambda e, c=c: e.tensor_tensor(out=ons[:, c, :], in0=os_[bi][:, c, :], in1=rrs[0:64, :], op=ALU.mult), reads=[d_os[bi], d_rrs], writes=[d_ons])
            for t in range(4):
                tt = g * 4 + t
                tb = tt % 2
                hp_, d_hp, junk, d_junk, st, d_st = hp_l[tb], d_hp_l[tb], junk_l[tb], d_junk_l[tb], st_l[tb], d_st_l[tb]
                if tt + 1 < NT:
                    load_xt(tt + 1, 1 - tb)
                S.op('pool', lambda e: e.memset(st[:, :], 0.0), writes=[d_st])
                for half in range(2):
                    pa, d_pa = pF[half], d_pF[half]
                    for c in range(4):
                        S.op('pe', lambda e, c=c, t=t, half=half, pa=pa: e.matmul(pa[:, :], lhsT=onm[:, c, t * 128:(t + 1) * 128], rhs=wo_m[:, c, half * 512:(half + 1) * 512],
                                                                                 start=(c == 0), stop=False), reads=[d_onm, d_wo], writes=[d_pa])
                    for c in range(8):
                        S.op('pe', lambda e, c=c, t=t, half=half, pa=pa: e.matmul(pa[:, :], lhsT=ons[0:64, c, t * 128:(t + 1) * 128], rhs=wo_s[0:64, c, half * 512:(half + 1) * 512],
                                                                                 start=False, stop=(c == 7)), reads=[d_ons, d_wo], writes=[d_pa])
                    S.op('dve', lambda e, half=half, pa=pa, tb=tb: e.scalar_tensor_tensor(out=hp_[:, half * 512:(half + 1) * 512], in0=xt[tb][:, half * 512:(half + 1) * 512],
                                                                                        scalar=ALPHA, in1=pa[:, :], op0=ALU.mult, op1=ALU.add, accum_out=st[:, half:half + 1]),
                         reads=[d_xt[tb], d_pa, d_st], writes=[d_hp, d_st])
                layer_norm(hp_, d_hp, st, d_st, junk, d_junk, g1, b1, d_w, h1[tb], d_h1[tb])
                S.dma('sp', lambda e, tt=tt, tb=tb: e.dma_start(out=h1_d[tt * 128:(tt + 1) * 128, :], in_=h1[tb][:, :]), 'h1o%d' % tb, reads=[d_h1[tb]])
                S.op('act', lambda e, tb=tb: e.activation(out=h1b[tb][:, :], in_=h1[tb][:, :], func=AF.Copy), reads=[d_h1[tb]], writes=[d_h1b[tb]])
                for hf in range(2):
                    pX, d_pX = pF[4 + hf], d_pF[4 + hf]
                    for c in range(4):
                        cc = hf * 4 + c
                        S.op('pe', lambda e, c=c, cc=cc, pX=pX, tb=tb: e.transpose(out=pX[:, c * 128:(c + 1) * 128], in_=h1[tb][:, cc * 128:(cc + 1) * 128], identity=ident_f[:, :]),
                             reads=[d_h1[tb], d_const], writes=[d_pX])
                    S.op('act', lambda e, hf=hf, pX=pX: e.activation(out=h1T[:, hf * 4:(hf + 1) * 4, :], in_=pX[:, :].rearrange("p (c n) -> p c n", c=4), func=AF.Copy),
                         reads=[d_pX], writes=[d_h1T])
                pL, d_pL = pF[2], d_pF[2]
                for c in range(8):
                    S.op('pe', lambda e, c=c: e.matmul(pL[:, 0:NE], lhsT=h1T[:, c, :], rhs=wr_f[:, c, :], start=(c == 0), stop=False), reads=[d_h1T, d_w], writes=[d_pL])
                S.op('pe', lambda e: e.matmul(pL[:, 0:NE], lhsT=ones_f[0:1, :], rhs=br_f[0:1, :], start=False, stop=True), reads=[d_const, d_w], writes=[d_pL])
                S.op('dve', lambda e: e.tensor_copy(out=lg[:, :], in_=pL[:, 0:NE]), reads=[d_pL], writes=[d_lg])
                S.op('dve', lambda e: e.max(out=top8[:, :], in_=lg[:, :]), reads=[d_lg], writes=[d_top8])
                S.op('dve', lambda e: e.max_index(out=idx8[:, :], in_max=top8[:, :], in_values=lg[:, :]), reads=[d_lg, d_top8], writes=[d_idx8])
                S.op('dve', lambda e: e.tensor_copy(out=idxf[:, :], in_=idx8[:, 0:4]), reads=[d_idx8], writes=[d_idxf])
                S.op('dve', lambda e: e.tensor_scalar(out=st[:, 8:9], in0=top8[:, 0:1], scalar1=-1.0, scalar2=None, op0=ALU.mult), reads=[d_top8, d_st], writes=[d_st])
                S.op('act', lambda e: e.activation(out=e4[:, :], in_=top8[:, 0:4], func=AF.Exp, bias=st[:, 8:9], scale=1.0, accum_out=st[:, 9:10]),
                     reads=[d_top8, d_st], writes=[d_e4, d_st])
                S.op('dve', lambda e: e.reciprocal(out=st[:, 10:11], in_=st[:, 9:10]), reads=[d_st], writes=[d_st])
                S.op('dve', lambda e, tt=tt: e.tensor_scalar(out=gtab[:, tt, :], in0=e4[:, :], scalar1=st[:, 10:11], scalar2=None, op0=ALU.mult),
                     reads=[d_e4, d_st], writes=[d_gtab])
                S.op('dve', lambda e: e.tensor_scalar(out=maskb[:, :], in0=lg[:, :], scalar1=top8[:, 3:4], scalar2=None, op0=ALU.is_ge), reads=[d_lg, d_top8], writes=[d_mask])
                pK, d_pK = pF[3], d_pF[3]
                S.op('pe', lambda e: e.matmul(pK[:, 0:NE], lhsT=lst_b[:, :], rhs=maskb[:, :], start=True, stop=True), reads=[d_mask, d_const], writes=[d_pK])
                S.op('pe', lambda e: e.matmul(pK[:, NE:2 * NE], lhsT=ones_b[:, :], rhs=maskb[:, :], start=True, stop=True), reads=[d_mask, d_const], writes=[d_pK])
                S.op('dve', lambda e: e.tensor_tensor(out=rank[:, :], in0=pK[:, 0:NE], in1=base[:, :], op=ALU.add), reads=[d_pK, d_base], writes=[d_rank])
                S.op('dve', lambda e: e.tensor_tensor(out=base[:, :], in0=pK[:, NE:2 * NE], in1=base[:, :], op=ALU.add), reads=[d_pK, d_base], writes=[d_base])
                S.op('pool', lambda e: e.memset(rsel[:, :], 0.0), writes=[d_rsel])
                for k in range(4):
                    S.op('dve', lambda e, k=k: e.tensor_scalar(out=oh[:, :], in0=iota_f[:, :], scalar1=idxf[:, k:k + 1], scalar2=None, op0=ALU.is_equal),
                         reads=[d_const, d_idxf], writes=[d_oh])
                    S.op('dve', lambda e, k=k: e.scalar_tensor_tensor(out=oh2[:, :], in0=oh[:, :], scalar=1.0, in1=rank[:, :], op0=ALU.mult, op1=ALU.mult,
                                                                      accum_out=rsel[:, k:k + 1]), reads=[d_oh, d_rank, d_rsel], writes=[d_rsel, d_oh])
                S.op('dve', lambda e: e.tensor_scalar(out=rsel[:, :], in0=rsel[:, :], scalar1=float(CAP - 1), scalar2=None, op0=ALU.min), reads=[d_rsel], writes=[d_rsel])
                S.op('dve', lambda e: e.scalar_tensor_tensor(out=destf[:, :], in0=idxf[:, :], scalar=float(CAP), in1=rsel[:, :], op0=ALU.mult, op1=ALU.add),
                     reads=[d_idxf, d_rsel], writes=[d_destf])
                S.op('dve', lambda e, tt=tt: e.tensor_copy(out=dtab[:, tt, :], in_=destf[:, :]), reads=[d_destf], writes=[d_dtab])
                if debug:
                    S.dma('sp', lambda e, tt=tt: e.dma_start(out=dbg_d[tt * 128:(tt + 1) * 128, 0:4], in_=destf[:, :]), 'dbg%d' % tb, reads=[d_destf])
                    S.dma('sp', lambda e, tt=tt: e.dma_start(out=dbg_d[tt * 128:(tt + 1) * 128, 4:8], in_=gtab[:, tt, :]), 'dbg%d' % tb, reads=[d_gtab])
                S.wait_dma('pool', 'sc%d' % (1 - tb))
                for k in range(4):
                    S.dma('pool', lambda e, tt=tt, k=k, tb=tb: e.indirect_dma_start(
                        out=xbuf_d[:, :], out_offset=bass.IndirectOffsetOnAxis(ap=dtab[:, tt, k:k + 1], axis=0), in_=h1b[tb][:, :], in_offset=None), 'sc%d' % tb, reads=[d_h1b[tb], d_dtab, d_xbufz])
        S.barrier()

    def layer_norm(hp_, d_hp, st, d_st, junk, d_junk, gt, bt, d_gb, out, d_out):
        S.op('dve', lambda e: e.tensor_tensor(out=st[:, 2:3], in0=st[:, 0:1], in1=st[:, 1:2], op=ALU.add), reads=[d_st], writes=[d_st])
        S.op('dve', lambda e: e.tensor_scalar(out=st[:, 2:3], in0=st[:, 2:3], scalar1=-1.0 / DM, scalar2=None, op0=ALU.mult), reads=[d_st], writes=[d_st])
        S.op('act', lambda e: e.activation(out=junk[:, :], in_=hp_[:, :], func=AF.Square, bias=st[:, 2:3], scale=1.0, accum_out=st[:, 3:4]),
             reads=[d_hp, d_st], writes=[d_junk, d_st])
        S.op('act', lambda e: e.activation(out=st[:, 4:5], in_=st[:, 3:4], func=AF.Sqrt, bias=float(LN_EPS), scale=1.0 / DM), reads=[d_st], writes=[d_st])
        S.op('dve', lambda e: e.reciprocal(out=st[:, 4:5], in_=st[:, 4:5]), reads=[d_st], writes=[d_st])
        S.op('dve', lambda e: e.tensor_tensor(out=st[:, 5:6], in0=st[:, 2:3], in1=st[:, 4:5], op=ALU.mult), reads=[d_st], writes=[d_st])
        S.op('act', lambda e: e.activation(out=out[:, :], in_=hp_[:, :], func=AF.Identity, bias=st[:, 5:6], scale=st[:, 4:5]),
             reads=[d_hp, d_st], writes=[d_out])
        S.op('dve', lambda e: e.tensor_tensor(out=out[:, :], in0=out[:, :], in1=gt[:, :], op=ALU.mult), reads=[d_out, d_gb], writes=[d_out])
        S.op('dve', lambda e: e.tensor_tensor(out=out[:, :], in0=out[:, :], in1=bt[:, :], op=ALU.add), reads=[d_out, d_gb], writes=[d_out])

    def moe_phase():
        A = Arena(nc, PBASE, "e")
        wg_b = [A.alloc([128, 8, DM], BF16) for _ in range(2)]
        wu_b = [A.alloc([128, 8, DM], BF16) for _ in range(2)]
        wd_b = [A.alloc([128, 8, DM], BF16) for _ in range(2)]
        d_wg = [Dep(), Dep()]; d_wu = [Dep(), Dep()]; d_wd = [Dep(), Dep()]
        bgu = A.alloc([128, NE, 16], F32); d_bgu = Dep()
        bd_f = [A.alloc([1, DM], F32) for _ in range(2)]; d_bdf = [Dep(), Dep()]
        bdb = [A.alloc([128, DM], F32) for _ in range(2)]; d_bdb = [Dep(), Dep()]
        xs = [A.alloc([128, 4, DM], BF16) for _ in range(2)]; d_xs = [Dep(), Dep()]
        XsT = A.alloc([128, 8, 512], BF16); d_XsT = Dep()
        g32 = [A.alloc([128, 512], F32) for _ in range(2)]; d_g32 = [Dep(), Dep()]
        s32 = [A.alloc([128, 512], F32) for _ in range(2)]; d_s32 = [Dep(), Dep()]
        u32 = [A.alloc([128, 512], F32) for _ in range(2)]; d_u32 = [Dep(), Dep()]
        aT = [A.alloc([128, 8, 512], BF16) for _ in range(2)]; d_aT = [Dep(), Dep()]
        ys = [A.alloc([128, DM], F32) for _ in range(2)]; d_ys = [Dep(), Dep()]
        S.dma('sp', lambda e: e.dma_start(out=bgu[:], in_=bgu_d[:, :, :]), 'w2', writes=[d_bgu])

        def load_w(ex, bi):
            for (wt, src, dd, key) in ((wg_b, wg_d, d_wg, 'we'), (wu_b, wu_d, d_wg, 'we'), (wd_b, wd_d, d_wg, 'we')):
                for hf in range(2):
                    S.dma('pool', lambda e, wt=wt, src=src, hf=hf: e.dma_start(out=wt[bi][:, hf * 4:(hf + 1) * 4, :],
                                                                                in_=src[ex, hf * 512:(hf + 1) * 512, :].rearrange("(kc p) f -> p kc f", p=128)),
                          '%s%d' % (key, bi), writes=[dd[bi]])
            S.dma('sp', lambda e: e.dma_start(out=bd_f[bi][:, :], in_=bd_d[ex:ex + 1, :]), 'bd%d' % bi, writes=[d_bdf[bi]])
        groups = []
        off = 0
        while off < CAP:
            n = min(512, CAP - off)
            groups.append((off, n))
            off += n
        load_w(0, 0)
        gcount = 0
        ycount = 0
        for ex in range(NE):
            wi = ex % 2
            if ex + 1 < NE:
                load_w(ex + 1, 1 - wi)
            for half in range(2):
                S.op('pe', lambda e, half=half: e.matmul(pF[5][:, :], lhsT=ones_f[0:1, :], rhs=bd_f[wi][0:1, half * 512:(half + 1) * 512], start=True, stop=True),
                     reads=[d_const, d_bdf[wi]], writes=[d_pF[5]])
                S.op('act', lambda e, half=half: e.activation(out=bdb[wi][:, half * 512:(half + 1) * 512], in_=pF[5][:, :], func=AF.Copy), reads=[d_pF[5]], writes=[d_bdb[wi]])
            for (soff, N) in groups:
                xi = gcount % 2
                gcount += 1
                ntile = N // 128
                row0 = ex * CAP + soff
                S.dma('sp', lambda e, xi=xi, row0=row0, ntile=ntile, N=N: e.dma_start(out=xs[xi][:, 0:ntile, :],
                                                                                     in_=xbuf_d[row0:row0 + N, :].rearrange("(t p) d -> p t d", p=128)),
                      'xs%d' % xi, writes=[d_xs[xi]])
                for t in range(ntile):
                    pi = t % 2
                    for c in range(8):
                        S.op('pe', lambda e, t=t, c=c, pi=pi, xi=xi: e.transpose(out=pT[pi][:, c * 128:(c + 1) * 128], in_=xs[xi][:, t, c * 128:(c + 1) * 128], identity=ident_b[:, :]),
                             reads=[d_xs[xi], d_const], writes=[d_pT[pi]])
                    S.op('dve', lambda e, t=t, pi=pi: e.tensor_copy(out=XsT[:, :, t * 128:(t + 1) * 128], in_=pT[pi][:, :].rearrange("p (c n) -> p c n", c=8)),
                         reads=[d_pT[pi]], writes=[d_XsT])
                ai = gcount % 2
                for fc in range(8):
                    fi = fc % 2
                    for kc in range(8):
                        S.op('pe', lambda e, fc=fc, kc=kc, N=N: e.matmul(pF[0][:, 0:N], lhsT=wg_b[wi][:, kc, fc * 128:(fc + 1) * 128], rhs=XsT[:, kc, 0:N], start=(kc == 0), stop=(kc == 7)),
                             reads=[d_wg[wi], d_XsT], writes=[d_pF[0]])
                    for kc in range(8):
                        S.op('pe', lambda e, fc=fc, kc=kc, N=N: e.matmul(pF[1][:, 0:N], lhsT=wu_b[wi][:, kc, fc * 128:(fc + 1) * 128], rhs=XsT[:, kc, 0:N], start=(kc == 0), stop=(kc == 7)),
                             reads=[d_wg[wi], d_XsT], writes=[d_pF[1]])
                    S.op('dve', lambda e, fc=fc, fi=fi, N=N, ex=ex: e.tensor_scalar(out=g32[fi][:, 0:N], in0=pF[0][:, 0:N], scalar1=bgu[:, ex, fc:fc + 1], scalar2=7.0, op0=ALU.add, op1=ALU.min),
                         reads=[d_pF[0], d_bgu], writes=[d_g32[fi]])
                    S.op('act', lambda e, fi=fi, N=N: e.activation(out=s32[fi][:, 0:N], in_=g32[fi][:, 0:N], func=AF.Silu, scale=1.702), reads=[d_g32[fi]], writes=[d_s32[fi]])
                    S.op('dve', lambda e, fc=fc, fi=fi, N=N, ex=ex: e.tensor_scalar(out=u32[fi][:, 0:N], in0=pF[1][:, 0:N], scalar1=bgu[:, ex, 8 + fc:9 + fc], scalar2=7.0, op0=ALU.add, op1=ALU.min),
                         reads=[d_pF[1], d_bgu], writes=[d_u32[fi]])
                    S.op('dve', lambda e, fi=fi, N=N: e.tensor_scalar(out=u32[fi][:, 0:N], in0=u32[fi][:, 0:N], scalar1=-7.0, scalar2=1.0, op0=ALU.max, op1=ALU.add),
                         reads=[d_u32[fi]], writes=[d_u32[fi]])
                    S.op('dve', lambda e, fc=fc, fi=fi, N=N, ai=ai: e.scalar_tensor_tensor(out=aT[ai][:, fc, 0:N], in0=s32[fi][:, 0:N], scalar=1.0 / 1.702, in1=u32[fi][:, 0:N],
                                                                                          op0=ALU.mult, op1=ALU.mult),
                         reads=[d_s32[fi], d_u32[fi]], writes=[d_aT[ai]])
                for t in range(ntile):
                    yi = ycount % 2
                    ycount += 1
                    for half in range(2):
                        pY, d_pY = pF[2 + half], d_pF[2 + half]
                        for fc in range(8):
                            S.op('pe', lambda e, t=t, fc=fc, half=half, pY=pY, ai=ai: e.matmul(pY[:, :], lhsT=aT[ai][:, fc, t * 128:(t + 1) * 128], rhs=wd_b[wi][:, fc, half * 512:(half + 1) * 512],
                                                                                              start=(fc == 0), stop=(fc == 7)), reads=[d_aT[ai], d_wg[wi]], writes=[d_pY])
                        S.op('dve', lambda e, half=half, pY=pY, yi=yi: e.tensor_tensor(out=ys[yi][:, half * 512:(half + 1) * 512], in0=pY[:, :], in1=bdb[wi][:, half * 512:(half + 1) * 512], op=ALU.add),
                             reads=[d_pY, d_bdb[wi]], writes=[d_ys[yi]])
                    r0 = row0 + t * 128
                    S.dma('sp', lambda e, yi=yi, r0=r0: e.dma_start(out=ybuf_d[r0:r0 + 128, :], in_=ys[yi][:, :]), 'yo%d' % yi, reads=[d_ys[yi]])
        S.barrier()

    def comb_phase():
        A = Arena(nc, PBASE, "f")
        g2 = A.alloc([128, DM], F32)
        b2 = A.alloc([128, DM], F32)
        d_gb = Dep()
        yk = [[A.alloc([128, DM], F32) for _ in range(4)] for _ in range(2)]
        d_yk = [[Dep() for _ in range(4)] for _ in range(2)]
        hh = [A.alloc([128, DM], F32) for _ in range(2)]; d_hh = [Dep(), Dep()]
        acc_l = [A.alloc([128, DM], F32) for _ in range(2)]; d_acc_l = [Dep(), Dep()]
        junk_l = [A.alloc([128, DM], BF16) for _ in range(2)]; d_junk_l = [Dep(), Dep()]
        st_l = [A.alloc([128, 16], F32) for _ in range(2)]; d_st_l = [Dep(), Dep()]
        ot = [A.alloc([128, DM], F32) for _ in range(2)]; d_ot = [Dep(), Dep()]
        S.dma('sp', lambda e: e.dma_start(out=g2[:], in_=ln2g_d[:, :]), 'w2', writes=[d_gb])
        S.dma('sp', lambda e: e.dma_start(out=b2[:], in_=ln2b_d[:, :]), 'w2', writes=[d_gb])

        def load(tt, bi):
            S.dma('sp', lambda e: e.dma_start(out=hh[bi][:, :], in_=h1_d[tt * 128:(tt + 1) * 128, :]), 'hh%d' % bi, writes=[d_hh[bi]])
            S.wait_dma('pool', 'yk%d' % (1 - bi))
            for k in range(4):
                S.dma('pool', lambda e, k=k: e.indirect_dma_start(out=yk[bi][k][:, :], out_offset=None, in_=ybuf_d[:, :],
                                                                  in_offset=bass.IndirectOffsetOnAxis(ap=dtab[:, tt, k:k + 1], axis=0)),
                      'yk%d' % bi, reads=[d_dtab], writes=[d_yk[bi][0]])
        load(0, 0)
        for tt in range(NT):
            bi = tt % 2
            acc, d_acc, junk, d_junk, st, d_st = acc_l[bi], d_acc_l[bi], junk_l[bi], d_junk_l[bi], st_l[bi], d_st_l[bi]
            if tt + 1 < NT:
                load(tt + 1, 1 - bi)
            S.op('pool', lambda e: e.memset(st[:, :], 0.0), writes=[d_st])
            S.op('act', lambda e: e.activation(out=acc[:, :], in_=hh[bi][:, :], func=AF.Copy, scale=ALPHA), reads=[d_hh[bi]], writes=[d_acc])
            for k in range(3):
                eng = 'dve'
                S.op(eng, lambda e, k=k: e.scalar_tensor_tensor(out=acc[:, :], in0=yk[bi][k][:, :], scalar=gtab[:, tt, k:k + 1], in1=acc[:, :], op0=ALU.mult, op1=ALU.add),
                     reads=[d_yk[bi][0], d_gtab, d_acc], writes=[d_acc])
            S.op('dve', lambda e: e.scalar_tensor_tensor(out=acc[:, :], in0=yk[bi][3][:, :], scalar=gtab[:, tt, 3:4], in1=acc[:, :], op0=ALU.mult, op1=ALU.add,
                                                          accum_out=st[:, 0:1]), reads=[d_yk[bi][0], d_gtab, d_acc, d_st], writes=[d_acc, d_st])
            layer_norm(acc, d_acc, st, d_st, junk, d_junk, g2, b2, d_gb, ot[bi], d_ot[bi])
            S.dma('sp', lambda e, tt=tt: e.dma_start(out=out_d[tt * 128:(tt + 1) * 128, :], in_=ot[bi][:, :]), 'out%d' % bi, reads=[d_ot[bi]])

    for ph in phases:
        if ph == 'mla0':
            mla_phase(0)
        elif ph == 'mla1':
            mla_phase(1)
        elif ph == 'sb0':
            sb_phase(0)
        elif ph == 'sb1':
            sb_phase(1)
        elif ph == 'merge':
            merge_phase()
        elif ph == 'moe':
            moe_phase()
        elif ph == 'comb':
            comb_phase()
    S.barrier()

    from contextlib import ExitStack
    with ExitStack() as es:
        sems = {}
        for k in S.semkeys:
            nm = "s_" + "_".join(str(a) for a in k)
            sems[k] = es.enter_context(nc.semaphore(nm))
        with nc.Block() as block:
            S.emit(block, sems)
    return nc, S


def rope_tables(npos):
    half = 32
    freqs = (np.float32(10000.0) ** (-(np.arange(half, dtype=np.float32) * np.float32(2.0)) / np.float32(64))).astype(np.float32)
    ang = (np.arange(npos, dtype=np.float32)[:, None] * freqs[None, :]).astype(np.float32)
    c = np.cos(ang.astype(np.float64)).astype(np.float32).T
    s = np.sin(ang.astype(np.float64)).astype(np.float32).T
    return np.ascontiguousarray(np.tile(c, (4, 1))), np.ascontiguousarray(np.tile(s, (4, 1)))


def make_consts(T):
    k = np.arange(128)
    cos2, sin2 = rope_tables(T + 16)
    return {
        "ident": np.eye(128, dtype=np.float32),
        "mle": (k[:, None] <= k[None, :]).astype(np.float32),
        "mlt": (k[:, None] < k[None, :]).astype(np.float32),
        "negu": -(k[:, None] >= k[None, :]).astype(np.float32),
        "lst": (k[:, None] < k[None, :]).astype(np.float32),
        "iota": np.tile(np.arange(NE, dtype=np.float32)[None, :], (128, 1)),
        "cos2": cos2, "sin2": sin2,
    }


def make_shared(inp):
    f = lambda a: np.ascontiguousarray(a, dtype=np.float32)
    d = {}
    d["meta"] = f(inp["meta_tokens"])
    d["w_in"] = f(inp["w_in"][0])
    d["qg"] = f(inp["q_norm_g"][0].reshape(2, 128).T)
    d["w_uq"] = f(inp["w_uq"][0])
    d["kvg"] = f(inp["kv_norm_g"][0].reshape(2, 128).T)
    d["w_ukv"] = f(inp["w_ukv"][0])
    d["gm"] = f(inp["mla_out_g"][0].reshape(4, 128).T)
    d["gs"] = f(inp["sb_out_g"][0].reshape(8, 64).T)
    d["w_o"] = f(inp["w_o"][0])
    d["ln1g"] = f(np.tile(inp["ln1_g"][0][None, :], (128, 1)))
    d["ln1b"] = f(np.tile(inp["ln1_b"][0][None, :], (128, 1)))
    d["wr"] = f(inp["w_router"][0])
    d["br"] = f(inp["b_router"][0][None, :])
    d["wg"] = f(inp["w_gate"][0])
    d["wu"] = f(inp["w_up"][0])
    d["wd"] = f(inp["w_down"][0])
    bg = np.asarray(inp["b_gate"][0]).reshape(NE, 8, 128).transpose(2, 0, 1)
    bu = np.asarray(inp["b_up"][0]).reshape(NE, 8, 128).transpose(2, 0, 1)
    d["bgu"] = f(np.concatenate([bg, bu], axis=2))
    d["bd"] = f(inp["b_down"][0])
    d["ln2g"] = f(np.tile(inp["ln2_g"][0][None, :], (128, 1)))
    d["ln2b"] = f(np.tile(inp["ln2_b"][0][None, :], (128, 1)))
    return d


CAP_FULL = 1280


def kernel(**inputs):
    x = np.asarray(inputs["x"])
    B, L, _ = x.shape
    NG = L // 512
    nc, _ = build(NG, CAP_FULL)
    shared = make_shared(inputs)
    shared.update(make_consts(L))
    in_maps = []
    for b in range(B):
        m = dict(shared)
        m["x"] = np.ascontiguousarray(x[b], dtype=np.float32)
        in_maps.append(m)
    res = run_bass_kernel_spmd(nc, in_maps, core_ids=list(range(B)))
    return np.stack([np.asarray(r["out"]) for r in res.results], axis=0).astype(np.float32)
```
